# Optimizing a Trainium2 kernel written in Bass

```python
import math
import jax, jax.numpy as jnp
from jax import lax
import numpy as np

D_MODEL = 1024
BATCH = 2
SEQ = 8192
DEPTH = 2

CHUNK = 64
Q_BLOCK = 128
RMS_EPS = 1e-6
CONV_WIDTH = 4

MLA_HEADS = 4
MLA_NOPE = 128
MLA_ROPE = 64
MLA_V = 128
MLA_QK_DIM = MLA_NOPE + MLA_ROPE
MLA_Q_RANK = 512
MLA_KV_RANK = 256
MLA_WIDTH = MLA_HEADS * MLA_V
MLA_SCALE = MLA_QK_DIM ** -0.5
ROPE_THETA = 10000.0

LRU_WIDTH = 512
LRU_BLOCKS = 8
LRU_BLOCK = LRU_WIDTH // LRU_BLOCKS
LRU_C = 8.0

GDN_HEADS = 4
GDN_DK = 128
GDN_DV = 128
GDN_WIDTH = GDN_HEADS * GDN_DV

D_MIX = MLA_WIDTH + LRU_WIDTH + GDN_WIDTH
IN_WIDTHS = (MLA_Q_RANK, MLA_KV_RANK, MLA_ROPE, LRU_WIDTH, LRU_WIDTH, GDN_HEADS * GDN_DK, GDN_HEADS * GDN_DK, GDN_WIDTH, GDN_WIDTH, GDN_HEADS, GDN_HEADS)
D_IN = sum(IN_WIDTHS)

D_FF_DENSE = 2816
N_EXPERTS = 8
TOP_K = 2
D_FF_EXPERT = 3584
N_DENSE = (DEPTH + 1) // 2
N_MOE = DEPTH // 2

kernel_name = "hybrid_mla_rglru_gdn_moe_trunk"


def rmsnorm(x, gain):
    xf = x.astype(jnp.float32)
    y = xf * lax.rsqrt(jnp.mean(xf * xf, axis=-1, keepdims=True) + RMS_EPS)
    return (y * gain.astype(jnp.float32)).astype(x.dtype)


def l2norm(x):
    return x * lax.rsqrt(jnp.sum(x * x, axis=-1, keepdims=True) + RMS_EPS)


def causal_depthwise_conv(x, w):
    c = x.shape[-1]
    return lax.conv_general_dilated(x, w[:, None, :].astype(x.dtype), window_strides=(1,), padding=[(w.shape[0] - 1, 0)], dimension_numbers=('NWC', 'WIO', 'NWC'), feature_group_count=c)


def rope_tables(positions):
    inv_freq = ROPE_THETA ** (-jnp.arange(0, MLA_ROPE, 2, dtype=jnp.float32) / MLA_ROPE)
    ang = positions.astype(jnp.float32)[..., None] * inv_freq
    return jnp.cos(ang), jnp.sin(ang)


def apply_rope(x, cos, sin):
    half = x.shape[-1] // 2
    x1 = x[..., :half].astype(jnp.float32)
    x2 = x[..., half:].astype(jnp.float32)
    c = cos[:, :, None, :]
    s = sin[:, :, None, :]
    return jnp.concatenate([x1 * c - x2 * s, x2 * c + x1 * s], axis=-1).astype(x.dtype)


def chunk_causal_attention(q, k, v):
    b, s, h, dqk = q.shape
    n_blocks = s // Q_BLOCK
    q_blocks = jnp.moveaxis(q.reshape(b, n_blocks, Q_BLOCK, h, dqk), 1, 0)
    key_chunk = jnp.arange(s) // CHUNK

    def one_block(args):
        blk, q_blk = args
        scores = jnp.einsum('bqhd,bkhd->bhqk', q_blk, k, preferred_element_type=jnp.float32) * MLA_SCALE
        q_chunk = (blk * Q_BLOCK + jnp.arange(Q_BLOCK)) // CHUNK
        visible = q_chunk[:, None] >= key_chunk[None, :]
        probs = jax.nn.softmax(jnp.where(visible, scores, -jnp.inf), axis=-1)
        return jnp.einsum('bhqk,bkhd->bqhd', probs.astype(v.dtype), v)

    out = lax.map(one_block, (jnp.arange(n_blocks), q_blocks))
    return jnp.moveaxis(out, 0, 1).reshape(b, s, h, v.shape[-1])


def mla_mixer(c_q, c_kv, k_rope, cos, sin, q_norm, w_uq, kv_norm, w_ukv, q_head_norm, k_head_norm, out_norm):
    b, s, _ = c_q.shape
    q = (rmsnorm(c_q, q_norm) @ w_uq).reshape(b, s, MLA_HEADS, MLA_QK_DIM)
    kv = (rmsnorm(c_kv, kv_norm) @ w_ukv).reshape(b, s, MLA_HEADS, MLA_NOPE + MLA_V)
    k_nope, v = kv[..., :MLA_NOPE], kv[..., MLA_NOPE:]
    k_pe = jnp.broadcast_to(k_rope[:, :, None, :], (b, s, MLA_HEADS, MLA_ROPE))
    k = jnp.concatenate([k_nope, k_pe], axis=-1)
    q = rmsnorm(q, q_head_norm)
    k = rmsnorm(k, k_head_norm)
    q = jnp.concatenate([q[..., :MLA_NOPE], apply_rope(q[..., MLA_NOPE:], cos, sin)], axis=-1)
    k = jnp.concatenate([k[..., :MLA_NOPE], apply_rope(k[..., MLA_NOPE:], cos, sin)], axis=-1)
    o = chunk_causal_attention(q, k, v)
    return rmsnorm(o.reshape(b, s, MLA_WIDTH), out_norm)


def linear_recurrence(a, u):
    def combine(left, right):
        a_l, b_l = left
        a_r, b_r = right
        return a_l * a_r, a_r * b_l + b_r
    _, h = lax.associative_scan(combine, (a, u), axis=1)
    return h


def rglru_mixer(x_in, gate_in, conv_w, conv_b, w_a, b_a, w_x, b_x, lam, out_norm):
    b, s, _ = x_in.shape
    xc = (causal_depthwise_conv(x_in, conv_w) + conv_b).astype(jnp.float32)
    xg = xc.reshape(b, s, LRU_BLOCKS, LRU_BLOCK)
    r = jax.nn.sigmoid(jnp.einsum('bsgi,gij->bsgj', xg, w_a.astype(jnp.float32)).reshape(b, s, LRU_WIDTH) + b_a.astype(jnp.float32))
    i = jax.nn.sigmoid(jnp.einsum('bsgi,gij->bsgj', xg, w_x.astype(jnp.float32)).reshape(b, s, LRU_WIDTH) + b_x.astype(jnp.float32))
    log_a = -LRU_C * r * jax.nn.softplus(-lam.astype(jnp.float32))
    a = jnp.exp(log_a)
    u = jnp.sqrt(-jnp.expm1(2.0 * log_a)) * (i * xc)
    h = linear_recurrence(a, u)
    y = h * jax.nn.gelu(gate_in.astype(jnp.float32))
    return rmsnorm(y, out_norm).astype(x_in.dtype)


def chunk_gated_delta_rule(q, k, v, g, beta):
    b, s, h, dk = q.shape
    dv = v.shape[-1]
    nc = s // CHUNK

    def to_chunks(t):
        return jnp.moveaxis(t.reshape((b, nc, CHUNK) + t.shape[2:]), 3, 1)

    q, k, v, g, beta = map(to_chunks, (q * dk ** -0.5, k, v, g, beta))
    g = jnp.cumsum(g, axis=-1)
    idx = jnp.arange(CHUNK)
    causal = idx[:, None] >= idx[None, :]
    strict = idx[:, None] > idx[None, :]
    gdiff = g[..., :, None] - g[..., None, :]
    decay = jnp.where(causal, jnp.exp(jnp.where(causal, gdiff, 0.0)), 0.0)
    k_beta = k * beta[..., None]
    a_low = jnp.where(strict, jnp.einsum('bhncd,bhnsd->bhncs', k_beta, k) * decay, 0.0)
    rhs = jnp.concatenate([v * beta[..., None], k_beta * jnp.exp(g)[..., None]], axis=-1)
    sol = lax.linalg.triangular_solve(a_low, rhs, left_side=True, lower=True, unit_diagonal=True)
    u, w = sol[..., :dv], sol[..., dv:]
    attn_intra = jnp.einsum('bhncd,bhnsd->bhncs', q, k) * decay
    q_decayed = q * jnp.exp(g)[..., None]
    k_to_end = k * jnp.exp(g[..., -1:] - g)[..., None]
    chunk_decay = jnp.exp(g[..., -1])

    def step(state, inp):
        u_c, w_c, qd_c, att_c, ke_c, dec_c = inp
        v_new = u_c - jnp.einsum('bhcd,bhde->bhce', w_c, state)
        o_c = jnp.einsum('bhcd,bhde->bhce', qd_c, state) + jnp.einsum('bhcs,bhse->bhce', att_c, v_new)
        state = state * dec_c[..., None, None] + jnp.einsum('bhcd,bhce->bhde', ke_c, v_new)
        return state, o_c

    xs = tuple(jnp.moveaxis(t, 2, 0) for t in (u, w, q_decayed, attn_intra, k_to_end, chunk_decay))
    _, o = lax.scan(step, jnp.zeros((b, h, dk, dv), jnp.float32), xs)
    return jnp.transpose(jnp.moveaxis(o, 0, 2), (0, 2, 3, 1, 4)).reshape(b, s, h, dv)


def gdn_mixer(q_in, k_in, v_in, z_in, a_in, b_in, conv_w, a_log, dt_bias, out_norm):
    bsz, s, _ = q_in.shape
    qkv = jax.nn.silu(causal_depthwise_conv(jnp.concatenate([q_in, k_in, v_in], axis=-1), conv_w)).astype(jnp.float32)
    qd = GDN_HEADS * GDN_DK
    q = l2norm(qkv[..., :qd].reshape(bsz, s, GDN_HEADS, GDN_DK))
    k = l2norm(qkv[..., qd:2 * qd].reshape(bsz, s, GDN_HEADS, GDN_DK))
    v = qkv[..., 2 * qd:].reshape(bsz, s, GDN_HEADS, GDN_DV)
    beta = jax.nn.sigmoid(b_in.astype(jnp.float32))
    g = -jnp.exp(a_log.astype(jnp.float32)) * jax.nn.softplus(a_in.astype(jnp.float32) + dt_bias.astype(jnp.float32))
    o = chunk_gated_delta_rule(q, k, v, g, beta)
    z = z_in.astype(jnp.float32).reshape(bsz, s, GDN_HEADS, GDN_DV)
    o = rmsnorm(o, out_norm) * jax.nn.silu(z)
    return o.reshape(bsz, s, GDN_WIDTH).astype(q_in.dtype)


def swiglu(h, w_gate, w_up, w_down):
    return (jax.nn.silu(h @ w_gate) * (h @ w_up)) @ w_down


def moe_swiglu(h, router_w, w_gate, w_up, w_down):
    b, s, d = h.shape
    t = h.reshape(b * s, d)
    logits = (t @ router_w).astype(jnp.float32)
    top_val, top_idx = lax.top_k(logits, TOP_K)
    gates = jax.nn.softmax(top_val, axis=-1)
    combine = jnp.sum(jax.nn.one_hot(top_idx, N_EXPERTS, dtype=jnp.float32) * gates[..., None], axis=1)
    out = jnp.zeros_like(t)
    for e in range(N_EXPERTS):
        out = out + combine[:, e:e + 1].astype(t.dtype) * swiglu(t, w_gate[e], w_up[e], w_down[e])
    return out.reshape(b, s, d)


def setup_inputs(seed: int = 0) -> dict:
    key = jax.random.key(seed)
    ks = list(jax.random.split(key, 48))

    def nk():
        return ks.pop()

    def nrm(shape, scale):
        return scale * jax.random.normal(nk(), shape, jnp.float32)

    def gain(shape):
        return 1.0 + 0.02 * jax.random.normal(nk(), shape, jnp.float32)

    L = DEPTH
    x = jax.random.normal(nk(), (BATCH, SEQ, D_MODEL), jnp.float32)
    steps = jax.random.randint(nk(), (BATCH, SEQ), 1, 3, dtype=jnp.int32)
    offset = jax.random.randint(nk(), (BATCH, 1), 0, 4096, dtype=jnp.int32)
    positions = offset + jnp.cumsum(steps, axis=1, dtype=jnp.int32) - steps[:, :1]

    u_lam = jax.random.uniform(nk(), (L, LRU_WIDTH), jnp.float32, 0.9, 0.999)
    a0 = u_lam ** (1.0 / LRU_C)
    lru_lambda = jnp.log(a0) - jnp.log1p(-a0)
    dt = jnp.exp(jax.random.uniform(nk(), (L, GDN_HEADS), jnp.float32, math.log(1e-3), math.log(1e-1)))
    gdn_dt_bias = dt + jnp.log(-jnp.expm1(-dt))
    gdn_a_log = jnp.log(jax.random.uniform(nk(), (L, GDN_HEADS), jnp.float32, 1.0, 16.0))

    return {
        'x': x,
        'positions': positions,
        'mix_norm': gain((L, D_MODEL)),
        'w_in': nrm((L, D_MODEL, D_IN), D_MODEL ** -0.5),
        'mla_q_norm': gain((L, MLA_Q_RANK)),
        'mla_w_uq': nrm((L, MLA_Q_RANK, MLA_HEADS * MLA_QK_DIM), MLA_Q_RANK ** -0.5),
        'mla_kv_norm': gain((L, MLA_KV_RANK)),
        'mla_w_ukv': nrm((L, MLA_KV_RANK, MLA_HEADS * (MLA_NOPE + MLA_V)), MLA_KV_RANK ** -0.5),
        'mla_q_head_norm': gain((L, MLA_QK_DIM)),
        'mla_k_head_norm': gain((L, MLA_QK_DIM)),
        'mla_out_norm': gain((L, MLA_WIDTH)),
        'lru_conv_w': nrm((L, CONV_WIDTH, LRU_WIDTH), CONV_WIDTH ** -0.5),
        'lru_conv_b': nrm((L, LRU_WIDTH), 0.02),
        'lru_w_a': nrm((L, LRU_BLOCKS, LRU_BLOCK, LRU_BLOCK), LRU_BLOCK ** -0.5),
        'lru_b_a': nrm((L, LRU_WIDTH), 0.02),
        'lru_w_x': nrm((L, LRU_BLOCKS, LRU_BLOCK, LRU_BLOCK), LRU_BLOCK ** -0.5),
        'lru_b_x': nrm((L, LRU_WIDTH), 0.02),
        'lru_lambda': lru_lambda,
        'lru_out_norm': gain((L, LRU_WIDTH)),
        'gdn_conv_w': nrm((L, CONV_WIDTH, 2 * GDN_HEADS * GDN_DK + GDN_WIDTH), CONV_WIDTH ** -0.5),
        'gdn_a_log': gdn_a_log,
        'gdn_dt_bias': gdn_dt_bias,
        'gdn_out_norm': gain((L, GDN_DV)),
        'w_out': nrm((L, D_MIX, D_MODEL), D_MIX ** -0.5),
        'ffn_norm': gain((L, D_MODEL)),
        'dense_w_gate': nrm((N_DENSE, D_MODEL, D_FF_DENSE), D_MODEL ** -0.5),
        'dense_w_up': nrm((N_DENSE, D_MODEL, D_FF_DENSE), D_MODEL ** -0.5),
        'dense_w_down': nrm((N_DENSE, D_FF_DENSE, D_MODEL), D_FF_DENSE ** -0.5),
        'router_w': nrm((N_MOE, D_MODEL, N_EXPERTS), D_MODEL ** -0.5),
        'moe_w_gate': nrm((N_MOE, N_EXPERTS, D_MODEL, D_FF_EXPERT), D_MODEL ** -0.5),
        'moe_w_up': nrm((N_MOE, N_EXPERTS, D_MODEL, D_FF_EXPERT), D_MODEL ** -0.5),
        'moe_w_down': nrm((N_MOE, N_EXPERTS, D_FF_EXPERT, D_MODEL), D_FF_EXPERT ** -0.5),
    }


def reference(x, positions, mix_norm, w_in, mla_q_norm, mla_w_uq, mla_kv_norm, mla_w_ukv, mla_q_head_norm, mla_k_head_norm, mla_out_norm, lru_conv_w, lru_conv_b, lru_w_a, lru_b_a, lru_w_x, lru_b_x, lru_lambda, lru_out_norm, gdn_conv_w, gdn_a_log, gdn_dt_bias, gdn_out_norm, w_out, ffn_norm, dense_w_gate, dense_w_up, dense_w_down, router_w, moe_w_gate, moe_w_up, moe_w_down):
    cos, sin = rope_tables(positions)
    split_at = tuple(int(v) for v in np.cumsum(IN_WIDTHS)[:-1])
    for layer in range(DEPTH):
        h = rmsnorm(x, mix_norm[layer])
        (c_q, c_kv, k_rope, lru_x, lru_gate, g_q, g_k, g_v, g_z, g_a, g_b) = jnp.split(h @ w_in[layer], split_at, axis=-1)
        y_mla = mla_mixer(c_q, c_kv, k_rope, cos, sin, mla_q_norm[layer], mla_w_uq[layer], mla_kv_norm[layer], mla_w_ukv[layer], mla_q_head_norm[layer], mla_k_head_norm[layer], mla_out_norm[layer])
        y_lru = rglru_mixer(lru_x, lru_gate, lru_conv_w[layer], lru_conv_b[layer], lru_w_a[layer], lru_b_a[layer], lru_w_x[layer], lru_b_x[layer], lru_lambda[layer], lru_out_norm[layer])
        y_gdn = gdn_mixer(g_q, g_k, g_v, g_z, g_a, g_b, gdn_conv_w[layer], gdn_a_log[layer], gdn_dt_bias[layer], gdn_out_norm[layer])
        x = x + jnp.concatenate([y_mla, y_lru, y_gdn], axis=-1) @ w_out[layer]
        h = rmsnorm(x, ffn_norm[layer])
        if layer % 2 == 0:
            x = x + swiglu(h, dense_w_gate[layer // 2], dense_w_up[layer // 2], dense_w_down[layer // 2])
        else:
            x = x + moe_swiglu(h, router_w[layer // 2], moe_w_gate[layer // 2], moe_w_up[layer // 2], moe_w_down[layer // 2])
    return x
```

```python
import contextlib, math
import ml_dtypes
import numpy as np
import concourse.bass as bass
import concourse.mybir as mybir
from concourse.bass_utils import run_bass_kernel_spmd

F32 = mybir.dt.float32
BF16 = mybir.dt.bfloat16
I32 = mybir.dt.int32
ALU = mybir.AluOpType
AF = mybir.ActivationFunctionType
AX = mybir.AxisListType


def I(name, *args, **kw):
    return (name, args, kw)


class Buf:
    __slots__ = ("name", "w", "r", "sem", "cnt", "t")

    def __init__(self, name, t=None):
        self.name = name
        self.w = {}
        self.r = {}
        self.sem = None
        self.cnt = 0
        self.t = t

    def __getitem__(self, idx):
        return self.t[idx]


def merge_streams(streams):
    out = []
    idx = [0] * len(streams)
    n = [len(x) for x in streams]
    while True:
        best, bi = None, -1
        for i in range(len(streams)):
            if idx[i] < n[i]:
                f = (idx[i] + 0.5) / n[i]
                if best is None or f < best:
                    best, bi = f, i
        if bi < 0:
            return out
        out.append(streams[bi][idx[bi]])
        idx[bi] += 1


class Prog:
    ENG = ("pe", "dve", "act", "pool", "sp")

    def __init__(self, nc):
        self.nc = nc
        self.q = {e: [] for e in self.ENG}
        self.cnt = {e: 0 for e in self.ENG}
        self.sem = {e: nc.alloc_semaphore("prog_" + e) for e in self.ENG}
        self.waited = {e: {} for e in self.ENG}
        self.nsem = 5
        self.free_sems = []
        self.sem_bufs = []
        self.extra_sems = []
        self.all_bufs = []
        self.ccsem = None
        self._cap = None
        self.cccnt = 0
        self.uid = 0

    def sb(self, name, shape, dt):
        t = self.nc.alloc_sbuf_tensor(name, list(shape), dt)
        return self.reg(Buf(name, t))

    def ps(self, name, shape, dt=F32):
        t = self.nc.alloc_psum_tensor(name, list(shape), dt)
        return self.reg(Buf(name, t))

    def dram(self, name, shape, dt, kind="Internal"):
        t = self.nc.dram_tensor(name, list(shape), dt, kind=kind)
        return self.reg(Buf(name, t))

    def reg(self, b):
        self.all_bufs.append(b)
        return b

    def _bufsem(self, b):
        if b.sem is None:
            if self.free_sems:
                b.sem, b.cnt = self.free_sems.pop()
            else:
                b.sem = self.nc.alloc_semaphore("bs_%d" % self.nsem)
                self.nsem += 1
                b.cnt = 0
            self.sem_bufs.append(b)
        return b.sem

    def barrier(self):
        allw = [(self.sem[e], self.cnt[e]) for e in self.ENG if self.cnt[e] > 0]
        allw += [(b.sem, b.cnt) for b in self.sem_bufs if b.cnt > 0]
        allw += [(s, c) for s, c in self.free_sems if c > 0]
        allw += [(s, c) for s, c in self.extra_sems if c > 0]
        if self.ccsem is not None and self.cccnt > 0:
            allw.append((self.ccsem, self.cccnt))
        for e in self.ENG:
            wd = self.waited[e]
            wl = []
            for s, v in allw:
                if wd.get(s, 0) < v:
                    wd[s] = v
                    wl.append((s, v))
            self.q[e].append((wl, None, None, 0))
        for b in self.sem_bufs:
            self.free_sems.append((b.sem, b.cnt))
            b.sem = None
            b.w = {}
            b.r = {}
        self.sem_bufs = []
        for b in self.all_bufs:
            b.w = {}
            b.r = {}

    def _deps(self, eng, reads, writes):
        deps = {}

        def add(s, v):
            if deps.get(s, 0) < v:
                deps[s] = v
        for b in reads:
            for s, v in b.w.items():
                add(s, v)
        for b in writes:
            for s, v in b.w.items():
                add(s, v)
            for s, v in b.r.items():
                add(s, v)
        wl = []
        wd = self.waited[eng]
        for s, v in deps.items():
            if wd.get(s, 0) < v:
                wd[s] = v
                wl.append((s, v))
        return wl

    def capture(self):
        self._cap = []

    def end_capture(self):
        c, self._cap = self._cap, None
        return c

    def replay(self, items):
        for kind, a, kw in items:
            getattr(self, kind)(*a, **kw)

    def op(self, eng, ins, reads=(), writes=()):
        if self._cap is not None:
            self._cap.append(("op", (eng, ins, reads, writes), {}))
            return None
        if isinstance(ins, tuple):
            ins = [ins]
        e = self._eng(eng)

        def fn(ins=ins, e=e):
            r = None
            for name, args, kw in ins:
                r = getattr(e, name)(*args, **kw)
            return r
        waits = self._deps(eng, reads, writes)
        self.cnt[eng] += 1
        s, v = self.sem[eng], self.cnt[eng]
        self.q[eng].append((waits, fn, s, 1))
        for b in reads:
            if b.r.get(s, 0) < v:
                b.r[s] = v
        for b in writes:
            b.w = {s: v}
            b.r = {}
        return (s, v)

    def dma(self, eng, out, in_, owner, reads=(), writes=(), merge=False, **kw):
        if self._cap is not None:
            self._cap.append(("dma", (eng, out, in_, owner, reads, writes, merge), kw))
            return None
        waits = self._deps(eng, reads, writes if not merge else ())
        s = self._bufsem(owner)
        owner.cnt += 16
        v = owner.cnt
        e = self._eng(eng)

        def fn():
            return e.dma_start(out=out, in_=in_, **kw)
        self.q[eng].append((waits, fn, s, 16))
        for b in reads:
            if b.r.get(s, 0) < v:
                b.r[s] = v
        for b in writes:
            if merge:
                b.w[s] = v
            else:
                b.w = {s: v}
                b.r = {}
        return (s, v)

    def dma_fn(self, eng, fn, owner, reads=(), writes=(), merge=False):
        waits = self._deps(eng, reads, writes if not merge else ())
        s = self._bufsem(owner)
        owner.cnt += 16
        v = owner.cnt
        self.q[eng].append((waits, fn, s, 16))
        for b in reads:
            if b.r.get(s, 0) < v:
                b.r[s] = v
        for b in writes:
            if merge:
                b.w[s] = v
            else:
                b.w = {s: v}
                b.r = {}
        return (s, v)

    def allgather(self, in_ap, out_ap, groups, waits=()):
        nc = self.nc
        if self.ccsem is None:
            self.ccsem = nc.alloc_semaphore("prog_cc")
        self.cccnt += 1

        def fn():
            return nc.gpsimd.collective_compute("AllGather", ALU.bypass, replica_groups=groups, ins=[in_ap.opt()], outs=[out_ap.opt()])
        self.q["pool"].append((list(waits), fn, self.ccsem, 1))

    def wait_all(self, eng, bufs):
        waits = self._deps(eng, bufs, ())
        self.q[eng].append((waits, None, None, 0))

    def _eng(self, e):
        nc = self.nc
        return {"pe": nc.tensor, "dve": nc.vector, "act": nc.scalar, "pool": nc.gpsimd, "sp": nc.sync}[e]

    def emit(self):
        nc = self.nc
        with nc.Block() as block:
            def mk(ename):
                def body(engine):
                    for waits, fn, s, inc in self.q[ename]:
                        for ws, wv in waits:
                            engine.wait_ge(ws, wv)
                        if fn is not None:
                            ins = fn()
                            if s is not None:
                                ins.then_inc(s, inc)
                return body
            block.tensor(mk("pe"))
            block.vector(mk("dve"))
            block.scalar(mk("act"))
            block.gpsimd(mk("pool"))
            block.sync(mk("sp"))


D = 1024
NT = 16
TOK = 2048
EPS = 1e-6


def consts(P, cdram):
    nc = P.nc
    c = {}
    c["ident_f"] = P.sb("ident_f", [128, 128], F32)
    c["ident_b"] = P.sb("ident_b", [128, 128], BF16)
    c["ones_b"] = P.sb("ones_b", [128, 128], BF16)
    c["ones_f"] = P.sb("ones_f", [128, 128], F32)
    idf, idb, ob, of = c["ident_f"], c["ident_b"], c["ones_b"], c["ones_f"]
    P.dma("sp", idf[:, :], cdram[:, 0:128], idf, writes=[idf])
    P.dma("sp", of[:, :], cdram[:, 128:256], of, writes=[of])
    P.op("pool", I("tensor_copy", idb[:, :], idf[:, :]), reads=[idf], writes=[idb])
    P.op("pool", I("tensor_copy", ob[:, :], of[:, :]), reads=[of], writes=[ob])
    return c


def scope(P, es):
    nc = P.nc
    P.uid += 1
    sfx = "_u%d" % P.uid
    def sb(name, shape, dt):
        t = es.enter_context(nc.sbuf_tensor(name + sfx, list(shape), dt))
        return Buf(name, t)
    def ps(name):
        t = es.enter_context(nc.psum_tensor(name + sfx, [128, 512], F32))
        return Buf(name, t)
    return sb, ps


def emit_norm_T(P, C, xt, gain_bc, hT, t, ps_bank, tmpb, junk, ssb, also_f32=None):
    P.op("act", I("activation", junk[:, :], xt[:, :], AF.Square, accum_out=ssb[:, 0:1]), reads=[xt], writes=[junk, ssb])
    P.op("act", I("activation", ssb[:, 1:2], ssb[:, 0:1], AF.Ln, bias=EPS, scale=1.0 / D), reads=[ssb], writes=[ssb])
    P.op("act", I("activation", ssb[:, 2:3], ssb[:, 1:2], AF.Exp, scale=-0.5), reads=[ssb], writes=[ssb])
    if also_f32 is not None:
        hf, hTf, psf = also_f32
        P.op("dve", I("scalar_tensor_tensor", hf[:, :], xt[:, :], ssb[:, 2:3], gain_bc[:, :], ALU.mult, ALU.mult),
             reads=[xt, ssb, gain_bc], writes=[hf])
        P.op("pool", I("tensor_copy", tmpb[:, :], hf[:, :]), reads=[hf], writes=[tmpb])
        for half in range(2):
            P.op("pe", [I("transpose", psf[:, k * 128:(k + 1) * 128], hf[:, (half * 4 + k) * 128:(half * 4 + k + 1) * 128], C["ident_f"][:, :]) for k in range(4)],
                 reads=[hf, C["ident_f"]], writes=[psf])
            P.op("act", I("copy", hTf[:, half * 4:(half + 1) * 4, :], psf[:, :].rearrange("p (k n) -> p k n", k=4)), reads=[psf], writes=[hTf])
    else:
        P.op("dve", I("scalar_tensor_tensor", tmpb[:, :], xt[:, :], ssb[:, 2:3], gain_bc[:, :], ALU.mult, ALU.mult),
             reads=[xt, ssb, gain_bc], writes=[tmpb])
    pb = ps_bank[:, :].bitcast(BF16)
    P.op("pe", [I("transpose", pb[:, kc * 128:(kc + 1) * 128], tmpb[:, kc * 128:(kc + 1) * 128], C["ident_b"][:, :]) for kc in range(8)],
         reads=[tmpb, C["ident_b"]], writes=[ps_bank])
    P.op("act", I("copy", hT[:, :, t * 128:(t + 1) * 128], pb.rearrange("p (k n) -> p k n", k=8)), reads=[ps_bank], writes=[hT])


def store_hT(P, hT, io):
    if "hx" in io:
        hv = io["hx"].rearrange("b (k p) n -> p k b n", p=128)
        for k in range(8):
            P.dma("sp" if k % 2 == 0 else "pool", hv[:, k, :, :], hT[:, k, :].rearrange("p (b n) -> p b n", b=4), hT, reads=[hT], writes=[io["hT_buf"]], merge=True)
        return
    hv = io["hT_out"].rearrange("(k p) n -> p k n", p=128)
    for k in range(8):
        P.dma("sp" if k % 2 == 0 else "pool", hv[:, k, :], hT[:, k, :], hT, reads=[hT], writes=[io["hT_buf"]], merge=True)


def phase_N0(P, C, io):
    with contextlib.ExitStack() as es:
        sb, ps = scope(P, es)
        gain = sb("n0_gain", [128, D], F32)
        hT = sb("n0_hT", [128, 8, TOK], BF16)
        xts = [sb(f"n0_x{i}", [128, D], F32) for i in range(4)]
        tmpb = [sb(f"n0_tb{i}", [128, D], BF16) for i in range(2)]
        junk = sb("n0_junk", [128, D], F32)
        ssb = [sb(f"n0_ss{i}", [128, 4], F32) for i in range(2)]
        pbk = [ps(f"n0_ps{i}") for i in range(2)]
        P.dma("sp", gain[:, :], io["mix_gain_bc"], gain, writes=[gain])
        xv = io["xloc"].rearrange("(t p) d -> t p d", p=128)
        for t in range(NT):
            xt = xts[t % 4]
            P.dma("sp" if t % 2 == 0 else "pool", xt[:, :], xv[t], xt, reads=[io["xloc_buf"]], writes=[xt])
            emit_norm_T(P, C, xt, gain, hT, t, pbk[t % 2], tmpb[t % 2], junk, ssb[t % 2])
        store_hT(P, hT, io)
        P.barrier()


def phase_F(P, C, io, cfg):
    nc = P.nc
    moe = cfg["moe"]
    E = cfg["n_exp"]
    FF = cfg["d_ff"]
    NFC = FF // 128
    with contextlib.ExitStack() as es:
        sb, ps = scope(P, es)
        xl = [sb(f"f_xl{t}", [128, D], F32) for t in range(NT)]
        pbk = [ps(f"f_ps{i}") for i in range(8)]
        xv = io["xloc"].rearrange("(t p) d -> t p d", p=128)
        for t in range(NT):
            P.dma("sp" if t % 2 == 0 else "pool", xl[t][:, :], xv[t], xl[t], reads=[io["xloc_buf"]], writes=[xl[t]])
        with contextlib.ExitStack() as es2:
            sb2, _ = scope(P, es2)
            yT = [sb2(f"f_yT{s}", [128, 3, TOK], BF16) for s in range(4)]
            wo = [sb2(f"f_wo{j}", [128, D], BF16) for j in range(12)]
            wst = [sb2(f"f_wst{i}", [128, D], F32) for i in range(2)]
            gm = sb2("f_gm", [128, 8], F32)
            sq = [sb2(f"f_sq{i}", [128, 4, 2, 128], BF16) for i in range(2)]
            rs = [sb2(f"f_rs{i}", [128, 8], F32) for i in range(2)]
            P.dma("sp", gm[:, 0:4], io["g_mla"], gm, writes=[gm])
            P.dma("sp", gm[:, 4:8], io["g_lru"], gm, writes=[gm], merge=True)
            if "yall" in io:
                yidx = sb2("f_yidx", [128, 12], I32)
                P.dma("sp", yidx[:, :], io["yidx"], yidx, writes=[yidx])
                for s in range(4):
                    for br in range(3):
                        col = br * 4 + s

                        def gfn(s=s, br=br, col=col):
                            return nc.gpsimd.indirect_dma_start(out=yT[s][:, br, :], out_offset=None, in_=io["yall"],
                                                                in_offset=bass.IndirectOffsetOnAxis(ap=yidx[:, col:col + 1], axis=0))
                        P.dma_fn("pool", gfn, yT[s], reads=[yidx, io["yrecv_buf"]], writes=[yT[s]], merge=(br > 0))
            else:
                for s in range(4):
                    P.dma("sp" if s % 2 == 0 else "pool", yT[s][:, :, :], io["yrecv"][s].rearrange("b f n -> f b n"), yT[s],
                          reads=[io["yrecv_buf"]], writes=[yT[s]])
            for j in range(12):
                br, s = j // 4, j % 4
                st = wst[j % 2]
                P.dma("act", st[:, :], io["w_out_r"][br, s], st, writes=[st])
                if br < 2:
                    P.op("pool", I("tensor_scalar", wo[j][:, :], st[:, :], gm[:, br * 4 + s:br * 4 + s + 1], None, ALU.mult),
                         reads=[st, gm], writes=[wo[j]])
                else:
                    P.op("pool", I("tensor_copy", wo[j][:, :], st[:, :]), reads=[st], writes=[wo[j]])
            for t in range(NT):
                tsl = slice(t * 128, (t + 1) * 128)
                sqb = sq[t % 2]
                rsb = rs[t % 2]
                pss = pbk[6 + (t % 2)]
                for s in range(4):
                    P.op("act", I("activation", sqb[:, s, :, :], yT[s][:, 0:2, tsl], AF.Square), reads=[yT[s]], writes=[sqb])
                P.op("pe", [I("matmul", pss[:, 0:1], sqb[:, s, 0, :], C["ones_b"][:, 0:1], start=(s == 0), stop=(s == 3)) for s in range(4)]
                     + [I("matmul", pss[:, 1:2], sqb[:, s, 1, :], C["ones_b"][:, 0:1], start=(s == 0), stop=(s == 3)) for s in range(4)],
                     reads=[sqb, C["ones_b"]], writes=[pss])
                P.op("act", I("activation", rsb[:, 0:2], pss[:, 0:2], AF.Ln, bias=EPS, scale=1.0 / 512), reads=[pss], writes=[rsb])
                P.op("act", I("activation", rsb[:, 2:4], rsb[:, 0:2], AF.Exp, scale=-0.5), reads=[rsb], writes=[rsb])
                xs = xl[t]
                for br in range(3):
                    for dh in range(2):
                        pb_ = pbk[(br * 2 + dh) % 6]
                        dsl = slice(dh * 512, (dh + 1) * 512)
                        P.op("pe", [I("matmul", pb_[:, :], yT[s][:, br, tsl], wo[br * 4 + s][:, dsl], start=(s == 0), stop=(s == 3)) for s in range(4)],
                             reads=yT + wo[br * 4:br * 4 + 4], writes=[pb_])
                        if br < 2:
                            P.op("dve", I("scalar_tensor_tensor", xs[:, dsl], pb_[:, :], rsb[:, 2 + br:3 + br], xs[:, dsl], ALU.mult, ALU.add),
                                 reads=[pb_, rsb, xs], writes=[xs])
                        else:
                            P.op("dve", I("tensor_tensor", xs[:, dsl], pb_[:, :], xs[:, dsl], ALU.add), reads=[pb_, xs], writes=[xs])
            P.barrier()
        h2T = sb("f_h2T", [128, 8, TOK], BF16)
        comb = sb("f_comb", [128, NT, 8], F32) if moe else None
        with contextlib.ExitStack() as es3:
            sb3, _ = scope(P, es3)
            gain = sb3("f_gain", [128, D], F32)
            tmpb = [sb3(f"f_tb{i}", [128, D], BF16) for i in range(2)]
            junk = sb3("f_junk", [128, D], F32)
            ssb = [sb3(f"f_ss{i}", [128, 4], F32) for i in range(2)]
            P.dma("sp", gain[:, :], io["ffn_gain_bc"], gain, writes=[gain])
            if moe:
                hf = [sb3(f"f_hf{i}", [128, D], F32) for i in range(2)]
                hTf = [sb3(f"f_hTf{i}", [128, 8, 128], F32) for i in range(2)]
                rw = sb3("f_rw", [128, 8, 8], F32)
                lg = [sb3(f"f_lg{i}", [128, 40], F32) for i in range(2)]
                P.dma("sp", rw[:, :, :], io["router"].rearrange("(k p) e -> p k e", p=128), rw, writes=[rw])
            for t in range(NT):
                af = (hf[t % 2], hTf[t % 2], pbk[2 + (t % 2)]) if moe else None
                emit_norm_T(P, C, xl[t], gain, h2T, t, pbk[t % 2], tmpb[t % 2], junk, ssb[t % 2], also_f32=af)
                if moe:
                    hTf_, g_, pl = hTf[t % 2], lg[t % 2], pbk[4 + (t % 2)]
                    P.op("pe", [I("matmul", pl[:, 0:8], hTf_[:, kc, :], rw[:, kc, :], start=(kc == 0), stop=(kc == 7)) for kc in range(8)],
                         reads=[hTf_, rw], writes=[pl])
                    P.op("dve", I("tensor_copy", g_[:, 0:8], pl[:, 0:8]), reads=[pl], writes=[g_])
                    P.op("dve", I("tensor_reduce", g_[:, 8:9], g_[:, 0:8], AX.X, ALU.max), reads=[g_], writes=[g_])
                    P.op("dve", I("tensor_scalar", g_[:, 9:17], g_[:, 0:8], g_[:, 8:9], None, ALU.is_equal), reads=[g_], writes=[g_])
                    P.op("dve", I("scalar_tensor_tensor", g_[:, 17:25], g_[:, 9:17], -1e30, g_[:, 0:8], ALU.mult, ALU.add), reads=[g_], writes=[g_])
                    P.op("dve", I("tensor_reduce", g_[:, 25:26], g_[:, 17:25], AX.X, ALU.max), reads=[g_], writes=[g_])
                    P.op("dve", I("tensor_scalar", g_[:, 26:34], g_[:, 17:25], g_[:, 25:26], None, ALU.is_equal), reads=[g_], writes=[g_])
                    P.op("dve", I("tensor_tensor", g_[:, 34:35], g_[:, 25:26], g_[:, 8:9], ALU.subtract), reads=[g_], writes=[g_])
                    P.op("act", I("activation", g_[:, 34:35], g_[:, 34:35], AF.Sigmoid), reads=[g_], writes=[g_])
                    P.op("dve", I("tensor_scalar", g_[:, 35:36], g_[:, 34:35], -1.0, 1.0, ALU.mult, ALU.add), reads=[g_], writes=[g_])
                    P.op("dve", I("tensor_scalar", g_[:, 9:17], g_[:, 9:17], g_[:, 35:36], None, ALU.mult), reads=[g_], writes=[g_])
                    P.op("dve", I("scalar_tensor_tensor", comb[:, t, :], g_[:, 26:34], g_[:, 34:35], g_[:, 9:17], ALU.mult, ALU.add),
                         reads=[g_], writes=[comb])
            P.barrier()
        with contextlib.ExitStack() as es4:
            sb4, _ = scope(P, es4)
            groups = []
            GMAX = 8 if NFC % 7 else 7
            f0 = 0
            while f0 < NFC:
                n = min(GMAX, NFC - f0)
                if NFC == 22:
                    n = 8 if f0 == 0 else 7
                groups.append((f0, n))
                f0 += n
            act = sb4("f_act", [128, GMAX, TOK], BF16)
            wgs = [sb4(f"f_wgs{i}", [128, 8, 128], F32) for i in range(2)]
            wus = [sb4(f"f_wus{i}", [128, 8, 128], F32) for i in range(2)]
            wgb = [sb4(f"f_wgb{i}", [128, 8, 128], BF16) for i in range(2)]
            wub = [sb4(f"f_wub{i}", [128, 8, 128], BF16) for i in range(2)]
            wds = [sb4(f"f_wds{i}", [128, D], F32) for i in range(2)]
            wdb = [sb4(f"f_wdb{i}", [128, D], BF16) for i in range(GMAX)]
            sil = [sb4(f"f_sil{i}", [128, 512], F32) for i in range(2)]
            ci = 0
            for e in range(E):
                wgv = io["wg"][e].rearrange("(k p) f -> p k f", p=128)
                wuv = io["wu"][e].rearrange("(k p) f -> p k f", p=128)
                for (g0, gn) in groups:
                    for j in range(gn):
                        fc = g0 + j
                        b = ci % 2
                        ci += 1
                        fsl = slice(fc * 128, (fc + 1) * 128)
                        P.dma("sp", wgs[b][:, :, :], wgv[:, :, fsl], wgs[b], writes=[wgs[b]])
                        P.dma("pool", wus[b][:, :, :], wuv[:, :, fsl], wus[b], writes=[wus[b]])
                        P.dma("sp", wds[b][:, :], io["wd"][e, fsl, :], wds[b], writes=[wds[b]])
                        P.op("pool", I("tensor_copy", wgb[b][:, :, :], wgs[b][:, :, :]), reads=[wgs[b]], writes=[wgb[b]])
                        P.op("pool", I("tensor_copy", wub[b][:, :, :], wus[b][:, :, :]), reads=[wus[b]], writes=[wub[b]])
                        P.op("pool", I("tensor_copy", wdb[j][:, :], wds[b][:, :]), reads=[wds[b]], writes=[wdb[j]])
                        for nt in range(4):
                            pg, pu = pbk[(nt % 2) * 2], pbk[(nt % 2) * 2 + 1]
                            nsl = slice(nt * 512, (nt + 1) * 512)
                            P.op("pe", [I("matmul", pg[:, :], wgb[b][:, kc, :], h2T[:, kc, nsl], start=(kc == 0), stop=(kc == 7)) for kc in range(8)],
                                 reads=[wgb[b], h2T], writes=[pg])
                            P.op("pe", [I("matmul", pu[:, :], wub[b][:, kc, :], h2T[:, kc, nsl], start=(kc == 0), stop=(kc == 7)) for kc in range(8)],
                                 reads=[wub[b], h2T], writes=[pu])
                            sl_ = sil[nt % 2]
                            P.op("act", I("activation", sl_[:, :], pg[:, :], AF.Silu), reads=[pg], writes=[sl_])
                            P.op("dve", I("tensor_tensor", act[:, j, nsl], pu[:, :], sl_[:, :], ALU.mult), reads=[pu, sl_], writes=[act])
                    for t in range(NT):
                        tsl = slice(t * 128, (t + 1) * 128)
                        xs = xl[t]
                        for dh in range(2):
                            pd = pbk[4 + ((t * 2 + dh) % 4)]
                            dsl = slice(dh * 512, (dh + 1) * 512)
                            P.op("pe", [I("matmul", pd[:, :], act[:, j, tsl], wdb[j][:, dsl], start=(j == 0), stop=(j == gn - 1)) for j in range(gn)],
                                 reads=[act] + wdb[:gn], writes=[pd])
                            if moe:
                                P.op("dve", I("scalar_tensor_tensor", xs[:, dsl], pd[:, :], comb[:, t, e:e + 1], xs[:, dsl], ALU.mult, ALU.add),
                                     reads=[pd, comb, xs], writes=[xs])
                            else:
                                P.op("dve", I("tensor_tensor", xs[:, dsl], pd[:, :], xs[:, dsl], ALU.add), reads=[pd, xs], writes=[xs])
            P.barrier()
        ov = io["xout"].rearrange("(t p) d -> t p d", p=128)
        for t in range(NT):
            P.dma("sp" if t % 2 == 0 else "pool", ov[t], xl[t][:, :], xl[t], reads=[xl[t]], writes=[io["xout_buf"]], merge=True)
        if not cfg["last"]:
            with contextlib.ExitStack() as es5:
                sb5, _ = scope(P, es5)
                gain = sb5("f_ngain", [128, D], F32)
                tmpb = [sb5(f"f_ntb{i}", [128, D], BF16) for i in range(2)]
                junk = sb5("f_njunk", [128, D], F32)
                ssb = [sb5(f"f_nss{i}", [128, 4], F32) for i in range(2)]
                P.dma("sp", gain[:, :], io["mix_gain_bc"], gain, writes=[gain])
                for t in range(NT):
                    emit_norm_T(P, C, xl[t], gain, h2T, t, pbk[t % 2], tmpb[t % 2], junk, ssb[t % 2])
                store_hT(P, h2T, io)
                P.barrier()
        P.barrier()


S_LEN = 8192
NG = 16
EPS = 1e-6
NFM = 1664
NTM = 130
NV = 32
SCALE = 192 ** -0.5
O_CQ, O_CKV, O_KR, O_LX, O_LG, O_GQ, O_GK, O_GV, O_BR = 0, 512, 768, 896, 1024, 1152, 1280, 1408, 1536


def phase_M(P, C, io, ngroups=NG, stages=('att', 'lru', 'gdn'), gstop=99):
    nc = P.nc
    ident_f, ones_f, ones_b = C["ident_f"], C["ones_f"], C["ones_b"]
    with contextlib.ExitStack() as es:
        sb, ps = scope(P, es)
        bank = [ps(f"m_ps{i}") for i in range(8)]
        rot = [0]

        def nb():
            b = bank[2 + rot[0] % 2]
            rot[0] += 1
            return b
        OPS, OB, LB = bank[5], bank[6], bank[7]
        Wfm = sb("m_Wfm", [128, 8, NFM], BF16)
        Wtm = sb("m_Wtm", [128, 8, NTM], BF16)
        Wuq = sb("m_Wuq", [128, 4, 256], BF16)
        Wukv = sb("m_Wukv", [128, 2, 256], BF16)
        wst = sb("m_wst", [128, 4, 256], F32)
        wst2 = sb("m_wst2", [128, 2, 256], F32)
        Wa = sb("m_Wa", [128, 128], BF16)
        Wx = sb("m_Wx", [128, 128], BF16)
        vec = sb("m_vec", [128, NV], F32)
        dv = sb("m_dv", [128, 16], F32)
        gq = sb("m_gq", [128, 8], F32)
        mc = sb("m_mc", [128, 7, 128], F32)
        ggbc = sb("m_ggbc", [128, 128], F32)
        KnT = sb("m_KnT", [128, S_LEN], BF16)
        KrT = sb("m_KrT", [128, S_LEN // 2], BF16)
        Vr = sb("m_V", [128, S_LEN // 128, 128], BF16)
        for kc in range(8):
            P.dma("pool", Wfm[:, kc, :], io["win_fm"][kc * 128:(kc + 1) * 128, :], Wfm, writes=[Wfm], merge=True)
            P.dma("pool", Wtm[:, kc, :], io["win_tm"][kc * 128:(kc + 1) * 128, :], Wtm, writes=[Wtm], merge=True)
        P.dma("pool", Wa[:, :], io["wa_bd"], Wa, writes=[Wa])
        P.dma("pool", Wx[:, :], io["wx_bd"], Wx, writes=[Wx])
        P.dma("sp", vec[:, :], io["vecs"], vec, writes=[vec])
        P.dma("sp", gq[:, 0:4], io["gq"], gq, writes=[gq])
        P.dma("sp", gq[:, 4:6], io["gkv"], gq, writes=[gq], merge=True)
        P.dma("sp", mc[:, :, :], io["mconst"].rearrange("p (a b) -> p a b", a=7), mc, writes=[mc])
        P.dma("sp", ggbc[:, :], io["gdn_gain_bc"], ggbc, writes=[ggbc])
        P.dma("sp", wst[:, :, :], io["wuq"].rearrange("(k p) n -> p k n", p=128), wst, writes=[wst])
        P.dma("sp", wst2[:, :, :], io["wukv"].rearrange("(k p) n -> p k n", p=128), wst2, writes=[wst2])
        for kc in range(4):
            P.op("pool", I("tensor_scalar", Wuq[:, kc, :], wst[:, kc, :], gq[:, kc:kc + 1], None, ALU.mult), reads=[wst, gq], writes=[Wuq])
        for kc in range(2):
            P.op("pool", I("tensor_scalar", Wukv[:, kc, :], wst2[:, kc, :], gq[:, 4 + kc:5 + kc], None, ALU.mult), reads=[wst2, gq], writes=[Wukv])
        U2, B2, MLs, MU, SU01, SI, CI = [mc[:, i, :] for i in range(7)]
        P.op("dve", I("tensor_scalar", dv[:, 0:1], vec[:, 0:1], SCALE, None, ALU.mult), reads=[vec], writes=[dv])
        P.op("dve", I("tensor_scalar", dv[:, 1:2], vec[:, 1:2], vec[:, 4:5], SCALE, ALU.mult, ALU.mult), reads=[vec, dv], writes=[dv])
        P.op("dve", I("tensor_scalar", dv[:, 2:3], vec[:, 3:4], vec[:, 4:5], None, ALU.mult), reads=[vec, dv], writes=[dv])
        P.op("act", I("activation", dv[:, 6:7], vec[:, 12:13], AF.Exp, scale=-1.0), reads=[vec, dv], writes=[dv])
        P.op("act", I("activation", dv[:, 6:7], dv[:, 6:7], AF.Ln, bias=1.0), reads=[dv], writes=[dv])
        P.op("dve", I("tensor_scalar", dv[:, 3:4], dv[:, 6:7], -8.0, None, ALU.mult), reads=[dv], writes=[dv])
        P.op("dve", I("tensor_scalar", dv[:, 4:5], dv[:, 6:7], -16.0, None, ALU.mult), reads=[dv], writes=[dv])
        P.op("act", I("activation", dv[:, 5:6], vec[:, 25:26], AF.Exp), reads=[vec, dv], writes=[dv])
        P.op("dve", I("tensor_scalar", dv[:, 5:6], dv[:, 5:6], -1.0, None, ALU.mult), reads=[dv], writes=[dv])
        hTg = [sb(f"m_hTg{i}", [128, 8, 512], BF16) for i in range(2)]
        posg = sb("m_posg", [128, 512], I32)
        ti = sb("m_ti", [128, 512], I32)
        f1 = sb("m_f1", [128, 512], F32)
        f2 = sb("m_f2", [128, 512], F32)
        tab = sb("m_tab", [128, 512], F32)
        cq_f = sb("m_cqf", [128, 4, 512], F32)
        ckv_f = sb("m_ckvf", [128, 2, 512], F32)
        cqn = sb("m_cqn", [128, 4, 512], BF16)
        ckvn = sb("m_ckvn", [128, 2, 512], BF16)
        sqb = [sb(f"m_sqb{i}", [128, 512], BF16) for i in range(2)]
        rstd = [sb(f"m_rstd{i}", [128, 512], F32) for i in range(2)]
        krf = sb("m_krf", [128, 512], F32)
        QnT = sb("m_QnT", [128, 512], BF16)
        QrT = sb("m_QrT", [128, 512], BF16)
        PT = [sb(f"m_PT{i}", [128, 512], BF16) for i in range(3)]
        yst = [sb(f"m_yst{i}", [128, 3, 512], BF16) for i in range(2)]
        ysv = [[Buf("m_ys%d%s" % (i, c), yst[i].t) for c in "ALG"] for i in range(2)]
        xpl = sb("m_xpl", [128, 515], F32)
        lgf = sb("m_lgf", [128, 512], F32)
        xc = sb("m_xc", [128, 512], F32)
        xcb = sb("m_xcb", [128, 512], BF16)
        lr = sb("m_lr", [128, 512], F32)
        li = sb("m_li", [128, 512], F32)
        la = sb("m_la", [128, 512], F32)
        lu = sb("m_lu", [128, 512], F32)
        lh = sb("m_lh", [128, 512], F32)
        hlast = sb("m_hlast", [128, 1], F32)
        xpq = sb("m_xpq", [128, 515], F32)
        xpk = sb("m_xpk", [128, 515], F32)
        xpv = sb("m_xpv", [128, 515], F32)
        gt1 = sb("m_gt1", [128, 512], F32)
        QT = sb("m_QT", [128, 512], F32)
        KT = sb("m_KT", [128, 512], F32)
        VT = sb("m_VT", [128, 512], F32)
        bbc = sb("m_bbc", [128, 512], F32)
        Sst = sb("m_S", [128, 128], F32)
        tl_names = ["sz", "gU", "gneg", "DmL", "DmU", "decLs", "decU", "decUs", "egbc", "KTb", "QDT", "X0", "Y0", "attnT",
                    "TtA", "TtB", "XA", "XB", "YA", "YB", "ke", "vb", "kbg", "u", "wT", "vnew", "t1", "t2", "junk"]
        T = {n: sb("m_t_" + n, [128, 128], F32) for n in tl_names}
        T2 = dict(T)
        for n_ in ("wT", "u", "QDT", "attnT", "ke", "sz"):
            T2[n_] = sb("m_t2_" + n_, [128, 128], F32)
        TT = [T, T2]
        gbs = [sb("m_gb%d" % i, [128, 16], F32) for i in range(2)]
        decbs = [sb("m_decb%d" % i, [128, 2], F32) for i in range(2)]
        for xp in (xpl, xpq, xpk, xpv):
            P.op("pool", I("memset", xp[:, 0:3], 0.0), writes=[xp])
        P.op("pool", I("memset", hlast[:, :], 0.0), writes=[hlast])
        P.op("pool", I("memset", Sst[:, :], 0.0), writes=[Sst])

        def rsq(dst, src_ps, n, reads):
            P.op("act", I("activation", dst, src_ps, AF.Ln, bias=EPS, scale=1.0 / n), reads=reads[0], writes=reads[1])
            P.op("act", I("activation", dst, dst, AF.Exp, scale=-0.5), reads=reads[1], writes=reads[1])

        for g in range(ngroups):
            h = hTg[g % 2]
            gsl = slice(g * 512, (g + 1) * 512)
            if "hall" in io:
                P.dma("sp", h[:, :, :], io["hall"][g % 4][(g // 4) * 1024:(g // 4 + 1) * 1024, :].rearrange("(k p) n -> p k n", p=128), h,
                      reads=[io["hT_buf"]], writes=[h])
            else:
                P.dma("sp", h[:, :, :], io["hT_all"][g // 4].rearrange("(k p) n -> p k n", p=128)[:, :, (g % 4) * 512:(g % 4 + 1) * 512], h,
                      reads=[io["hT_buf"]], writes=[h])
            P.dma("act", posg[:, :], io["posb"][:, gsl], posg, writes=[posg])
            ys = yst[g % 2]
            ysA, ysL, ysG = ysv[g % 2]
            P.op("dve", I("tensor_copy", f1[:, :], posg[:, :]), reads=[posg], writes=[f1])
            P.op("dve", I("tensor_scalar", f1[:, :], f1[:, :], vec[:, 27:28], None, ALU.mult), reads=[f1, vec], writes=[f1])
            P.op("dve", I("tensor_scalar", f1[:, :], f1[:, :], 1.0 / (2 * math.pi), vec[:, 28:29], ALU.mult, ALU.add), reads=[f1, vec], writes=[f1])
            P.op("dve", I("tensor_copy", ti[:, :], f1[:, :]), reads=[f1], writes=[ti])
            P.op("dve", I("tensor_copy", f2[:, :], ti[:, :]), reads=[ti], writes=[f2])
            P.op("dve", I("tensor_tensor", f1[:, :], f1[:, :], f2[:, :], ALU.subtract), reads=[f1, f2], writes=[f1])
            P.op("dve", I("scalar_tensor_tensor", f2[:, :], f1[:, :], 0.5, f1[:, :], ALU.is_gt, ALU.subtract), reads=[f1], writes=[f2])
            P.op("dve", I("scalar_tensor_tensor", f1[:, :], f2[:, :], 0.5, f2[:, :], ALU.is_gt, ALU.subtract), reads=[f2], writes=[f1])
            P.op("act", I("activation", tab[:, :], f1[:, :], AF.Sin, scale=6.2831845), reads=[f1], writes=[tab])
            ssq, sskv = bank[3], bank[4]

            def proj(off):
                pb_ = bank[proj.i % 3]
                proj.i += 1
                P.op("pe", [I("matmul", pb_[:, :], Wfm[:, kc, off:off + 128], h[:, kc, :], start=(kc == 0), stop=(kc == 7)) for kc in range(8)],
                     reads=[Wfm, h], writes=[pb_])
                return pb_
            proj.i = 0
            for m in range(4):
                pb_ = proj(O_CQ + m * 128)
                s_ = sqb[m % 2]
                P.op("act", I("copy", cq_f[:, m, :], pb_[:, :]), reads=[pb_], writes=[cq_f])
                P.op("act", I("activation", s_[:, :], pb_[:, :], AF.Square), reads=[pb_], writes=[s_])
                P.op("pe", I("matmul", ssq[:, :], ones_b[:, :], s_[:, :], start=(m == 0), stop=(m == 3)), reads=[s_, ones_b], writes=[ssq])
            for m in range(2):
                pb_ = proj(O_CKV + m * 128)
                s_ = sqb[m % 2]
                P.op("act", I("copy", ckv_f[:, m, :], pb_[:, :]), reads=[pb_], writes=[ckv_f])
                P.op("act", I("activation", s_[:, :], pb_[:, :], AF.Square), reads=[pb_], writes=[s_])
                P.op("pe", I("matmul", sskv[:, :], ones_b[:, :], s_[:, :], start=(m == 0), stop=(m == 1)), reads=[s_, ones_b], writes=[sskv])
            rsq(rstd[0][:, :], ssq[:, :], 512, ([ssq], [rstd[0]]))
            rsq(rstd[1][:, :], sskv[:, :], 256, ([sskv], [rstd[1]]))
            for m in range(4):
                P.op("dve" if m % 2 == 0 else "pool", I("tensor_tensor", cqn[:, m, :], cq_f[:, m, :], rstd[0][:, :], ALU.mult), reads=[cq_f, rstd[0]], writes=[cqn])
            for m in range(2):
                P.op("dve" if m % 2 == 0 else "pool", I("tensor_tensor", ckvn[:, m, :], ckv_f[:, m, :], rstd[1][:, :], ALU.mult), reads=[ckv_f, rstd[1]], writes=[ckvn])
            pb_ = proj(O_KR)
            P.op("act", I("copy", krf[:, :], pb_[:, :]), reads=[pb_], writes=[krf])
            for off, dst in ((O_LX, xpl), (O_GQ, xpq), (O_GK, xpk), (O_GV, xpv)):
                pb_ = proj(off)
                P.op("act", I("copy", dst[:, 3:515], pb_[:, :]), reads=[pb_], writes=[dst])
            pb_ = proj(O_LG)
            P.op("act", I("copy", lgf[:, :], pb_[:, :]), reads=[pb_], writes=[lgf])
            pb_ = proj(O_BR)
            P.op("act", I("activation", bbc[:, :], pb_[:, :], AF.Sigmoid), reads=[pb_], writes=[bbc])
            qn_ps, qr_ps, ss_ps, ro_ps = bank[0], bank[1], bank[2], bank[3]
            P.op("pe", [I("matmul", qn_ps[:, :], Wuq[:, kc, 0:128], cqn[:, kc, :], start=(kc == 0), stop=(kc == 3)) for kc in range(4)], reads=[Wuq, cqn], writes=[qn_ps])
            P.op("pe", [I("matmul", qr_ps[:, :], Wuq[:, kc, 128:256], cqn[:, kc, :], start=(kc == 0), stop=(kc == 3)) for kc in range(4)], reads=[Wuq, cqn], writes=[qr_ps])
            P.op("act", I("activation", sqb[0][:, :], qn_ps[:, :], AF.Square), reads=[qn_ps], writes=[sqb[0]])
            P.op("act", I("activation", sqb[1][0:64, :], qr_ps[0:64, :], AF.Square), reads=[qr_ps], writes=[sqb[1]])
            P.op("pe", [I("matmul", ss_ps[:, :], ones_b[:, :], sqb[0][:, :], start=True, stop=False),
                        I("matmul", ss_ps[:, :], ones_b[0:64, :], sqb[1][0:64, :], start=False, stop=True)], reads=[sqb[0], sqb[1], ones_b], writes=[ss_ps])
            rsq(rstd[0][:, :], ss_ps[:, :], 192, ([ss_ps], [rstd[0]]))
            P.op("dve", I("scalar_tensor_tensor", QnT[:, :], qn_ps[:, :], dv[:, 0:1], rstd[0][:, :], ALU.mult, ALU.mult), reads=[qn_ps, dv, rstd[0]], writes=[QnT])
            P.op("dve", I("scalar_tensor_tensor", f2[:, :], qr_ps[:, :], dv[:, 1:2], rstd[0][:, :], ALU.mult, ALU.mult), reads=[qr_ps, dv, rstd[0]], writes=[f2])
            P.op("pool", I("tensor_tensor", f2[:, :], f2[:, :], tab[:, :], ALU.mult), reads=[f2, tab], writes=[f2])
            P.op("pe", I("matmul", ro_ps[:, :], SI, f2[:, :], start=True, stop=True), reads=[mc, f2], writes=[ro_ps])
            P.op("act", I("copy", QrT[:, :], ro_ps[:, :]), reads=[ro_ps], writes=[QrT])
            kn_ps = bank[0]
            P.op("pe", [I("matmul", kn_ps[:, :], Wukv[:, kc, 0:128], ckvn[:, kc, :], start=(kc == 0), stop=(kc == 1)) for kc in range(2)], reads=[Wukv, ckvn], writes=[kn_ps])
            P.op("act", I("activation", sqb[0][:, :], kn_ps[:, :], AF.Square), reads=[kn_ps], writes=[sqb[0]])
            P.op("act", I("activation", sqb[1][0:64, :], krf[0:64, :], AF.Square), reads=[krf], writes=[sqb[1]])
            ss_ps = bank[2]
            P.op("pe", [I("matmul", ss_ps[:, :], ones_b[:, :], sqb[0][:, :], start=True, stop=False),
                        I("matmul", ss_ps[:, :], ones_b[0:64, :], sqb[1][0:64, :], start=False, stop=True)], reads=[sqb[0], sqb[1], ones_b], writes=[ss_ps])
            rsq(rstd[1][:, :], ss_ps[:, :], 192, ([ss_ps], [rstd[1]]))
            P.op("dve", I("scalar_tensor_tensor", KnT[:, gsl], kn_ps[:, :], vec[:, 2:3], rstd[1][:, :], ALU.mult, ALU.mult), reads=[kn_ps, vec, rstd[1]], writes=[KnT])
            P.op("dve", I("scalar_tensor_tensor", f2[:, :], krf[:, :], dv[:, 2:3], rstd[1][:, :], ALU.mult, ALU.mult), reads=[krf, dv, rstd[1]], writes=[f2])
            P.op("pool", I("tensor_tensor", f2[:, :], f2[:, :], tab[:, :], ALU.mult), reads=[f2, tab], writes=[f2])
            ro_ps = bank[3]
            P.op("pe", I("matmul", ro_ps[:, :], SI, f2[:, :], start=True, stop=True), reads=[mc, f2], writes=[ro_ps])
            kr_rows = slice(0, 64) if g < 8 else slice(64, 128)
            kr_cols = slice((g % 8) * 512, (g % 8 + 1) * 512)
            P.op("act", I("copy", KrT[kr_rows, kr_cols], ro_ps[kr_rows, :]), reads=[ro_ps], writes=[KrT])
            for tl in range(4):
                v_ps = bank[tl % 2]
                P.op("pe", [I("matmul", v_ps[:, 0:128], ckvn[:, kc, tl * 128:(tl + 1) * 128], Wukv[:, kc, 128:256], start=(kc == 0), stop=(kc == 1)) for kc in range(2)],
                     reads=[ckvn, Wukv], writes=[v_ps])
                P.op("act", I("copy", Vr[:, 4 * g + tl, :], v_ps[:, 0:128]), reads=[v_ps], writes=[Vr])
            P.capture()
            nkb = 4 * g + 4 if 'att' in stages else 0
            for kb in range(nkb):
                c = kb - 4 * g
                q0 = 128 * max(c, 0)
                st = bank[kb % 2]
                pt = PT[kb % 3]
                rr = slice(0, 64) if kb < 32 else slice(64, 128)
                kc0 = (kb % 32) * 128
                P.op("pe", [I("matmul", st[:, q0:512], KnT[:, kb * 128:(kb + 1) * 128], QnT[:, q0:512], start=True, stop=False),
                            I("matmul", st[:, q0:512], KrT[rr, kc0:kc0 + 128], QrT[rr, q0:512], start=False, stop=True)],
                     reads=[KnT, KrT, QnT, QrT], writes=[st])
                P.op("act", I("activation", pt[:, q0:512], st[:, q0:512], AF.Exp), reads=[st], writes=[pt])
                if c >= 0:
                    P.op("pool", I("memset", pt[64:128, q0:q0 + 64], 0.0), reads=[pt], writes=[pt])
                P.op("pe", [I("matmul", OB[:, q0:512], Vr[:, kb, :], pt[:, q0:512], start=(kb == 0), stop=(kb == nkb - 1)),
                            I("matmul", LB[:, q0:512], ones_b[:, :], pt[:, q0:512], start=(kb == 0), stop=(kb == nkb - 1))],
                     reads=[Vr, pt, ones_b], writes=[OB, LB])
            if 'att' in stages:
                P.op("dve", I("reciprocal", f2[:, :], LB[:, :]), reads=[LB], writes=[f2])
                P.op("dve", I("tensor_tensor", ys[:, 0, :], OB[:, :], f2[:, :], ALU.mult), reads=[OB, f2], writes=[ysA])
            else:
                P.op("dve", I("tensor_copy", ys[:, 0, :], QnT[:, :]), reads=[QnT], writes=[ysA])
                P.op("dve", I("tensor_copy", ys[:, 1, :], QrT[:, :]), reads=[QrT], writes=[ysA])
                P.op("dve", I("tensor_copy", ys[:, 2, :], KnT[:, gsl]), reads=[KnT], writes=[ysA])
            s_att = P.end_capture()
            P.capture()
            if 'lru' in stages:
                P.op("dve", I("tensor_scalar", xc[:, :], xpl[:, 0:512], vec[:, 6:7], vec[:, 5:6], ALU.mult, ALU.add), reads=[xpl, vec], writes=[xc])
                for k in range(1, 4):
                    P.op("dve", I("scalar_tensor_tensor", xc[:, :], xpl[:, k:k + 512], vec[:, 6 + k:7 + k], xc[:, :], ALU.mult, ALU.add), reads=[xpl, vec, xc], writes=[xc])
                P.op("pool", I("tensor_copy", xpl[:, 0:3], xpl[:, 512:515]), reads=[xpl], writes=[xpl])
                P.op("pool", I("tensor_copy", xcb[:, :], xc[:, :]), reads=[xc], writes=[xcb])
                r_ps, i_ps = nb(), nb()
                P.op("pe", I("matmul", r_ps[:, :], Wa[:, :], xcb[:, :], start=True, stop=True), reads=[Wa, xcb], writes=[r_ps])
                P.op("pe", I("matmul", i_ps[:, :], Wx[:, :], xcb[:, :], start=True, stop=True), reads=[Wx, xcb], writes=[i_ps])
                P.op("act", I("activation", lr[:, :], r_ps[:, :], AF.Sigmoid, bias=vec[:, 10:11]), reads=[r_ps, vec], writes=[lr])
                P.op("act", I("activation", li[:, :], i_ps[:, :], AF.Sigmoid, bias=vec[:, 11:12]), reads=[i_ps, vec], writes=[li])
                P.op("act", I("activation", la[:, :], lr[:, :], AF.Exp, scale=dv[:, 3:4]), reads=[lr, dv], writes=[la])
                P.op("act", I("activation", lu[:, :], lr[:, :], AF.Exp, scale=dv[:, 4:5]), reads=[lr, dv], writes=[lu])
                P.op("dve", I("tensor_scalar", lu[:, :], lu[:, :], -1.0, 1.0, ALU.mult, ALU.add), reads=[lu], writes=[lu])
                P.op("act", I("activation", lu[:, :], lu[:, :], AF.Sqrt), reads=[lu], writes=[lu])
                P.op("pool", I("tensor_tensor", li[:, :], li[:, :], xc[:, :], ALU.mult), reads=[li, xc], writes=[li])
                P.op("pool", I("tensor_tensor", lu[:, :], lu[:, :], li[:, :], ALU.mult), reads=[lu, li], writes=[lu])
                P.op("dve", I("tensor_tensor_scan", lh[:, :], la[:, :], lu[:, :], hlast[:, 0:1], ALU.mult, ALU.add), reads=[la, lu, hlast], writes=[lh])
                P.op("dve", I("tensor_copy", hlast[:, 0:1], lh[:, 511:512]), reads=[lh], writes=[hlast])
                P.op("pool", I("tensor_tensor", lr[:, :], lgf[:, :], lgf[:, :], ALU.mult), reads=[lgf], writes=[lr])
                P.op("pool", I("tensor_scalar", lr[:, :], lr[:, :], 0.044715, 1.0, ALU.mult, ALU.add), reads=[lr], writes=[lr])
                P.op("pool", I("tensor_tensor", lr[:, :], lr[:, :], lgf[:, :], ALU.mult), reads=[lr, lgf], writes=[lr])
                P.op("act", I("activation", lr[:, :], lr[:, :], AF.Sigmoid, scale=1.5957691216), reads=[lr], writes=[lr])
                P.op("pool", I("tensor_tensor", lr[:, :], lr[:, :], lgf[:, :], ALU.mult), reads=[lr, lgf], writes=[lr])
                P.op("dve", I("tensor_tensor", ys[:, 1, :], lh[:, :], lr[:, :], ALU.mult), reads=[lh, lr], writes=[ysL])
            s_lru = P.end_capture()
            P.capture()
            if 'gdn' in stages:
                for xp, dst, w0 in ((xpq, QT, 13), (xpk, KT, 17), (xpv, VT, 21)):
                    P.op("pool", I("tensor_scalar", gt1[:, :], xp[:, 0:512], vec[:, w0:w0 + 1], None, ALU.mult), reads=[xp, vec], writes=[gt1])
                    for k in range(1, 4):
                        P.op("dve", I("scalar_tensor_tensor", gt1[:, :], xp[:, k:k + 512], vec[:, w0 + k:w0 + k + 1], gt1[:, :], ALU.mult, ALU.add), reads=[xp, vec, gt1], writes=[gt1])
                    P.op("pool", I("tensor_copy", xp[:, 0:3], xp[:, 512:515]), reads=[xp], writes=[xp])
                    P.op("act", I("activation", dst[:, :], gt1[:, :], AF.Silu), reads=[gt1], writes=[dst])
                for src, sc in ((QT, 128 ** -0.5), (KT, 1.0)):
                    P.op("act", I("activation", sqb[0][:, :], src[:, :], AF.Square), reads=[src], writes=[sqb[0]])
                    ss_ps = nb()
                    P.op("pe", I("matmul", ss_ps[:, :], ones_b[:, :], sqb[0][:, :], start=True, stop=True), reads=[sqb[0], ones_b], writes=[ss_ps])
                    rsq(rstd[0][:, :], ss_ps[:, :], 1.0, ([ss_ps], [rstd[0]]))
                    P.op("dve", I("scalar_tensor_tensor", src[:, :], src[:, :], sc, rstd[0][:, :], ALU.mult, ALU.mult), reads=[src, rstd[0]], writes=[src])
                s_gpre = P.end_capture()
                s_prep, s_chain = [], []
                for tl in range(4):
                    T_, gb, decb = TT[tl % 2], gbs[tl % 2], decbs[tl % 2]
                    P.capture()
                    tsl = slice(tl * 128, (tl + 1) * 128)
                    tm = nb()
                    P.op("pe", [I("matmul", tm[:, 0:NTM], h[:, kc, tsl], Wtm[:, kc, :], start=(kc == 0), stop=(kc == 7)) for kc in range(8)], reads=[h, Wtm], writes=[tm])
                    P.op("act", I("activation", T_["sz"][:, :], tm[:, 0:128], AF.Silu), reads=[tm], writes=[T_["sz"]])
                    P.op("act", I("activation", gb[:, 0:1], tm[:, 128:129], AF.Exp, bias=vec[:, 26:27]), reads=[tm, vec, gb], writes=[gb])
                    P.op("act", I("activation", gb[:, 1:2], gb[:, 0:1], AF.Ln, bias=1.0), reads=[gb], writes=[gb])
                    P.op("act", I("activation", gb[:, 3:4], tm[:, 129:130], AF.Exp, scale=-1.0), reads=[tm, gb], writes=[gb])
                    P.op("dve", I("tensor_scalar", gb[:, 3:4], gb[:, 3:4], 1.0, None, ALU.add), reads=[gb], writes=[gb])
                    P.op("dve", I("reciprocal", gb[:, 3:4], gb[:, 3:4]), reads=[gb], writes=[gb])
                    P.op("dve", I("tensor_tensor", gb[:, 2:3], gb[:, 1:2], dv[:, 5:6], ALU.mult), reads=[gb, dv], writes=[gb])
                    P.op("dve", I("tensor_scalar", T_["gU"][:, :], U2, gb[:, 2:3], None, ALU.mult), reads=[mc, gb], writes=[T_["gU"]])
                    P.op("dve", I("tensor_scalar", T_["gneg"][:, :], ones_f[:, :], gb[:, 2:3], -1.0, ALU.mult, ALU.mult), reads=[ones_f, gb], writes=[T_["gneg"]])
                    D_ps = nb()
                    P.op("pe", [I("matmul", D_ps[:, 0:128], T_["gU"][:, :], ones_f[:, :], start=True, stop=False),
                                I("matmul", D_ps[:, 0:128], T_["gneg"][:, :], U2, start=False, stop=True),
                                I("matmul", D_ps[:, 128:129], U2, gb[:, 2:3], start=True, stop=True),
                                I("matmul", D_ps[:, 129:130], B2, gb[:, 2:3], start=True, stop=True)],
                         reads=[T_["gU"], T_["gneg"], ones_f, mc, gb], writes=[D_ps])
                    G_ps = nb()
                    P.op("pe", [I("matmul", G_ps[:, 0:128], T_["gneg"][:, :], U2, start=True, stop=True),
                                I("matmul", G_ps[:, 128:130], T_["gneg"][:, :], CI[:, 0:2], start=True, stop=True)],
                         reads=[T_["gneg"], mc], writes=[G_ps])
                    P.op("dve", I("tensor_tensor", T_["DmL"][:, :], D_ps[:, 0:128], MLs, ALU.add), reads=[D_ps, mc], writes=[T_["DmL"]])
                    P.op("dve", I("scalar_tensor_tensor", T_["DmU"][:, :], D_ps[:, 0:128], -1.0, MU, ALU.mult, ALU.add), reads=[D_ps, mc], writes=[T_["DmU"]])
                    P.op("act", I("activation", T_["decLs"][:, :], T_["DmL"][:, :], AF.Exp), reads=[T_["DmL"]], writes=[T_["decLs"]])
                    P.op("act", I("activation", T_["decU"][:, :], T_["DmU"][:, :], AF.Exp), reads=[T_["DmU"]], writes=[T_["decU"]])
                    P.op("pool", I("tensor_tensor", T_["decUs"][:, :], T_["decU"][:, :], SU01, ALU.mult), reads=[T_["decU"], mc], writes=[T_["decUs"]])
                    P.op("act", I("copy", gb[:, 10:12], D_ps[:, 128:130]), reads=[D_ps, gb], writes=[gb])
                    P.op("act", I("activation", gb[:, 4:5], gb[:, 10:11], AF.Exp), reads=[gb], writes=[gb])
                    P.op("dve", I("tensor_tensor", gb[:, 6:7], gb[:, 11:12], gb[:, 10:11], ALU.subtract), reads=[gb], writes=[gb])
                    P.op("act", I("activation", gb[:, 5:6], gb[:, 6:7], AF.Exp), reads=[gb], writes=[gb])
                    P.op("dve", I("tensor_tensor", gb[:, 7:8], gb[:, 3:4], gb[:, 4:5], ALU.mult), reads=[gb], writes=[gb])
                    P.op("act", I("activation", T_["egbc"][:, :], G_ps[:, 0:128], AF.Exp, scale=-1.0), reads=[G_ps], writes=[T_["egbc"]])
                    P.op("act", I("activation", decb[:, 0:2], G_ps[:, 128:130], AF.Exp, scale=-1.0), reads=[G_ps], writes=[decb])
                    P.op("pool", I("tensor_tensor", T_["KTb"][:, :], KT[:, tsl], bbc[:, tsl], ALU.mult), reads=[KT, bbc], writes=[T_["KTb"]])
                    P.op("pool", I("tensor_tensor", T_["QDT"][:, :], QT[:, tsl], T_["egbc"][:, :], ALU.mult), reads=[QT, T_["egbc"]], writes=[T_["QDT"]])
                    X_ps, Y_ps = nb(), nb()
                    A_ps = X_ps
                    P.op("pe", I("matmul", X_ps[:, 0:128], KT[:, tsl], T_["KTb"][:, :], start=True, stop=True), reads=[KT, T_["KTb"]], writes=[X_ps])
                    P.op("pe", I("matmul", Y_ps[:, 0:128], T_["KTb"][:, :], KT[:, tsl], start=True, stop=True), reads=[KT, T_["KTb"]], writes=[Y_ps])
                    P.op("pe", I("matmul", A_ps[:, 128:256], KT[:, tsl], QT[:, tsl], start=True, stop=True), reads=[KT, QT], writes=[A_ps])
                    P.op("dve", I("tensor_tensor", T_["X0"][:, :], X_ps[:, 0:128], T_["decUs"][:, :], ALU.mult), reads=[X_ps, T_["decUs"]], writes=[T_["X0"]])
                    P.op("dve", I("tensor_tensor", T_["Y0"][:, :], Y_ps[:, 0:128], T_["decLs"][:, :], ALU.mult), reads=[Y_ps, T_["decLs"]], writes=[T_["Y0"]])
                    P.op("dve", I("tensor_tensor", T_["attnT"][:, :], A_ps[:, 128:256], T_["decU"][:, :], ALU.mult), reads=[A_ps, T_["decU"]], writes=[T_["attnT"]])
                    P.op("pool", I("tensor_tensor", T_["TtA"][:, :], ident_f[:, :], T_["X0"][:, :], ALU.subtract), reads=[ident_f, T_["X0"]], writes=[T_["TtA"]])
                    Xp, Yp, Tt = T_["X0"], T_["Y0"], T_["TtA"]
                    for n in range(1, 6):
                        Yn = T_["YA"] if n % 2 else T_["YB"]
                        Xn = T_["XA"] if n % 2 else T_["XB"]
                        Tn = T_["TtB"] if n % 2 else T_["TtA"]
                        yp_ = nb()
                        P.op("pe", I("matmul", yp_[:, 0:128], Xp[:, :], Yp[:, :], start=True, stop=True), reads=[Xp, Yp], writes=[yp_])
                        P.op("act", I("copy", Yn[:, :], yp_[:, 0:128]), reads=[yp_], writes=[Yn])
                        if n < 5:
                            xp_ = nb()
                            P.op("pe", I("matmul", xp_[:, 0:128], Yp[:, :], Xp[:, :], start=True, stop=True), reads=[Xp, Yp], writes=[xp_])
                            P.op("act", I("copy", Xn[:, :], xp_[:, 0:128]), reads=[xp_], writes=[Xn])
                        cp_ = nb()
                        P.op("pe", I("matmul", cp_[:, 0:128], Yn[:, :], Tt[:, :], start=True, stop=True), reads=[Yn, Tt], writes=[cp_])
                        P.op("dve", I("tensor_tensor", Tn[:, :], cp_[:, 0:128], Tt[:, :], ALU.add), reads=[cp_, Tt], writes=[Tn])
                        Xp, Yp, Tt = Xn, Yn, Tn
                    kt_ps, vt_ps = nb(), nb()
                    P.op("pe", I("transpose", kt_ps[:, 0:128], KT[:, tsl], ident_f[:, :]), reads=[KT, ident_f], writes=[kt_ps])
                    P.op("pe", I("transpose", vt_ps[:, 0:128], VT[:, tsl], ident_f[:, :]), reads=[VT, ident_f], writes=[vt_ps])
                    P.op("dve", I("tensor_scalar", T_["kbg"][:, :], kt_ps[:, 0:128], gb[:, 7:8], None, ALU.mult), reads=[kt_ps, gb], writes=[T_["kbg"]])
                    P.op("dve", I("tensor_scalar", T_["ke"][:, :], kt_ps[:, 0:128], gb[:, 5:6], None, ALU.mult), reads=[kt_ps, gb], writes=[T_["ke"]])
                    P.op("dve", I("tensor_scalar", T_["vb"][:, :], vt_ps[:, 0:128], gb[:, 3:4], None, ALU.mult), reads=[vt_ps, gb], writes=[T_["vb"]])
                    u_ps, w_ps = nb(), nb()
                    P.op("pe", I("matmul", u_ps[:, 0:128], Tt[:, :], T_["vb"][:, :], start=True, stop=True), reads=[Tt, T_["vb"]], writes=[u_ps])
                    P.op("pe", I("matmul", w_ps[:, 0:128], T_["kbg"][:, :], Tt[:, :], start=True, stop=True), reads=[Tt, T_["kbg"]], writes=[w_ps])
                    P.op("act", I("copy", T_["u"][:, :], u_ps[:, 0:128]), reads=[u_ps], writes=[T_["u"]])
                    P.op("act", I("copy", T_["wT"][:, :], w_ps[:, 0:128]), reads=[w_ps], writes=[T_["wT"]])
                    s_prep.append(P.end_capture())
                    P.capture()
                    for c in range(2):
                        cs = slice(64 * c, 64 * c + 64)
                        vn_ps = bank[4]
                        P.op("pe", I("matmul", vn_ps[cs, 0:128], T_["wT"][:, cs], Sst[:, :], start=True, stop=True), reads=[T_["wT"], Sst], writes=[vn_ps])
                        P.op("dve", I("tensor_tensor", T_["vnew"][cs, :], T_["u"][cs, :], vn_ps[cs, 0:128], ALU.subtract), reads=[T_["u"], vn_ps], writes=[T_["vnew"]])
                        P.op("pe", [I("matmul", OPS[cs, 0:128], T_["QDT"][:, cs], Sst[:, :], start=True, stop=False),
                                    I("matmul", OPS[cs, 0:128], T_["attnT"][cs, cs], T_["vnew"][cs, :], start=False, stop=True)],
                             reads=[T_["QDT"], Sst, T_["attnT"], T_["vnew"]], writes=[OPS])
                        s_ps = bank[4]
                        P.op("pe", I("matmul", s_ps[:, 128:256], T_["ke"][cs, :], T_["vnew"][cs, :], start=True, stop=True), reads=[T_["ke"], T_["vnew"]], writes=[s_ps])
                        P.op("dve", I("scalar_tensor_tensor", Sst[:, :], Sst[:, :], decb[:, c:c + 1], s_ps[:, 128:256], ALU.mult, ALU.add), reads=[Sst, decb, s_ps], writes=[Sst])
                    P.op("act", I("activation", T_["junk"][:, :], OPS[:, 0:128], AF.Square, accum_out=gb[:, 8:9]), reads=[OPS, gb], writes=[T_["junk"], gb])
                    P.op("act", I("activation", gb[:, 9:10], gb[:, 8:9], AF.Ln, bias=EPS, scale=1.0 / 128), reads=[gb], writes=[gb])
                    P.op("act", I("activation", gb[:, 9:10], gb[:, 9:10], AF.Exp, scale=-0.5), reads=[gb], writes=[gb])
                    P.op("dve", I("scalar_tensor_tensor", T_["t1"][:, :], OPS[:, 0:128], gb[:, 9:10], ggbc[:, :], ALU.mult, ALU.mult), reads=[OPS, gb, ggbc], writes=[T_["t1"]])
                    P.op("pool", I("tensor_tensor", T_["t2"][:, :], T_["t1"][:, :], T_["sz"][:, :], ALU.mult), reads=[T_["t1"], T_["sz"]], writes=[T_["t2"]])
                    o_ps = bank[4]
                    P.op("pe", I("transpose", o_ps[:, 256:384], T_["t2"][:, :], ident_f[:, :]), reads=[T_["t2"], ident_f], writes=[o_ps])
                    P.op("act", I("copy", ys[:, 2, tsl], o_ps[:, 256:384]), reads=[o_ps], writes=[ysG])
                    s_chain.append(P.end_capture())
            if 'gdn' in stages:
                s_gdn = s_gpre + s_prep[0]
                for tl in range(4):
                    s_gdn = s_gdn + (merge_streams([s_chain[tl], s_prep[tl + 1]]) if tl < 3 else s_chain[tl])
            else:
                s_gdn = P.end_capture()
            P.replay(s_lru)
            P.replay(merge_streams([s_att, s_gdn]))
            P.dma("sp", io["yT_out"][g // 4][:, :, (g % 4) * 512:(g % 4 + 1) * 512].rearrange("b f n -> f b n"), ys[:, :, :], ysA,
                  reads=[ysA, ysL, ysG], writes=[io["yT_buf"]], merge=True)
            if "y_ag" in io and g % 4 == 3:
                io["y_ag"](g // 4)
        P.barrier()


def consts_FN():
    return np.concatenate([np.eye(128, dtype=np.float32), np.ones((128, 128), np.float32)], 1)

def mconst():
    k = np.arange(128)[:, None]; i = np.arange(128)[None, :]
    same = (k // 64) == (i // 64)
    U2 = (same & (k <= i)).astype(np.float32)
    B2 = same.astype(np.float32)
    MLs = np.where(same & (k > i), 0.0, -3e4).astype(np.float32)
    MU = np.where(same & (i >= k), 0.0, -3e4).astype(np.float32)
    SU01 = (same & (i > k)).astype(np.float32)
    SI = ((k % 64) == (i % 64)).astype(np.float32)
    CI = np.zeros((128, 128), np.float32); CI[:64, 0] = 1; CI[64:, 1] = 1
    return np.ascontiguousarray(np.concatenate([U2, B2, MLs, MU, SU01, SI, CI], 1))

def prep_M(inp, L, b, j):
    w = inp['w_in'][L]
    kr = w[:, 768:832]
    krp = np.concatenate([kr[:, 32:64], kr[:, 0:32]], 1)
    cols = [w[:, 0:512], w[:, 512:768], kr, krp, w[:, 832 + 128 * j:832 + 128 * (j + 1)], w[:, 1344 + 128 * j:1344 + 128 * (j + 1)],
            w[:, 1856 + 128 * j:1856 + 128 * (j + 1)], w[:, 2368 + 128 * j:2368 + 128 * (j + 1)], w[:, 2880 + 128 * j:2880 + 128 * (j + 1)],
            np.repeat(w[:, 3908 + j:3909 + j], 128, axis=1)]
    win_fm = np.ascontiguousarray(np.concatenate(cols, 1))
    win_tm = np.ascontiguousarray(np.concatenate([w[:, 3392 + 128 * j:3392 + 128 * (j + 1)], w[:, 3904 + j:3905 + j], w[:, 3908 + j:3909 + j]], 1))
    uq = inp['mla_w_uq'][L][:, j * 192:(j + 1) * 192]
    wuq = np.ascontiguousarray(np.concatenate([uq, uq[:, 160:192], uq[:, 128:160]], 1))
    wukv = np.ascontiguousarray(inp['mla_w_ukv'][L][:, j * 256:(j + 1) * 256])
    vec = np.zeros((128, 32), np.float32)
    qh = inp['mla_q_head_norm'][L]; kh = inp['mla_k_head_norm'][L]
    vec[:, 0] = qh[0:128]
    vec[:, 1] = np.concatenate([qh[128:192], qh[160:192], qh[128:160]])
    vec[:, 2] = kh[0:128]
    vec[:, 3] = np.concatenate([kh[128:192], kh[160:192], kh[128:160]])
    vec[:, 4] = np.concatenate([np.ones(64), -np.ones(32), np.ones(32)])
    cs = slice(128 * j, 128 * (j + 1))
    vec[:, 5] = inp['lru_conv_b'][L][cs]
    for k in range(4):
        vec[:, 6 + k] = inp['lru_conv_w'][L][k, cs]
    vec[:, 10] = inp['lru_b_a'][L][cs]; vec[:, 11] = inp['lru_b_x'][L][cs]; vec[:, 12] = inp['lru_lambda'][L][cs]
    gw = inp['gdn_conv_w'][L]
    for k in range(4):
        vec[:, 13 + k] = gw[k, 128 * j:128 * (j + 1)]
        vec[:, 17 + k] = gw[k, 512 + 128 * j:512 + 128 * (j + 1)]
        vec[:, 21 + k] = gw[k, 1024 + 128 * j:1024 + 128 * (j + 1)]
    vec[:, 25] = inp['gdn_a_log'][L][j]; vec[:, 26] = inp['gdn_dt_bias'][L][j]
    invf = (np.float32(10000.0) ** (-np.arange(0, 64, 2, dtype=np.float32) / np.float32(64))).astype(np.float32)
    vec[:, 27] = np.tile(invf, 4)
    vec[:, 28] = np.concatenate([np.full(64, 0.25), np.zeros(64)])
    wa = np.zeros((128, 128), np.float32); wx = np.zeros((128, 128), np.float32)
    for t in range(2):
        wa[64 * t:64 * (t + 1), 64 * t:64 * (t + 1)] = inp['lru_w_a'][L][2 * j + t]
        wx[64 * t:64 * (t + 1), 64 * t:64 * (t + 1)] = inp['lru_w_x'][L][2 * j + t]
    return dict(win_fm=win_fm, win_tm=win_tm, wuq=wuq, wukv=wukv, vecs=vec,
                gq=np.ascontiguousarray(inp['mla_q_norm'][L].reshape(4, 128).T), gkv=np.ascontiguousarray(inp['mla_kv_norm'][L].reshape(2, 128).T),
                wa_bd=wa, wx_bd=wx, gdn_gain_bc=np.ascontiguousarray(np.broadcast_to(inp['gdn_out_norm'][L], (128, 128))),
                posb=np.ascontiguousarray(np.broadcast_to(inp['positions'][b].astype(np.int32), (128, 8192))), mconst=mconst())

M_SHAPES = dict(win_fm=([1024, 1664], 'f'), win_tm=([1024, 130], 'f'), wuq=([512, 256], 'f'), wukv=([256, 256], 'f'), vecs=([128, 32], 'f'),
                gq=([128, 4], 'f'), gkv=([128, 2], 'f'), wa_bd=([128, 128], 'f'), wx_bd=([128, 128], 'f'), gdn_gain_bc=([128, 128], 'f'),
                posb=([128, 8192], 'i'), mconst=([128, 896], 'f'))


BF_NP = ml_dtypes.bfloat16
N_CORES = 8
DEPTH_L = 2
GROUPS = [[0, 1, 2, 3], [4, 5, 6, 7]]
SHARED_M = ("posb", "mconst")


def _din(nc, name, shape, dt=F32):
    return nc.dram_tensor(name, list(shape), dt, kind="ExternalInput").ap()


def _dout(nc, name, shape, dt=F32):
    return nc.dram_tensor(name, list(shape), dt, kind="ExternalOutput").ap()


def _dint(nc, name, shape, dt):
    return nc.dram_tensor(name, list(shape), dt, kind="Internal").ap()


def build_fused():
    nc = bass.Bass("TRN2", target_bir_lowering=False)
    P = Prog(nc)
    x_in = _din(nc, "xloc", [2048, 1024])
    x_out = _dout(nc, "xout", [2048, 1024])
    yidx = _din(nc, "yidx", [128, 12], I32)
    gains = [_din(nc, "mix_gain_bc_%d" % L, [128, 1024]) for L in range(DEPTH_L)]
    shared = {k: _din(nc, k, M_SHAPES[k][0], F32 if M_SHAPES[k][1] == 'f' else I32) for k in SHARED_M}
    hx = _dint(nc, "hx", [4, 1024, 512], BF16)
    hall = _dint(nc, "hall", [4, 4096, 512], BF16)
    ycin = _dint(nc, "ycin", [4, 3, 128, 2048], BF16)
    yall = _dint(nc, "yall", [12, 512, 2048], BF16)
    xmid = _dint(nc, "xmid", [2048, 1024], F32)
    bufs = {k: P.reg(Buf(k)) for k in ["xloc_buf", "yrecv_buf", "xout_buf", "hT_buf", "yT_buf"]}
    C = consts(P, _din(nc, "consts", [128, 256]))

    def gather_h():
        P.barrier()
        for k in range(4):
            P.allgather(hx[k], hall[k], GROUPS)
        P.barrier()

    io = dict(bufs)
    io.update(xloc=x_in, mix_gain_bc=gains[0], hx=hx)
    phase_N0(P, C, io)
    gather_h()
    for L in range(DEPTH_L):
        last = L == DEPTH_L - 1
        moe = L % 2 == 1
        io = dict(bufs)
        for k, (shp, t) in M_SHAPES.items():
            io[k] = shared[k] if k in SHARED_M else _din(nc, "%s_%d" % (k, L), shp, F32 if t == 'f' else I32)
        def y_ag(qq):
            waits = P._deps("pool", [bufs["yT_buf"]], [])
            for br in range(3):
                P.allgather(ycin[qq][br], yall[qq * 3 + br], GROUPS, waits=waits if br == 0 else ())
        io.update(hall=hall, yT_out=ycin, y_ag=y_ag)
        phase_M(P, C, io)
        P.barrier()
        E, FF = (8, 3584) if moe else (1, 2816)
        io = dict(bufs)
        io.update(xloc=x_in if L == 0 else xmid, xout=x_out if last else xmid, yall=yall.rearrange("e r n -> (e r) n"), yidx=yidx,
                  w_out_r=_din(nc, "w_out_r_%d" % L, [3, 4, 128, 1024]), g_mla=_din(nc, "g_mla_%d" % L, [128, 4]),
                  g_lru=_din(nc, "g_lru_%d" % L, [128, 4]), ffn_gain_bc=_din(nc, "ffn_gain_bc_%d" % L, [128, 1024]),
                  wg=_din(nc, "wg_%d" % L, [E, 1024, FF]), wu=_din(nc, "wu_%d" % L, [E, 1024, FF]), wd=_din(nc, "wd_%d" % L, [E, FF, 1024]))
        if moe:
            io["router"] = _din(nc, "router_%d" % L, [1024, 8])
        if not last:
            io.update(mix_gain_bc=gains[L + 1], hx=hx)
        phase_F(P, C, io, dict(moe=moe, n_exp=E, d_ff=FF, last=last))
        if not last:
            gather_h()
    P.barrier()
    P.emit()
    return nc


def _bc(v, n):
    return np.ascontiguousarray(np.broadcast_to(np.asarray(v, np.float32), (128, n)))


def kernel(**inputs):
    inp = {k: np.asarray(v) for k, v in inputs.items()}
    cores = list(range(N_CORES))
    cst = consts_FN()
    x = inp['x'].astype(np.float32, copy=False)
    ins = []
    for c in cores:
        b, q = divmod(c, 4)
        d = dict(xloc=np.ascontiguousarray(x[b, 2048 * q:2048 * (q + 1)]), consts=cst)
        f = np.arange(128, dtype=np.int32)[:, None]
        col = np.arange(12, dtype=np.int32)[None, :]
        d["yidx"] = np.ascontiguousarray((((q * 3 + col // 4) * 4 + col % 4) * 128 + f).astype(np.int32))
        for L in range(DEPTH_L):
            moe = L % 2 == 1
            dm = prep_M(inp, L, b, q)
            for k, v in dm.items():
                d[k if k in SHARED_M else "%s_%d" % (k, L)] = v
            d["mix_gain_bc_%d" % L] = _bc(inp['mix_norm'][L], 1024)
            d["w_out_r_%d" % L] = np.ascontiguousarray(inp['w_out'][L].reshape(3, 4, 128, 1024))
            d["g_mla_%d" % L] = np.ascontiguousarray(inp['mla_out_norm'][L].reshape(4, 128).T)
            d["g_lru_%d" % L] = np.ascontiguousarray(inp['lru_out_norm'][L].reshape(4, 128).T)
            d["ffn_gain_bc_%d" % L] = _bc(inp['ffn_norm'][L], 1024)
            if moe:
                d["wg_%d" % L] = inp['moe_w_gate'][L // 2]
                d["wu_%d" % L] = inp['moe_w_up'][L // 2]
                d["wd_%d" % L] = inp['moe_w_down'][L // 2]
                d["router_%d" % L] = inp['router_w'][L // 2]
            else:
                d["wg_%d" % L] = inp['dense_w_gate'][L // 2][None]
                d["wu_%d" % L] = inp['dense_w_up'][L // 2][None]
                d["wd_%d" % L] = inp['dense_w_down'][L // 2][None]
        ins.append(d)
    res = run_bass_kernel_spmd(build_fused(), ins, core_ids=cores)
    out = np.empty((2, 8192, 1024), np.float32)
    for c in cores:
        out[c // 4, 2048 * (c % 4):2048 * (c % 4 + 1)] = np.asarray(res.results[c]["xout"])
    return out
```

```python
import contextlib, math
import ml_dtypes
import numpy as np
import concourse.bass as bass
import concourse.mybir as mybir
from concourse.bass_utils import run_bass_kernel_spmd

F32 = mybir.dt.float32
BF16 = mybir.dt.bfloat16
I32 = mybir.dt.int32
ALU = mybir.AluOpType
AF = mybir.ActivationFunctionType
AX = mybir.AxisListType


def I(name, *args, **kw):
    return (name, args, kw)


class Buf:
    __slots__ = ("name", "w", "r", "sem", "cnt", "t")

    def __init__(self, name, t=None):
        self.name = name
        self.w = {}
        self.r = {}
        self.sem = None
        self.cnt = 0
        self.t = t

    def __getitem__(self, idx):
        return self.t[idx]


def merge_streams(streams):
    out = []
    idx = [0] * len(streams)
    n = [len(x) for x in streams]
    while True:
        best, bi = None, -1
        for i in range(len(streams)):
            if idx[i] < n[i]:
                f = (idx[i] + 0.5) / n[i]
                if best is None or f < best:
                    best, bi = f, i
        if bi < 0:
            return out
        out.append(streams[bi][idx[bi]])
        idx[bi] += 1


class Prog:
    ENG = ("pe", "dve", "act", "pool", "sp")

    def __init__(self, nc):
        self.nc = nc
        self.q = {e: [] for e in self.ENG}
        self.cnt = {e: 0 for e in self.ENG}
        self.sem = {e: nc.alloc_semaphore("prog_" + e) for e in self.ENG}
        self.waited = {e: {} for e in self.ENG}
        self.nsem = 5
        self.free_sems = []
        self.sem_bufs = []
        self.extra_sems = []
        self.all_bufs = []
        self.ccsem = None
        self._cap = None
        self.cccnt = 0
        self.uid = 0

    def sb(self, name, shape, dt):
        t = self.nc.alloc_sbuf_tensor(name, list(shape), dt)
        return self.reg(Buf(name, t))

    def ps(self, name, shape, dt=F32):
        t = self.nc.alloc_psum_tensor(name, list(shape), dt)
        return self.reg(Buf(name, t))

    def dram(self, name, shape, dt, kind="Internal"):
        t = self.nc.dram_tensor(name, list(shape), dt, kind=kind)
        return self.reg(Buf(name, t))

    def reg(self, b):
        self.all_bufs.append(b)
        return b

    def _bufsem(self, b):
        if b.sem is None:
            if self.free_sems:
                b.sem, b.cnt = self.free_sems.pop()
            else:
                b.sem = self.nc.alloc_semaphore("bs_%d" % self.nsem)
                self.nsem += 1
                b.cnt = 0
            self.sem_bufs.append(b)
        return b.sem

    def barrier(self):
        allw = [(self.sem[e], self.cnt[e]) for e in self.ENG if self.cnt[e] > 0]
        allw += [(b.sem, b.cnt) for b in self.sem_bufs if b.cnt > 0]
        allw += [(s, c) for s, c in self.free_sems if c > 0]
        allw += [(s, c) for s, c in self.extra_sems if c > 0]
        if self.ccsem is not None and self.cccnt > 0:
            allw.append((self.ccsem, self.cccnt))
        for e in self.ENG:
            wd = self.waited[e]
            wl = []
            for s, v in allw:
                if wd.get(s, 0) < v:
                    wd[s] = v
                    wl.append((s, v))
            self.q[e].append((wl, None, None, 0))
        for b in self.sem_bufs:
            self.free_sems.append((b.sem, b.cnt))
            b.sem = None
            b.w = {}
            b.r = {}
        self.sem_bufs = []
        for b in self.all_bufs:
            b.w = {}
            b.r = {}

    def _deps(self, eng, reads, writes):
        deps = {}

        def add(s, v):
            if deps.get(s, 0) < v:
                deps[s] = v
        for b in reads:
            for s, v in b.w.items():
                add(s, v)
        for b in writes:
            for s, v in b.w.items():
                add(s, v)
            for s, v in b.r.items():
                add(s, v)
        wl = []
        wd = self.waited[eng]
        for s, v in deps.items():
            if wd.get(s, 0) < v:
                wd[s] = v
                wl.append((s, v))
        return wl

    def capture(self):
        self._cap = []

    def end_capture(self):
        c, self._cap = self._cap, None
        return c

    def replay(self, items):
        for kind, a, kw in items:
            getattr(self, kind)(*a, **kw)

    def op(self, eng, ins, reads=(), writes=()):
        if self._cap is not None:
            self._cap.append(("op", (eng, ins, reads, writes), {}))
            return None
        if isinstance(ins, tuple):
            ins = [ins]
        e = self._eng(eng)

        def fn(ins=ins, e=e):
            r = None
            for name, args, kw in ins:
                r = getattr(e, name)(*args, **kw)
            return r
        waits = self._deps(eng, reads, writes)
        self.cnt[eng] += 1
        s, v = self.sem[eng], self.cnt[eng]
        self.q[eng].append((waits, fn, s, 1))
        for b in reads:
            if b.r.get(s, 0) < v:
                b.r[s] = v
        for b in writes:
            b.w = {s: v}
            b.r = {}
        return (s, v)

    def dma(self, eng, out, in_, owner, reads=(), writes=(), merge=False, **kw):
        if self._cap is not None:
            self._cap.append(("dma", (eng, out, in_, owner, reads, writes, merge), kw))
            return None
        waits = self._deps(eng, reads, writes if not merge else ())
        s = self._bufsem(owner)
        owner.cnt += 16
        v = owner.cnt
        e = self._eng(eng)

        def fn():
            return e.dma_start(out=out, in_=in_, **kw)
        self.q[eng].append((waits, fn, s, 16))
        for b in reads:
            if b.r.get(s, 0) < v:
                b.r[s] = v
        for b in writes:
            if merge:
                b.w[s] = v
            else:
                b.w = {s: v}
                b.r = {}
        return (s, v)

    def dma_fn(self, eng, fn, owner, reads=(), writes=(), merge=False):
        waits = self._deps(eng, reads, writes if not merge else ())
        s = self._bufsem(owner)
        owner.cnt += 16
        v = owner.cnt
        self.q[eng].append((waits, fn, s, 16))
        for b in reads:
            if b.r.get(s, 0) < v:
                b.r[s] = v
        for b in writes:
            if merge:
                b.w[s] = v
            else:
                b.w = {s: v}
                b.r = {}
        return (s, v)

    def allgather(self, in_ap, out_ap, groups, waits=()):
        nc = self.nc
        if self.ccsem is None:
            self.ccsem = nc.alloc_semaphore("prog_cc")
        self.cccnt += 1

        def fn():
            return nc.gpsimd.collective_compute("AllGather", ALU.bypass, replica_groups=groups, ins=[in_ap.opt()], outs=[out_ap.opt()])
        self.q["pool"].append((list(waits), fn, self.ccsem, 1))

    def wait_all(self, eng, bufs):
        waits = self._deps(eng, bufs, ())
        self.q[eng].append((waits, None, None, 0))

    def _eng(self, e):
        nc = self.nc
        return {"pe": nc.tensor, "dve": nc.vector, "act": nc.scalar, "pool": nc.gpsimd, "sp": nc.sync}[e]

    def emit(self):
        nc = self.nc
        with nc.Block() as block:
            def mk(ename):
                def body(engine):
                    for waits, fn, s, inc in self.q[ename]:
                        for ws, wv in waits:
                            engine.wait_ge(ws, wv)
                        if fn is not None:
                            ins = fn()
                            if s is not None:
                                ins.then_inc(s, inc)
                return body
            block.tensor(mk("pe"))
            block.vector(mk("dve"))
            block.scalar(mk("act"))
            block.gpsimd(mk("pool"))
            block.sync(mk("sp"))


D = 1024
NT = 16
TOK = 2048
EPS = 1e-6


def consts(P, cdram):
    nc = P.nc
    c = {}
    c["ident_f"] = P.sb("ident_f", [128, 128], F32)
    c["ident_b"] = P.sb("ident_b", [128, 128], BF16)
    c["ones_b"] = P.sb("ones_b", [128, 128], BF16)
    c["ones_f"] = P.sb("ones_f", [128, 128], F32)
    idf, idb, ob, of = c["ident_f"], c["ident_b"], c["ones_b"], c["ones_f"]
    P.dma("sp", idf[:, :], cdram[:, 0:128], idf, writes=[idf])
    P.dma("sp", of[:, :], cdram[:, 128:256], of, writes=[of])
    P.op("pool", I("tensor_copy", idb[:, :], idf[:, :]), reads=[idf], writes=[idb])
    P.op("pool", I("tensor_copy", ob[:, :], of[:, :]), reads=[of], writes=[ob])
    return c


def scope(P, es):
    nc = P.nc
    P.uid += 1
    sfx = "_u%d" % P.uid
    def sb(name, shape, dt):
        t = es.enter_context(nc.sbuf_tensor(name + sfx, list(shape), dt))
        return Buf(name, t)
    def ps(name):
        t = es.enter_context(nc.psum_tensor(name + sfx, [128, 512], F32))
        return Buf(name, t)
    return sb, ps


def emit_norm_T(P, C, xt, gain_bc, hT, t, ps_bank, tmpb, junk, ssb, also_f32=None):
    P.op("act", I("activation", junk[:, :], xt[:, :], AF.Square, accum_out=ssb[:, 0:1]), reads=[xt], writes=[junk, ssb])
    P.op("act", I("activation", ssb[:, 1:2], ssb[:, 0:1], AF.Ln, bias=EPS, scale=1.0 / D), reads=[ssb], writes=[ssb])
    P.op("act", I("activation", ssb[:, 2:3], ssb[:, 1:2], AF.Exp, scale=-0.5), reads=[ssb], writes=[ssb])
    if also_f32 is not None:
        hf, hTf, psf = also_f32
        P.op("dve", I("scalar_tensor_tensor", hf[:, :], xt[:, :], ssb[:, 2:3], gain_bc[:, :], ALU.mult, ALU.mult),
             reads=[xt, ssb, gain_bc], writes=[hf])
        P.op("pool", I("tensor_copy", tmpb[:, :], hf[:, :]), reads=[hf], writes=[tmpb])
        for half in range(2):
            P.op("pe", [I("transpose", psf[:, k * 128:(k + 1) * 128], hf[:, (half * 4 + k) * 128:(half * 4 + k + 1) * 128], C["ident_f"][:, :]) for k in range(4)],
                 reads=[hf, C["ident_f"]], writes=[psf])
            P.op("act", I("copy", hTf[:, half * 4:(half + 1) * 4, :], psf[:, :].rearrange("p (k n) -> p k n", k=4)), reads=[psf], writes=[hTf])
    else:
        P.op("dve", I("scalar_tensor_tensor", tmpb[:, :], xt[:, :], ssb[:, 2:3], gain_bc[:, :], ALU.mult, ALU.mult),
             reads=[xt, ssb, gain_bc], writes=[tmpb])
    pb = ps_bank[:, :].bitcast(BF16)
    P.op("pe", [I("transpose", pb[:, kc * 128:(kc + 1) * 128], tmpb[:, kc * 128:(kc + 1) * 128], C["ident_b"][:, :]) for kc in range(8)],
         reads=[tmpb, C["ident_b"]], writes=[ps_bank])
    P.op("act", I("copy", hT[:, :, t * 128:(t + 1) * 128], pb.rearrange("p (k n) -> p k n", k=8)), reads=[ps_bank], writes=[hT])


def store_hT(P, hT, io):
    if "hx" in io:
        hv = io["hx"].rearrange("b (k p) n -> p k b n", p=128)
        for k in range(8):
            P.dma("sp" if k % 2 == 0 else "pool", hv[:, k, :, :], hT[:, k, :].rearrange("p (b n) -> p b n", b=4), hT, reads=[hT], writes=[io["hT_buf"]], merge=True)
        return
    hv = io["hT_out"].rearrange("(k p) n -> p k n", p=128)
    for k in range(8):
        P.dma("sp" if k % 2 == 0 else "pool", hv[:, k, :], hT[:, k, :], hT, reads=[hT], writes=[io["hT_buf"]], merge=True)


def phase_N0(P, C, io):
    with contextlib.ExitStack() as es:
        sb, ps = scope(P, es)
        gain = sb("n0_gain", [128, D], F32)
        hT = sb("n0_hT", [128, 8, TOK], BF16)
        xts = [sb(f"n0_x{i}", [128, D], F32) for i in range(4)]
        tmpb = [sb(f"n0_tb{i}", [128, D], BF16) for i in range(2)]
        junk = sb("n0_junk", [128, D], F32)
        ssb = [sb(f"n0_ss{i}", [128, 4], F32) for i in range(2)]
        pbk = [ps(f"n0_ps{i}") for i in range(2)]
        P.dma("sp", gain[:, :], io["mix_gain_bc"], gain, writes=[gain])
        xv = io["xloc"].rearrange("(t p) d -> t p d", p=128)
        for t in range(NT):
            xt = xts[t % 4]
            P.dma("sp" if t % 2 == 0 else "pool", xt[:, :], xv[t], xt, reads=[io["xloc_buf"]], writes=[xt])
            emit_norm_T(P, C, xt, gain, hT, t, pbk[t % 2], tmpb[t % 2], junk, ssb[t % 2])
        store_hT(P, hT, io)
        P.barrier()


def phase_F(P, C, io, cfg):
    nc = P.nc
    moe = cfg["moe"]
    E = cfg["n_exp"]
    FF = cfg["d_ff"]
    NFC = FF // 128
    with contextlib.ExitStack() as es:
        sb, ps = scope(P, es)
        xl = [sb(f"f_xl{t}", [128, D], F32) for t in range(NT)]
        pbk = [ps(f"f_ps{i}") for i in range(8)]
        xv = io["xloc"].rearrange("(t p) d -> t p d", p=128)
        for t in range(NT):
            P.dma("sp" if t % 2 == 0 else "pool", xl[t][:, :], xv[t], xl[t], reads=[io["xloc_buf"]], writes=[xl[t]])
        with contextlib.ExitStack() as es2:
            sb2, _ = scope(P, es2)
            yT = [sb2(f"f_yT{s}", [128, 3, TOK], BF16) for s in range(4)]
            wo = [sb2(f"f_wo{j}", [128, D], BF16) for j in range(12)]
            wst = [sb2(f"f_wst{i}", [128, D], F32) for i in range(2)]
            gm = sb2("f_gm", [128, 8], F32)
            sq = [sb2(f"f_sq{i}", [128, 4, 2, 128], BF16) for i in range(2)]
            rs = [sb2(f"f_rs{i}", [128, 8], F32) for i in range(2)]
            P.dma("sp", gm[:, 0:4], io["g_mla"], gm, writes=[gm])
            P.dma("sp", gm[:, 4:8], io["g_lru"], gm, writes=[gm], merge=True)
            if "yall" in io:
                yidx = sb2("f_yidx", [128, 12], I32)
                P.dma("sp", yidx[:, :], io["yidx"], yidx, writes=[yidx])
                for s in range(4):
                    for br in range(3):
                        col = br * 4 + s

                        def gfn(s=s, br=br, col=col):
                            return nc.gpsimd.indirect_dma_start(out=yT[s][:, br, :], out_offset=None, in_=io["yall"],
                                                                in_offset=bass.IndirectOffsetOnAxis(ap=yidx[:, col:col + 1], axis=0))
                        P.dma_fn("pool", gfn, yT[s], reads=[yidx, io["yrecv_buf"]], writes=[yT[s]], merge=(br > 0))
            else:
                for s in range(4):
                    P.dma("sp" if s % 2 == 0 else "pool", yT[s][:, :, :], io["yrecv"][s].rearrange("b f n -> f b n"), yT[s],
                          reads=[io["yrecv_buf"]], writes=[yT[s]])
            for j in range(12):
                br, s = j // 4, j % 4
                st = wst[j % 2]
                P.dma("act", st[:, :], io["w_out_r"][br, s], st, writes=[st])
                if br < 2:
                    P.op("pool", I("tensor_scalar", wo[j][:, :], st[:, :], gm[:, br * 4 + s:br * 4 + s + 1], None, ALU.mult),
                         reads=[st, gm], writes=[wo[j]])
                else:
                    P.op("pool", I("tensor_copy", wo[j][:, :], st[:, :]), reads=[st], writes=[wo[j]])
            for t in range(NT):
                tsl = slice(t * 128, (t + 1) * 128)
                sqb = sq[t % 2]
                rsb = rs[t % 2]
                pss = pbk[6 + (t % 2)]
                for s in range(4):
                    P.op("act", I("activation", sqb[:, s, :, :], yT[s][:, 0:2, tsl], AF.Square), reads=[yT[s]], writes=[sqb])
                P.op("pe", [I("matmul", pss[:, 0:1], sqb[:, s, 0, :], C["ones_b"][:, 0:1], start=(s == 0), stop=(s == 3)) for s in range(4)]
                     + [I("matmul", pss[:, 1:2], sqb[:, s, 1, :], C["ones_b"][:, 0:1], start=(s == 0), stop=(s == 3)) for s in range(4)],
                     reads=[sqb, C["ones_b"]], writes=[pss])
                P.op("act", I("activation", rsb[:, 0:2], pss[:, 0:2], AF.Ln, bias=EPS, scale=1.0 / 512), reads=[pss], writes=[rsb])
                P.op("act", I("activation", rsb[:, 2:4], rsb[:, 0:2], AF.Exp, scale=-0.5), reads=[rsb], writes=[rsb])
                xs = xl[t]
                for br in range(3):
                    for dh in range(2):
                        pb_ = pbk[(br * 2 + dh) % 6]
                        dsl = slice(dh * 512, (dh + 1) * 512)
                        P.op("pe", [I("matmul", pb_[:, :], yT[s][:, br, tsl], wo[br * 4 + s][:, dsl], start=(s == 0), stop=(s == 3)) for s in range(4)],
                             reads=yT + wo[br * 4:br * 4 + 4], writes=[pb_])
                        if br < 2:
                            P.op("dve", I("scalar_tensor_tensor", xs[:, dsl], pb_[:, :], rsb[:, 2 + br:3 + br], xs[:, dsl], ALU.mult, ALU.add),
                                 reads=[pb_, rsb, xs], writes=[xs])
                        else:
                            P.op("dve", I("tensor_tensor", xs[:, dsl], pb_[:, :], xs[:, dsl], ALU.add), reads=[pb_, xs], writes=[xs])
            P.barrier()
        h2T = sb("f_h2T", [128, 8, TOK], BF16)
        comb = sb("f_comb", [128, NT, 8], F32) if moe else None
        with contextlib.ExitStack() as es3:
            sb3, _ = scope(P, es3)
            gain = sb3("f_gain", [128, D], F32)
            tmpb = [sb3(f"f_tb{i}", [128, D], BF16) for i in range(2)]
            junk = sb3("f_junk", [128, D], F32)
            ssb = [sb3(f"f_ss{i}", [128, 4], F32) for i in range(2)]
            P.dma("sp", gain[:, :], io["ffn_gain_bc"], gain, writes=[gain])
            if moe:
                hf = [sb3(f"f_hf{i}", [128, D], F32) for i in range(2)]
                hTf = [sb3(f"f_hTf{i}", [128, 8, 128], F32) for i in range(2)]
                rw = sb3("f_rw", [128, 8, 8], F32)
                lg = [sb3(f"f_lg{i}", [128, 40], F32) for i in range(2)]
                P.dma("sp", rw[:, :, :], io["router"].rearrange("(k p) e -> p k e", p=128), rw, writes=[rw])
            for t in range(NT):
                af = (hf[t % 2], hTf[t % 2], pbk[2 + (t % 2)]) if moe else None
                emit_norm_T(P, C, xl[t], gain, h2T, t, pbk[t % 2], tmpb[t % 2], junk, ssb[t % 2], also_f32=af)
                if moe:
                    hTf_, g_, pl = hTf[t % 2], lg[t % 2], pbk[4 + (t % 2)]
                    P.op("pe", [I("matmul", pl[:, 0:8], hTf_[:, kc, :], rw[:, kc, :], start=(kc == 0), stop=(kc == 7)) for kc in range(8)],
                         reads=[hTf_, rw], writes=[pl])
                    P.op("dve", I("tensor_copy", g_[:, 0:8], pl[:, 0:8]), reads=[pl], writes=[g_])
                    P.op("dve", I("tensor_reduce", g_[:, 8:9], g_[:, 0:8], AX.X, ALU.max), reads=[g_], writes=[g_])
                    P.op("dve", I("tensor_scalar", g_[:, 9:17], g_[:, 0:8], g_[:, 8:9], None, ALU.is_equal), reads=[g_], writes=[g_])
                    P.op("dve", I("scalar_tensor_tensor", g_[:, 17:25], g_[:, 9:17], -1e30, g_[:, 0:8], ALU.mult, ALU.add), reads=[g_], writes=[g_])
                    P.op("dve", I("tensor_reduce", g_[:, 25:26], g_[:, 17:25], AX.X, ALU.max), reads=[g_], writes=[g_])
                    P.op("dve", I("tensor_scalar", g_[:, 26:34], g_[:, 17:25], g_[:, 25:26], None, ALU.is_equal), reads=[g_], writes=[g_])
                    P.op("dve", I("tensor_tensor", g_[:, 34:35], g_[:, 25:26], g_[:, 8:9], ALU.subtract), reads=[g_], writes=[g_])
                    P.op("act", I("activation", g_[:, 34:35], g_[:, 34:35], AF.Sigmoid), reads=[g_], writes=[g_])
                    P.op("dve", I("tensor_scalar", g_[:, 35:36], g_[:, 34:35], -1.0, 1.0, ALU.mult, ALU.add), reads=[g_], writes=[g_])
                    P.op("dve", I("tensor_scalar", g_[:, 9:17], g_[:, 9:17], g_[:, 35:36], None, ALU.mult), reads=[g_], writes=[g_])
                    P.op("dve", I("scalar_tensor_tensor", comb[:, t, :], g_[:, 26:34], g_[:, 34:35], g_[:, 9:17], ALU.mult, ALU.add),
                         reads=[g_], writes=[comb])
            P.barrier()
        with contextlib.ExitStack() as es4:
            sb4, _ = scope(P, es4)
            groups = []
            GMAX = 8 if NFC % 7 else 7
            f0 = 0
            while f0 < NFC:
                n = min(GMAX, NFC - f0)
                if NFC == 22:
                    n = 8 if f0 == 0 else 7
                groups.append((f0, n))
                f0 += n
            act = sb4("f_act", [128, GMAX, TOK], BF16)
            wgs = [sb4(f"f_wgs{i}", [128, 8, 128], F32) for i in range(2)]
            wus = [sb4(f"f_wus{i}", [128, 8, 128], F32) for i in range(2)]
            wgb = [sb4(f"f_wgb{i}", [128, 8, 128], BF16) for i in range(2)]
            wub = [sb4(f"f_wub{i}", [128, 8, 128], BF16) for i in range(2)]
            wds = [sb4(f"f_wds{i}", [128, D], F32) for i in range(2)]
            wdb = [sb4(f"f_wdb{i}", [128, D], BF16) for i in range(GMAX)]
            sil = [sb4(f"f_sil{i}", [128, 512], F32) for i in range(2)]
            ci = 0
            for e in range(E):
                wgv = io["wg"][e].rearrange("(k p) f -> p k f", p=128)
                wuv = io["wu"][e].rearrange("(k p) f -> p k f", p=128)
                for (g0, gn) in groups:
                    for j in range(gn):
                        fc = g0 + j
                        b = ci % 2
                        ci += 1
                        fsl = slice(fc * 128, (fc + 1) * 128)
                        P.dma("sp", wgs[b][:, :, :], wgv[:, :, fsl], wgs[b], writes=[wgs[b]])
                        P.dma("pool", wus[b][:, :, :], wuv[:, :, fsl], wus[b], writes=[wus[b]])
                        P.dma("sp", wds[b][:, :], io["wd"][e, fsl, :], wds[b], writes=[wds[b]])
                        P.op("pool", I("tensor_copy", wgb[b][:, :, :], wgs[b][:, :, :]), reads=[wgs[b]], writes=[wgb[b]])
                        P.op("pool", I("tensor_copy", wub[b][:, :, :], wus[b][:, :, :]), reads=[wus[b]], writes=[wub[b]])
                        P.op("pool", I("tensor_copy", wdb[j][:, :], wds[b][:, :]), reads=[wds[b]], writes=[wdb[j]])
                        for nt in range(4):
                            pg, pu = pbk[(nt % 2) * 2], pbk[(nt % 2) * 2 + 1]
                            nsl = slice(nt * 512, (nt + 1) * 512)
                            P.op("pe", [I("matmul", pg[:, :], wgb[b][:, kc, :], h2T[:, kc, nsl], start=(kc == 0), stop=(kc == 7)) for kc in range(8)],
                                 reads=[wgb[b], h2T], writes=[pg])
                            P.op("pe", [I("matmul", pu[:, :], wub[b][:, kc, :], h2T[:, kc, nsl], start=(kc == 0), stop=(kc == 7)) for kc in range(8)],
                                 reads=[wub[b], h2T], writes=[pu])
                            sl_ = sil[nt % 2]
                            P.op("act", I("activation", sl_[:, :], pg[:, :], AF.Silu), reads=[pg], writes=[sl_])
                            P.op("dve", I("tensor_tensor", act[:, j, nsl], pu[:, :], sl_[:, :], ALU.mult), reads=[pu, sl_], writes=[act])
                    for t in range(NT):
                        tsl = slice(t * 128, (t + 1) * 128)
                        xs = xl[t]
                        for dh in range(2):
                            pd = pbk[4 + ((t * 2 + dh) % 4)]
                            dsl = slice(dh * 512, (dh + 1) * 512)
                            P.op("pe", [I("matmul", pd[:, :], act[:, j, tsl], wdb[j][:, dsl], start=(j == 0), stop=(j == gn - 1)) for j in range(gn)],
                                 reads=[act] + wdb[:gn], writes=[pd])
                            if moe:
                                P.op("dve", I("scalar_tensor_tensor", xs[:, dsl], pd[:, :], comb[:, t, e:e + 1], xs[:, dsl], ALU.mult, ALU.add),
                                     reads=[pd, comb, xs], writes=[xs])
                            else:
                                P.op("dve", I("tensor_tensor", xs[:, dsl], pd[:, :], xs[:, dsl], ALU.add), reads=[pd, xs], writes=[xs])
            P.barrier()
        ov = io["xout"].rearrange("(t p) d -> t p d", p=128)
        for t in range(NT):
            P.dma("sp" if t % 2 == 0 else "pool", ov[t], xl[t][:, :], xl[t], reads=[xl[t]], writes=[io["xout_buf"]], merge=True)
        if not cfg["last"]:
            with contextlib.ExitStack() as es5:
                sb5, _ = scope(P, es5)
                gain = sb5("f_ngain", [128, D], F32)
                tmpb = [sb5(f"f_ntb{i}", [128, D], BF16) for i in range(2)]
                junk = sb5("f_njunk", [128, D], F32)
                ssb = [sb5(f"f_nss{i}", [128, 4], F32) for i in range(2)]
                P.dma("sp", gain[:, :], io["mix_gain_bc"], gain, writes=[gain])
                for t in range(NT):
                    emit_norm_T(P, C, xl[t], gain, h2T, t, pbk[t % 2], tmpb[t % 2], junk, ssb[t % 2])
                store_hT(P, h2T, io)
                P.barrier()
        P.barrier()


S_LEN = 8192
NG = 16
EPS = 1e-6
NFM = 1664
NTM = 130
NV = 32
SCALE = 192 ** -0.5
O_CQ, O_CKV, O_KR, O_LX, O_LG, O_GQ, O_GK, O_GV, O_BR = 0, 512, 768, 896, 1024, 1152, 1280, 1408, 1536


def phase_M(P, C, io, ngroups=NG, stages=('att', 'lru', 'gdn'), gstop=99):
    nc = P.nc
    ident_f, ones_f, ones_b = C["ident_f"], C["ones_f"], C["ones_b"]
    with contextlib.ExitStack() as es:
        sb, ps = scope(P, es)
        bank = [ps(f"m_ps{i}") for i in range(8)]
        rot = [0]

        def nb():
            b = bank[2 + rot[0] % 2]
            rot[0] += 1
            return b
        OPS, OB, LB = bank[5], bank[6], bank[7]
        Wfm = sb("m_Wfm", [128, 8, NFM], BF16)
        Wtm = sb("m_Wtm", [128, 8, NTM], BF16)
        Wuq = sb("m_Wuq", [128, 4, 256], BF16)
        Wukv = sb("m_Wukv", [128, 2, 256], BF16)
        wst = sb("m_wst", [128, 4, 256], F32)
        wst2 = sb("m_wst2", [128, 2, 256], F32)
        Wa = sb("m_Wa", [128, 128], BF16)
        Wx = sb("m_Wx", [128, 128], BF16)
        vec = sb("m_vec", [128, NV], F32)
        dv = sb("m_dv", [128, 16], F32)
        gq = sb("m_gq", [128, 8], F32)
        mc = sb("m_mc", [128, 7, 128], F32)
        ggbc = sb("m_ggbc", [128, 128], F32)
        KnT = sb("m_KnT", [128, S_LEN], BF16)
        KrT = sb("m_KrT", [128, S_LEN // 2], BF16)
        Vr = sb("m_V", [128, S_LEN // 128, 128], BF16)
        for kc in range(8):
            P.dma("pool", Wfm[:, kc, :], io["win_fm"][kc * 128:(kc + 1) * 128, :], Wfm, writes=[Wfm], merge=True)
            P.dma("pool", Wtm[:, kc, :], io["win_tm"][kc * 128:(kc + 1) * 128, :], Wtm, writes=[Wtm], merge=True)
        P.dma("pool", Wa[:, :], io["wa_bd"], Wa, writes=[Wa])
        P.dma("pool", Wx[:, :], io["wx_bd"], Wx, writes=[Wx])
        P.dma("sp", vec[:, :], io["vecs"], vec, writes=[vec])
        P.dma("sp", gq[:, 0:4], io["gq"], gq, writes=[gq])
        P.dma("sp", gq[:, 4:6], io["gkv"], gq, writes=[gq], merge=True)
        P.dma("sp", mc[:, :, :], io["mconst"].rearrange("p (a b) -> p a b", a=7), mc, writes=[mc])
        P.dma("sp", ggbc[:, :], io["gdn_gain_bc"], ggbc, writes=[ggbc])
        P.dma("sp", wst[:, :, :], io["wuq"].rearrange("(k p) n -> p k n", p=128), wst, writes=[wst])
        P.dma("sp", wst2[:, :, :], io["wukv"].rearrange("(k p) n -> p k n", p=128), wst2, writes=[wst2])
        for kc in range(4):
            P.op("pool", I("tensor_scalar", Wuq[:, kc, :], wst[:, kc, :], gq[:, kc:kc + 1], None, ALU.mult), reads=[wst, gq], writes=[Wuq])
        for kc in range(2):
            P.op("pool", I("tensor_scalar", Wukv[:, kc, :], wst2[:, kc, :], gq[:, 4 + kc:5 + kc], None, ALU.mult), reads=[wst2, gq], writes=[Wukv])
        U2, B2, MLs, MU, SU01, SI, CI = [mc[:, i, :] for i in range(7)]
        P.op("dve", I("tensor_scalar", dv[:, 0:1], vec[:, 0:1], SCALE, None, ALU.mult), reads=[vec], writes=[dv])
        P.op("dve", I("tensor_scalar", dv[:, 1:2], vec[:, 1:2], vec[:, 4:5], SCALE, ALU.mult, ALU.mult), reads=[vec, dv], writes=[dv])
        P.op("dve", I("tensor_scalar", dv[:, 2:3], vec[:, 3:4], vec[:, 4:5], None, ALU.mult), reads=[vec, dv], writes=[dv])
        P.op("act", I("activation", dv[:, 6:7], vec[:, 12:13], AF.Exp, scale=-1.0), reads=[vec, dv], writes=[dv])
        P.op("act", I("activation", dv[:, 6:7], dv[:, 6:7], AF.Ln, bias=1.0), reads=[dv], writes=[dv])
        P.op("dve", I("tensor_scalar", dv[:, 3:4], dv[:, 6:7], -8.0, None, ALU.mult), reads=[dv], writes=[dv])
        P.op("dve", I("tensor_scalar", dv[:, 4:5], dv[:, 6:7], -16.0, None, ALU.mult), reads=[dv], writes=[dv])
        P.op("act", I("activation", dv[:, 5:6], vec[:, 25:26], AF.Exp), reads=[vec, dv], writes=[dv])
        P.op("dve", I("tensor_scalar", dv[:, 5:6], dv[:, 5:6], -1.0, None, ALU.mult), reads=[dv], writes=[dv])
        hTg = [sb(f"m_hTg{i}", [128, 8, 512], BF16) for i in range(2)]
        posg = sb("m_posg", [128, 512], I32)
        ti = sb("m_ti", [128, 512], I32)
        f1 = sb("m_f1", [128, 512], F32)
        f2 = sb("m_f2", [128, 512], F32)
        tab = sb("m_tab", [128, 512], F32)
        cq_f = sb("m_cqf", [128, 4, 512], F32)
        ckv_f = sb("m_ckvf", [128, 2, 512], F32)
        cqn = sb("m_cqn", [128, 4, 512], BF16)
        ckvn = sb("m_ckvn", [128, 2, 512], BF16)
        sqb = [sb(f"m_sqb{i}", [128, 512], BF16) for i in range(2)]
        rstd = [sb(f"m_rstd{i}", [128, 512], F32) for i in range(2)]
        krf = sb("m_krf", [128, 512], F32)
        QnT = sb("m_QnT", [128, 512], BF16)
        QrT = sb("m_QrT", [128, 512], BF16)
        PT = [sb(f"m_PT{i}", [128, 512], BF16) for i in range(3)]
        yst = [sb(f"m_yst{i}", [128, 3, 512], BF16) for i in range(2)]
        ysv = [[Buf("m_ys%d%s" % (i, c), yst[i].t) for c in "ALG"] for i in range(2)]
        xpl = sb("m_xpl", [128, 515], F32)
        lgf = sb("m_lgf", [128, 512], F32)
        xc = sb("m_xc", [128, 512], F32)
        xcb = sb("m_xcb", [128, 512], BF16)
        lr = sb("m_lr", [128, 512], F32)
        li = sb("m_li", [128, 512], F32)
        la = sb("m_la", [128, 512], F32)
        lu = sb("m_lu", [128, 512], F32)
        lh = sb("m_lh", [128, 512], F32)
        hlast = sb("m_hlast", [128, 1], F32)
        xpq = sb("m_xpq", [128, 515], F32)
        xpk = sb("m_xpk", [128, 515], F32)
        xpv = sb("m_xpv", [128, 515], F32)
        gt1 = sb("m_gt1", [128, 512], F32)
        QT = sb("m_QT", [128, 512], F32)
        KT = sb("m_KT", [128, 512], F32)
        VT = sb("m_VT", [128, 512], F32)
        bbc = sb("m_bbc", [128, 512], F32)
        Sst = sb("m_S", [128, 128], F32)
        tl_names = ["sz", "gU", "gneg", "DmL", "DmU", "decLs", "decU", "decUs", "egbc", "KTb", "QDT", "X0", "Y0", "attnT",
                    "TtA", "TtB", "XA", "XB", "YA", "YB", "ke", "vb", "kbg", "u", "wT", "vnew", "t1", "t2", "junk"]
        T = {n: sb("m_t_" + n, [128, 128], F32) for n in tl_names}
        T2 = dict(T)
        for n_ in ("wT", "u", "QDT", "attnT", "ke", "sz"):
            T2[n_] = sb("m_t2_" + n_, [128, 128], F32)
        TT = [T, T2]
        gbs = [sb("m_gb%d" % i, [128, 16], F32) for i in range(2)]
        decbs = [sb("m_decb%d" % i, [128, 2], F32) for i in range(2)]
        for xp in (xpl, xpq, xpk, xpv):
            P.op("pool", I("memset", xp[:, 0:3], 0.0), writes=[xp])
        P.op("pool", I("memset", hlast[:, :], 0.0), writes=[hlast])
        P.op("pool", I("memset", Sst[:, :], 0.0), writes=[Sst])

        def rsq(dst, src_ps, n, reads):
            P.op("act", I("activation", dst, src_ps, AF.Ln, bias=EPS, scale=1.0 / n), reads=reads[0], writes=reads[1])
            P.op("act", I("activation", dst, dst, AF.Exp, scale=-0.5), reads=reads[1], writes=reads[1])

        for g in range(ngroups):
            h = hTg[g % 2]
            gsl = slice(g * 512, (g + 1) * 512)
            if "hall" in io:
                P.dma("sp", h[:, :, :], io["hall"][g % 4][(g // 4) * 1024:(g // 4 + 1) * 1024, :].rearrange("(k p) n -> p k n", p=128), h,
                      reads=[io["hT_buf"]], writes=[h])
            else:
                P.dma("sp", h[:, :, :], io["hT_all"][g // 4].rearrange("(k p) n -> p k n", p=128)[:, :, (g % 4) * 512:(g % 4 + 1) * 512], h,
                      reads=[io["hT_buf"]], writes=[h])
            P.dma("act", posg[:, :], io["posb"][:, gsl], posg, writes=[posg])
            ys = yst[g % 2]
            ysA, ysL, ysG = ysv[g % 2]
            P.op("dve", I("tensor_copy", f1[:, :], posg[:, :]), reads=[posg], writes=[f1])
            P.op("dve", I("tensor_scalar", f1[:, :], f1[:, :], vec[:, 27:28], None, ALU.mult), reads=[f1, vec], writes=[f1])
            P.op("dve", I("tensor_scalar", f1[:, :], f1[:, :], 1.0 / (2 * math.pi), vec[:, 28:29], ALU.mult, ALU.add), reads=[f1, vec], writes=[f1])
            P.op("dve", I("tensor_copy", ti[:, :], f1[:, :]), reads=[f1], writes=[ti])
            P.op("dve", I("tensor_copy", f2[:, :], ti[:, :]), reads=[ti], writes=[f2])
            P.op("dve", I("tensor_tensor", f1[:, :], f1[:, :], f2[:, :], ALU.subtract), reads=[f1, f2], writes=[f1])
            P.op("dve", I("scalar_tensor_tensor", f2[:, :], f1[:, :], 0.5, f1[:, :], ALU.is_gt, ALU.subtract), reads=[f1], writes=[f2])
            P.op("dve", I("scalar_tensor_tensor", f1[:, :], f2[:, :], 0.5, f2[:, :], ALU.is_gt, ALU.subtract), reads=[f2], writes=[f1])
            P.op("act", I("activation", tab[:, :], f1[:, :], AF.Sin, scale=6.2831845), reads=[f1], writes=[tab])
            ssq, sskv = bank[3], bank[4]

            def proj(off):
                pb_ = bank[proj.i % 3]
                proj.i += 1
                P.op("pe", [I("matmul", pb_[:, :], Wfm[:, kc, off:off + 128], h[:, kc, :], start=(kc == 0), stop=(kc == 7)) for kc in range(8)],
                     reads=[Wfm, h], writes=[pb_])
                return pb_
            proj.i = 0
            for m in range(4):
                pb_ = proj(O_CQ + m * 128)
                s_ = sqb[m % 2]
                P.op("act", I("copy", cq_f[:, m, :], pb_[:, :]), reads=[pb_], writes=[cq_f])
                P.op("act", I("activation", s_[:, :], pb_[:, :], AF.Square), reads=[pb_], writes=[s_])
                P.op("pe", I("matmul", ssq[:, :], ones_b[:, :], s_[:, :], start=(m == 0), stop=(m == 3)), reads=[s_, ones_b], writes=[ssq])
            for m in range(2):
                pb_ = proj(O_CKV + m * 128)
                s_ = sqb[m % 2]
                P.op("act", I("copy", ckv_f[:, m, :], pb_[:, :]), reads=[pb_], writes=[ckv_f])
                P.op("act", I("activation", s_[:, :], pb_[:, :], AF.Square), reads=[pb_], writes=[s_])
                P.op("pe", I("matmul", sskv[:, :], ones_b[:, :], s_[:, :], start=(m == 0), stop=(m == 1)), reads=[s_, ones_b], writes=[sskv])
            rsq(rstd[0][:, :], ssq[:, :], 512, ([ssq], [rstd[0]]))
            rsq(rstd[1][:, :], sskv[:, :], 256, ([sskv], [rstd[1]]))
            for m in range(4):
                P.op("dve" if m % 2 == 0 else "pool", I("tensor_tensor", cqn[:, m, :], cq_f[:, m, :], rstd[0][:, :], ALU.mult), reads=[cq_f, rstd[0]], writes=[cqn])
            for m in range(2):
                P.op("dve" if m % 2 == 0 else "pool", I("tensor_tensor", ckvn[:, m, :], ckv_f[:, m, :], rstd[1][:, :], ALU.mult), reads=[ckv_f, rstd[1]], writes=[ckvn])
            pb_ = proj(O_KR)
            P.op("act", I("copy", krf[:, :], pb_[:, :]), reads=[pb_], writes=[krf])
            for off, dst in ((O_LX, xpl), (O_GQ, xpq), (O_GK, xpk), (O_GV, xpv)):
                pb_ = proj(off)
                P.op("act", I("copy", dst[:, 3:515], pb_[:, :]), reads=[pb_], writes=[dst])
            pb_ = proj(O_LG)
            P.op("act", I("copy", lgf[:, :], pb_[:, :]), reads=[pb_], writes=[lgf])
            pb_ = proj(O_BR)
            P.op("act", I("activation", bbc[:, :], pb_[:, :], AF.Sigmoid), reads=[pb_], writes=[bbc])
            qn_ps, qr_ps, ss_ps, ro_ps = bank[0], bank[1], bank[2], bank[3]
            P.op("pe", [I("matmul", qn_ps[:, :], Wuq[:, kc, 0:128], cqn[:, kc, :], start=(kc == 0), stop=(kc == 3)) for kc in range(4)], reads=[Wuq, cqn], writes=[qn_ps])
            P.op("pe", [I("matmul", qr_ps[:, :], Wuq[:, kc, 128:256], cqn[:, kc, :], start=(kc == 0), stop=(kc == 3)) for kc in range(4)], reads=[Wuq, cqn], writes=[qr_ps])
            P.op("act", I("activation", sqb[0][:, :], qn_ps[:, :], AF.Square), reads=[qn_ps], writes=[sqb[0]])
            P.op("act", I("activation", sqb[1][0:64, :], qr_ps[0:64, :], AF.Square), reads=[qr_ps], writes=[sqb[1]])
            P.op("pe", [I("matmul", ss_ps[:, :], ones_b[:, :], sqb[0][:, :], start=True, stop=False),
                        I("matmul", ss_ps[:, :], ones_b[0:64, :], sqb[1][0:64, :], start=False, stop=True)], reads=[sqb[0], sqb[1], ones_b], writes=[ss_ps])
            rsq(rstd[0][:, :], ss_ps[:, :], 192, ([ss_ps], [rstd[0]]))
            P.op("dve", I("scalar_tensor_tensor", QnT[:, :], qn_ps[:, :], dv[:, 0:1], rstd[0][:, :], ALU.mult, ALU.mult), reads=[qn_ps, dv, rstd[0]], writes=[QnT])
            P.op("dve", I("scalar_tensor_tensor", f2[:, :], qr_ps[:, :], dv[:, 1:2], rstd[0][:, :], ALU.mult, ALU.mult), reads=[qr_ps, dv, rstd[0]], writes=[f2])
            P.op("pool", I("tensor_tensor", f2[:, :], f2[:, :], tab[:, :], ALU.mult), reads=[f2, tab], writes=[f2])
            P.op("pe", I("matmul", ro_ps[:, :], SI, f2[:, :], start=True, stop=True), reads=[mc, f2], writes=[ro_ps])
            P.op("act", I("copy", QrT[:, :], ro_ps[:, :]), reads=[ro_ps], writes=[QrT])
            kn_ps = bank[0]
            P.op("pe", [I("matmul", kn_ps[:, :], Wukv[:, kc, 0:128], ckvn[:, kc, :], start=(kc == 0), stop=(kc == 1)) for kc in range(2)], reads=[Wukv, ckvn], writes=[kn_ps])
            P.op("act", I("activation", sqb[0][:, :], kn_ps[:, :], AF.Square), reads=[kn_ps], writes=[sqb[0]])
            P.op("act", I("activation", sqb[1][0:64, :], krf[0:64, :], AF.Square), reads=[krf], writes=[sqb[1]])
            ss_ps = bank[2]
            P.op("pe", [I("matmul", ss_ps[:, :], ones_b[:, :], sqb[0][:, :], start=True, stop=False),
                        I("matmul", ss_ps[:, :], ones_b[0:64, :], sqb[1][0:64, :], start=False, stop=True)], reads=[sqb[0], sqb[1], ones_b], writes=[ss_ps])
            rsq(rstd[1][:, :], ss_ps[:, :], 192, ([ss_ps], [rstd[1]]))
            P.op("dve", I("scalar_tensor_tensor", KnT[:, gsl], kn_ps[:, :], vec[:, 2:3], rstd[1][:, :], ALU.mult, ALU.mult), reads=[kn_ps, vec, rstd[1]], writes=[KnT])
            P.op("dve", I("scalar_tensor_tensor", f2[:, :], krf[:, :], dv[:, 2:3], rstd[1][:, :], ALU.mult, ALU.mult), reads=[krf, dv, rstd[1]], writes=[f2])
            P.op("pool", I("tensor_tensor", f2[:, :], f2[:, :], tab[:, :], ALU.mult), reads=[f2, tab], writes=[f2])
            ro_ps = bank[3]
            P.op("pe", I("matmul", ro_ps[:, :], SI, f2[:, :], start=True, stop=True), reads=[mc, f2], writes=[ro_ps])
            kr_rows = slice(0, 64) if g < 8 else slice(64, 128)
            kr_cols = slice((g % 8) * 512, (g % 8 + 1) * 512)
            P.op("act", I("copy", KrT[kr_rows, kr_cols], ro_ps[kr_rows, :]), reads=[ro_ps], writes=[KrT])
            for tl in range(4):
                v_ps = bank[tl % 2]
                P.op("pe", [I("matmul", v_ps[:, 0:128], ckvn[:, kc, tl * 128:(tl + 1) * 128], Wukv[:, kc, 128:256], start=(kc == 0), stop=(kc == 1)) for kc in range(2)],
                     reads=[ckvn, Wukv], writes=[v_ps])
                P.op("act", I("copy", Vr[:, 4 * g + tl, :], v_ps[:, 0:128]), reads=[v_ps], writes=[Vr])
            P.capture()
            nkb = 4 * g + 4 if 'att' in stages else 0
            for kb in range(nkb):
                c = kb - 4 * g
                q0 = 128 * max(c, 0)
                st = bank[kb % 2]
                pt = PT[kb % 3]
                rr = slice(0, 64) if kb < 32 else slice(64, 128)
                kc0 = (kb % 32) * 128
                P.op("pe", [I("matmul", st[:, q0:512], KnT[:, kb * 128:(kb + 1) * 128], QnT[:, q0:512], start=True, stop=False),
                            I("matmul", st[:, q0:512], KrT[rr, kc0:kc0 + 128], QrT[rr, q0:512], start=False, stop=True)],
                     reads=[KnT, KrT, QnT, QrT], writes=[st])
                P.op("act", I("activation", pt[:, q0:512], st[:, q0:512], AF.Exp), reads=[st], writes=[pt])
                if c >= 0:
                    P.op("pool", I("memset", pt[64:128, q0:q0 + 64], 0.0), reads=[pt], writes=[pt])
                P.op("pe", [I("matmul", OB[:, q0:512], Vr[:, kb, :], pt[:, q0:512], start=(kb == 0), stop=(kb == nkb - 1)),
                            I("matmul", LB[:, q0:512], ones_b[:, :], pt[:, q0:512], start=(kb == 0), stop=(kb == nkb - 1))],
                     reads=[Vr, pt, ones_b], writes=[OB, LB])
            if 'att' in stages:
                P.op("dve", I("reciprocal", f2[:, :], LB[:, :]), reads=[LB], writes=[f2])
                P.op("dve", I("tensor_tensor", ys[:, 0, :], OB[:, :], f2[:, :], ALU.mult), reads=[OB, f2], writes=[ysA])
            else:
                P.op("dve", I("tensor_copy", ys[:, 0, :], QnT[:, :]), reads=[QnT], writes=[ysA])
                P.op("dve", I("tensor_copy", ys[:, 1, :], QrT[:, :]), reads=[QrT], writes=[ysA])
                P.op("dve", I("tensor_copy", ys[:, 2, :], KnT[:, gsl]), reads=[KnT], writes=[ysA])
            s_att = P.end_capture()
            P.capture()
            if 'lru' in stages:
                P.op("dve", I("tensor_scalar", xc[:, :], xpl[:, 0:512], vec[:, 6:7], vec[:, 5:6], ALU.mult, ALU.add), reads=[xpl, vec], writes=[xc])
                for k in range(1, 4):
                    P.op("dve", I("scalar_tensor_tensor", xc[:, :], xpl[:, k:k + 512], vec[:, 6 + k:7 + k], xc[:, :], ALU.mult, ALU.add), reads=[xpl, vec, xc], writes=[xc])
                P.op("pool", I("tensor_copy", xpl[:, 0:3], xpl[:, 512:515]), reads=[xpl], writes=[xpl])
                P.op("pool", I("tensor_copy", xcb[:, :], xc[:, :]), reads=[xc], writes=[xcb])
                r_ps, i_ps = bank[0], bank[1]
                P.op("pe", I("matmul", r_ps[:, :], Wa[:, :], xcb[:, :], start=True, stop=True), reads=[Wa, xcb], writes=[r_ps])
                P.op("pe", I("matmul", i_ps[:, :], Wx[:, :], xcb[:, :], start=True, stop=True), reads=[Wx, xcb], writes=[i_ps])
                P.op("act", I("activation", lr[:, :], r_ps[:, :], AF.Sigmoid, bias=vec[:, 10:11]), reads=[r_ps, vec], writes=[lr])
                P.op("act", I("activation", li[:, :], i_ps[:, :], AF.Sigmoid, bias=vec[:, 11:12]), reads=[i_ps, vec], writes=[li])
                P.op("act", I("activation", la[:, :], lr[:, :], AF.Exp, scale=dv[:, 3:4]), reads=[lr, dv], writes=[la])
                P.op("act", I("activation", lu[:, :], lr[:, :], AF.Exp, scale=dv[:, 4:5]), reads=[lr, dv], writes=[lu])
                P.op("dve", I("tensor_scalar", lu[:, :], lu[:, :], -1.0, 1.0, ALU.mult, ALU.add), reads=[lu], writes=[lu])
                P.op("act", I("activation", lu[:, :], lu[:, :], AF.Sqrt), reads=[lu], writes=[lu])
                P.op("pool", I("tensor_tensor", li[:, :], li[:, :], xc[:, :], ALU.mult), reads=[li, xc], writes=[li])
                P.op("pool", I("tensor_tensor", lu[:, :], lu[:, :], li[:, :], ALU.mult), reads=[lu, li], writes=[lu])
                P.op("dve", I("tensor_tensor_scan", lh[:, :], la[:, :], lu[:, :], hlast[:, 0:1], ALU.mult, ALU.add), reads=[la, lu, hlast], writes=[lh])
                P.op("dve", I("tensor_copy", hlast[:, 0:1], lh[:, 511:512]), reads=[lh], writes=[hlast])
                P.op("pool", I("tensor_tensor", lr[:, :], lgf[:, :], lgf[:, :], ALU.mult), reads=[lgf], writes=[lr])
                P.op("pool", I("tensor_scalar", lr[:, :], lr[:, :], 0.044715, 1.0, ALU.mult, ALU.add), reads=[lr], writes=[lr])
                P.op("pool", I("tensor_tensor", lr[:, :], lr[:, :], lgf[:, :], ALU.mult), reads=[lr, lgf], writes=[lr])
                P.op("act", I("activation", lr[:, :], lr[:, :], AF.Sigmoid, scale=1.5957691216), reads=[lr], writes=[lr])
                P.op("pool", I("tensor_tensor", lr[:, :], lr[:, :], lgf[:, :], ALU.mult), reads=[lr, lgf], writes=[lr])
                P.op("dve", I("tensor_tensor", ys[:, 1, :], lh[:, :], lr[:, :], ALU.mult), reads=[lh, lr], writes=[ysL])
            s_lru = P.end_capture()
            P.capture()
            if 'gdn' in stages:
                for xp, dst, w0 in ((xpq, QT, 13), (xpk, KT, 17), (xpv, VT, 21)):
                    P.op("pool", I("tensor_scalar", gt1[:, :], xp[:, 0:512], vec[:, w0:w0 + 1], None, ALU.mult), reads=[xp, vec], writes=[gt1])
                    for k in range(1, 4):
                        P.op("dve", I("scalar_tensor_tensor", gt1[:, :], xp[:, k:k + 512], vec[:, w0 + k:w0 + k + 1], gt1[:, :], ALU.mult, ALU.add), reads=[xp, vec, gt1], writes=[gt1])
                    P.op("pool", I("tensor_copy", xp[:, 0:3], xp[:, 512:515]), reads=[xp], writes=[xp])
                    P.op("act", I("activation", dst[:, :], gt1[:, :], AF.Silu), reads=[gt1], writes=[dst])
                for src, sc in ((QT, 128 ** -0.5), (KT, 1.0)):
                    P.op("act", I("activation", sqb[0][:, :], src[:, :], AF.Square), reads=[src], writes=[sqb[0]])
                    ss_ps = nb()
                    P.op("pe", I("matmul", ss_ps[:, :], ones_b[:, :], sqb[0][:, :], start=True, stop=True), reads=[sqb[0], ones_b], writes=[ss_ps])
                    rsq(rstd[0][:, :], ss_ps[:, :], 1.0, ([ss_ps], [rstd[0]]))
                    P.op("dve", I("scalar_tensor_tensor", src[:, :], src[:, :], sc, rstd[0][:, :], ALU.mult, ALU.mult), reads=[src, rstd[0]], writes=[src])
                s_gpre = P.end_capture()
                s_prep, s_chain = [], []
                for tl in range(4):
                    T_, gb, decb = TT[tl % 2], gbs[tl % 2], decbs[tl % 2]
                    P.capture()
                    tsl = slice(tl * 128, (tl + 1) * 128)
                    tm = nb()
                    P.op("pe", [I("matmul", tm[:, 0:NTM], h[:, kc, tsl], Wtm[:, kc, :], start=(kc == 0), stop=(kc == 7)) for kc in range(8)], reads=[h, Wtm], writes=[tm])
                    P.op("act", I("activation", T_["sz"][:, :], tm[:, 0:128], AF.Silu), reads=[tm], writes=[T_["sz"]])
                    P.op("act", I("activation", gb[:, 0:1], tm[:, 128:129], AF.Exp, bias=vec[:, 26:27]), reads=[tm, vec, gb], writes=[gb])
                    P.op("act", I("activation", gb[:, 1:2], gb[:, 0:1], AF.Ln, bias=1.0), reads=[gb], writes=[gb])
                    P.op("act", I("activation", gb[:, 3:4], tm[:, 129:130], AF.Exp, scale=-1.0), reads=[tm, gb], writes=[gb])
                    P.op("dve", I("tensor_scalar", gb[:, 3:4], gb[:, 3:4], 1.0, None, ALU.add), reads=[gb], writes=[gb])
                    P.op("dve", I("reciprocal", gb[:, 3:4], gb[:, 3:4]), reads=[gb], writes=[gb])
                    P.op("dve", I("tensor_tensor", gb[:, 2:3], gb[:, 1:2], dv[:, 5:6], ALU.mult), reads=[gb, dv], writes=[gb])
                    P.op("dve", I("tensor_scalar", T_["gU"][:, :], U2, gb[:, 2:3], None, ALU.mult), reads=[mc, gb], writes=[T_["gU"]])
                    P.op("dve", I("tensor_scalar", T_["gneg"][:, :], ones_f[:, :], gb[:, 2:3], -1.0, ALU.mult, ALU.mult), reads=[ones_f, gb], writes=[T_["gneg"]])
                    D_ps = nb()
                    P.op("pe", [I("matmul", D_ps[:, 0:128], T_["gU"][:, :], ones_f[:, :], start=True, stop=False),
                                I("matmul", D_ps[:, 0:128], T_["gneg"][:, :], U2, start=False, stop=True),
                                I("matmul", D_ps[:, 128:129], U2, gb[:, 2:3], start=True, stop=True),
                                I("matmul", D_ps[:, 129:130], B2, gb[:, 2:3], start=True, stop=True)],
                         reads=[T_["gU"], T_["gneg"], ones_f, mc, gb], writes=[D_ps])
                    G_ps = nb()
                    P.op("pe", [I("matmul", G_ps[:, 0:128], T_["gneg"][:, :], U2, start=True, stop=True),
                                I("matmul", G_ps[:, 128:130], T_["gneg"][:, :], CI[:, 0:2], start=True, stop=True)],
                         reads=[T_["gneg"], mc], writes=[G_ps])
                    P.op("dve", I("tensor_tensor", T_["DmL"][:, :], D_ps[:, 0:128], MLs, ALU.add), reads=[D_ps, mc], writes=[T_["DmL"]])
                    P.op("dve", I("scalar_tensor_tensor", T_["DmU"][:, :], D_ps[:, 0:128], -1.0, MU, ALU.mult, ALU.add), reads=[D_ps, mc], writes=[T_["DmU"]])
                    P.op("act", I("activation", T_["decLs"][:, :], T_["DmL"][:, :], AF.Exp), reads=[T_["DmL"]], writes=[T_["decLs"]])
                    P.op("act", I("activation", T_["decU"][:, :], T_["DmU"][:, :], AF.Exp), reads=[T_["DmU"]], writes=[T_["decU"]])
                    P.op("pool", I("tensor_tensor", T_["decUs"][:, :], T_["decU"][:, :], SU01, ALU.mult), reads=[T_["decU"], mc], writes=[T_["decUs"]])
                    P.op("act", I("copy", gb[:, 10:12], D_ps[:, 128:130]), reads=[D_ps, gb], writes=[gb])
                    P.op("act", I("activation", gb[:, 4:5], gb[:, 10:11], AF.Exp), reads=[gb], writes=[gb])
                    P.op("dve", I("tensor_tensor", gb[:, 6:7], gb[:, 11:12], gb[:, 10:11], ALU.subtract), reads=[gb], writes=[gb])
                    P.op("act", I("activation", gb[:, 5:6], gb[:, 6:7], AF.Exp), reads=[gb], writes=[gb])
                    P.op("dve", I("tensor_tensor", gb[:, 7:8], gb[:, 3:4], gb[:, 4:5], ALU.mult), reads=[gb], writes=[gb])
                    P.op("act", I("activation", T_["egbc"][:, :], G_ps[:, 0:128], AF.Exp, scale=-1.0), reads=[G_ps], writes=[T_["egbc"]])
                    P.op("act", I("activation", decb[:, 0:2], G_ps[:, 128:130], AF.Exp, scale=-1.0), reads=[G_ps], writes=[decb])
                    P.op("pool", I("tensor_tensor", T_["KTb"][:, :], KT[:, tsl], bbc[:, tsl], ALU.mult), reads=[KT, bbc], writes=[T_["KTb"]])
                    P.op("pool", I("tensor_tensor", T_["QDT"][:, :], QT[:, tsl], T_["egbc"][:, :], ALU.mult), reads=[QT, T_["egbc"]], writes=[T_["QDT"]])
                    X_ps, Y_ps = nb(), nb()
                    A_ps = X_ps
                    P.op("pe", I("matmul", X_ps[:, 0:128], KT[:, tsl], T_["KTb"][:, :], start=True, stop=True), reads=[KT, T_["KTb"]], writes=[X_ps])
                    P.op("pe", I("matmul", Y_ps[:, 0:128], T_["KTb"][:, :], KT[:, tsl], start=True, stop=True), reads=[KT, T_["KTb"]], writes=[Y_ps])
                    P.op("pe", I("matmul", A_ps[:, 128:256], KT[:, tsl], QT[:, tsl], start=True, stop=True), reads=[KT, QT], writes=[A_ps])
                    P.op("dve", I("tensor_tensor", T_["X0"][:, :], X_ps[:, 0:128], T_["decUs"][:, :], ALU.mult), reads=[X_ps, T_["decUs"]], writes=[T_["X0"]])
                    P.op("dve", I("tensor_tensor", T_["Y0"][:, :], Y_ps[:, 0:128], T_["decLs"][:, :], ALU.mult), reads=[Y_ps, T_["decLs"]], writes=[T_["Y0"]])
                    P.op("dve", I("tensor_tensor", T_["attnT"][:, :], A_ps[:, 128:256], T_["decU"][:, :], ALU.mult), reads=[A_ps, T_["decU"]], writes=[T_["attnT"]])
                    P.op("pool", I("tensor_tensor", T_["TtA"][:, :], ident_f[:, :], T_["X0"][:, :], ALU.subtract), reads=[ident_f, T_["X0"]], writes=[T_["TtA"]])
                    Xp, Yp, Tt = T_["X0"], T_["Y0"], T_["TtA"]
                    for n in range(1, 6):
                        Yn = T_["YA"] if n % 2 else T_["YB"]
                        Xn = T_["XA"] if n % 2 else T_["XB"]
                        Tn = T_["TtB"] if n % 2 else T_["TtA"]
                        yp_ = nb()
                        P.op("pe", I("matmul", yp_[:, 0:128], Xp[:, :], Yp[:, :], start=True, stop=True), reads=[Xp, Yp], writes=[yp_])
                        P.op("act", I("copy", Yn[:, :], yp_[:, 0:128]), reads=[yp_], writes=[Yn])
                        if n < 5:
                            xp_ = nb()
                            P.op("pe", I("matmul", xp_[:, 0:128], Yp[:, :], Xp[:, :], start=True, stop=True), reads=[Xp, Yp], writes=[xp_])
                            P.op("act", I("copy", Xn[:, :], xp_[:, 0:128]), reads=[xp_], writes=[Xn])
                        cp_ = nb()
                        P.op("pe", I("matmul", cp_[:, 0:128], Yn[:, :], Tt[:, :], start=True, stop=True), reads=[Yn, Tt], writes=[cp_])
                        P.op("dve", I("tensor_tensor", Tn[:, :], cp_[:, 0:128], Tt[:, :], ALU.add), reads=[cp_, Tt], writes=[Tn])
                        Xp, Yp, Tt = Xn, Yn, Tn
                    kt_ps, vt_ps = nb(), nb()
                    P.op("pe", I("transpose", kt_ps[:, 0:128], KT[:, tsl], ident_f[:, :]), reads=[KT, ident_f], writes=[kt_ps])
                    P.op("pe", I("transpose", vt_ps[:, 0:128], VT[:, tsl], ident_f[:, :]), reads=[VT, ident_f], writes=[vt_ps])
                    P.op("dve", I("tensor_scalar", T_["kbg"][:, :], kt_ps[:, 0:128], gb[:, 7:8], None, ALU.mult), reads=[kt_ps, gb], writes=[T_["kbg"]])
                    P.op("dve", I("tensor_scalar", T_["ke"][:, :], kt_ps[:, 0:128], gb[:, 5:6], None, ALU.mult), reads=[kt_ps, gb], writes=[T_["ke"]])
                    P.op("dve", I("tensor_scalar", T_["vb"][:, :], vt_ps[:, 0:128], gb[:, 3:4], None, ALU.mult), reads=[vt_ps, gb], writes=[T_["vb"]])
                    u_ps, w_ps = nb(), nb()
                    P.op("pe", I("matmul", u_ps[:, 0:128], Tt[:, :], T_["vb"][:, :], start=True, stop=True), reads=[Tt, T_["vb"]], writes=[u_ps])
                    P.op("pe", I("matmul", w_ps[:, 0:128], T_["kbg"][:, :], Tt[:, :], start=True, stop=True), reads=[Tt, T_["kbg"]], writes=[w_ps])
                    P.op("act", I("copy", T_["u"][:, :], u_ps[:, 0:128]), reads=[u_ps], writes=[T_["u"]])
                    P.op("act", I("copy", T_["wT"][:, :], w_ps[:, 0:128]), reads=[w_ps], writes=[T_["wT"]])
                    s_prep.append(P.end_capture())
                    P.capture()
                    for c in range(2):
                        cs = slice(64 * c, 64 * c + 64)
                        vn_ps = bank[4]
                        P.op("pe", I("matmul", vn_ps[cs, 0:128], T_["wT"][:, cs], Sst[:, :], start=True, stop=True), reads=[T_["wT"], Sst], writes=[vn_ps])
                        P.op("dve", I("tensor_tensor", T_["vnew"][cs, :], T_["u"][cs, :], vn_ps[cs, 0:128], ALU.subtract), reads=[T_["u"], vn_ps], writes=[T_["vnew"]])
                        P.op("pe", [I("matmul", OPS[cs, 0:128], T_["QDT"][:, cs], Sst[:, :], start=True, stop=False),
                                    I("matmul", OPS[cs, 0:128], T_["attnT"][cs, cs], T_["vnew"][cs, :], start=False, stop=True)],
                             reads=[T_["QDT"], Sst, T_["attnT"], T_["vnew"]], writes=[OPS])
                        s_ps = bank[4]
                        P.op("pe", I("matmul", s_ps[:, 128:256], T_["ke"][cs, :], T_["vnew"][cs, :], start=True, stop=True), reads=[T_["ke"], T_["vnew"]], writes=[s_ps])
                        P.op("dve", I("scalar_tensor_tensor", Sst[:, :], Sst[:, :], decb[:, c:c + 1], s_ps[:, 128:256], ALU.mult, ALU.add), reads=[Sst, decb, s_ps], writes=[Sst])
                    P.op("act", I("activation", T_["junk"][:, :], OPS[:, 0:128], AF.Square, accum_out=gb[:, 8:9]), reads=[OPS, gb], writes=[T_["junk"], gb])
                    P.op("act", I("activation", gb[:, 9:10], gb[:, 8:9], AF.Ln, bias=EPS, scale=1.0 / 128), reads=[gb], writes=[gb])
                    P.op("act", I("activation", gb[:, 9:10], gb[:, 9:10], AF.Exp, scale=-0.5), reads=[gb], writes=[gb])
                    P.op("dve", I("scalar_tensor_tensor", T_["t1"][:, :], OPS[:, 0:128], gb[:, 9:10], ggbc[:, :], ALU.mult, ALU.mult), reads=[OPS, gb, ggbc], writes=[T_["t1"]])
                    P.op("pool", I("tensor_tensor", T_["t2"][:, :], T_["t1"][:, :], T_["sz"][:, :], ALU.mult), reads=[T_["t1"], T_["sz"]], writes=[T_["t2"]])
                    o_ps = bank[4]
                    P.op("pe", I("transpose", o_ps[:, 256:384], T_["t2"][:, :], ident_f[:, :]), reads=[T_["t2"], ident_f], writes=[o_ps])
                    P.op("act", I("copy", ys[:, 2, tsl], o_ps[:, 256:384]), reads=[o_ps], writes=[ysG])
                    s_chain.append(P.end_capture())
            if 'gdn' in stages:
                s_gdn = s_gpre + s_prep[0]
                for tl in range(4):
                    s_gdn = s_gdn + (merge_streams([s_chain[tl], s_prep[tl + 1]]) if tl < 3 else s_chain[tl])
            else:
                s_gdn = P.end_capture()
            P.replay(merge_streams([s_lru + s_att, s_gdn]))
            P.dma("sp", io["yT_out"][g // 4][:, :, (g % 4) * 512:(g % 4 + 1) * 512].rearrange("b f n -> f b n"), ys[:, :, :], ysA,
                  reads=[ysA, ysL, ysG], writes=[io["yT_buf"]], merge=True)
            if "y_ag" in io and g % 4 == 3:
                io["y_ag"](g // 4)
        P.barrier()


def consts_FN():
    return np.concatenate([np.eye(128, dtype=np.float32), np.ones((128, 128), np.float32)], 1)

def mconst():
    k = np.arange(128)[:, None]; i = np.arange(128)[None, :]
    same = (k // 64) == (i // 64)
    U2 = (same & (k <= i)).astype(np.float32)
    B2 = same.astype(np.float32)
    MLs = np.where(same & (k > i), 0.0, -3e4).astype(np.float32)
    MU = np.where(same & (i >= k), 0.0, -3e4).astype(np.float32)
    SU01 = (same & (i > k)).astype(np.float32)
    SI = ((k % 64) == (i % 64)).astype(np.float32)
    CI = np.zeros((128, 128), np.float32); CI[:64, 0] = 1; CI[64:, 1] = 1
    return np.ascontiguousarray(np.concatenate([U2, B2, MLs, MU, SU01, SI, CI], 1))

def prep_M(inp, L, b, j):
    w = inp['w_in'][L]
    kr = w[:, 768:832]
    krp = np.concatenate([kr[:, 32:64], kr[:, 0:32]], 1)
    cols = [w[:, 0:512], w[:, 512:768], kr, krp, w[:, 832 + 128 * j:832 + 128 * (j + 1)], w[:, 1344 + 128 * j:1344 + 128 * (j + 1)],
            w[:, 1856 + 128 * j:1856 + 128 * (j + 1)], w[:, 2368 + 128 * j:2368 + 128 * (j + 1)], w[:, 2880 + 128 * j:2880 + 128 * (j + 1)],
            np.repeat(w[:, 3908 + j:3909 + j], 128, axis=1)]
    win_fm = np.ascontiguousarray(np.concatenate(cols, 1))
    win_tm = np.ascontiguousarray(np.concatenate([w[:, 3392 + 128 * j:3392 + 128 * (j + 1)], w[:, 3904 + j:3905 + j], w[:, 3908 + j:3909 + j]], 1))
    uq = inp['mla_w_uq'][L][:, j * 192:(j + 1) * 192]
    wuq = np.ascontiguousarray(np.concatenate([uq, uq[:, 160:192], uq[:, 128:160]], 1))
    wukv = np.ascontiguousarray(inp['mla_w_ukv'][L][:, j * 256:(j + 1) * 256])
    vec = np.zeros((128, 32), np.float32)
    qh = inp['mla_q_head_norm'][L]; kh = inp['mla_k_head_norm'][L]
    vec[:, 0] = qh[0:128]
    vec[:, 1] = np.concatenate([qh[128:192], qh[160:192], qh[128:160]])
    vec[:, 2] = kh[0:128]
    vec[:, 3] = np.concatenate([kh[128:192], kh[160:192], kh[128:160]])
    vec[:, 4] = np.concatenate([np.ones(64), -np.ones(32), np.ones(32)])
    cs = slice(128 * j, 128 * (j + 1))
    vec[:, 5] = inp['lru_conv_b'][L][cs]
    for k in range(4):
        vec[:, 6 + k] = inp['lru_conv_w'][L][k, cs]
    vec[:, 10] = inp['lru_b_a'][L][cs]; vec[:, 11] = inp['lru_b_x'][L][cs]; vec[:, 12] = inp['lru_lambda'][L][cs]
    gw = inp['gdn_conv_w'][L]
    for k in range(4):
        vec[:, 13 + k] = gw[k, 128 * j:128 * (j + 1)]
        vec[:, 17 + k] = gw[k, 512 + 128 * j:512 + 128 * (j + 1)]
        vec[:, 21 + k] = gw[k, 1024 + 128 * j:1024 + 128 * (j + 1)]
    vec[:, 25] = inp['gdn_a_log'][L][j]; vec[:, 26] = inp['gdn_dt_bias'][L][j]
    invf = (np.float32(10000.0) ** (-np.arange(0, 64, 2, dtype=np.float32) / np.float32(64))).astype(np.float32)
    vec[:, 27] = np.tile(invf, 4)
    vec[:, 28] = np.concatenate([np.full(64, 0.25), np.zeros(64)])
    wa = np.zeros((128, 128), np.float32); wx = np.zeros((128, 128), np.float32)
    for t in range(2):
        wa[64 * t:64 * (t + 1), 64 * t:64 * (t + 1)] = inp['lru_w_a'][L][2 * j + t]
        wx[64 * t:64 * (t + 1), 64 * t:64 * (t + 1)] = inp['lru_w_x'][L][2 * j + t]
    return dict(win_fm=win_fm, win_tm=win_tm, wuq=wuq, wukv=wukv, vecs=vec,
                gq=np.ascontiguousarray(inp['mla_q_norm'][L].reshape(4, 128).T), gkv=np.ascontiguousarray(inp['mla_kv_norm'][L].reshape(2, 128).T),
                wa_bd=wa, wx_bd=wx, gdn_gain_bc=np.ascontiguousarray(np.broadcast_to(inp['gdn_out_norm'][L], (128, 128))),
                posb=np.ascontiguousarray(np.broadcast_to(inp['positions'][b].astype(np.int32), (128, 8192))), mconst=mconst())

M_SHAPES = dict(win_fm=([1024, 1664], 'f'), win_tm=([1024, 130], 'f'), wuq=([512, 256], 'f'), wukv=([256, 256], 'f'), vecs=([128, 32], 'f'),
                gq=([128, 4], 'f'), gkv=([128, 2], 'f'), wa_bd=([128, 128], 'f'), wx_bd=([128, 128], 'f'), gdn_gain_bc=([128, 128], 'f'),
                posb=([128, 8192], 'i'), mconst=([128, 896], 'f'))


BF_NP = ml_dtypes.bfloat16
N_CORES = 8
DEPTH_L = 2
GROUPS = [[0, 1, 2, 3], [4, 5, 6, 7]]
SHARED_M = ("posb", "mconst")


def _din(nc, name, shape, dt=F32):
    return nc.dram_tensor(name, list(shape), dt, kind="ExternalInput").ap()


def _dout(nc, name, shape, dt=F32):
    return nc.dram_tensor(name, list(shape), dt, kind="ExternalOutput").ap()


def _dint(nc, name, shape, dt):
    return nc.dram_tensor(name, list(shape), dt, kind="Internal").ap()


def build_fused():
    nc = bass.Bass("TRN2", target_bir_lowering=False)
    P = Prog(nc)
    x_in = _din(nc, "xloc", [2048, 1024])
    x_out = _dout(nc, "xout", [2048, 1024])
    yidx = _din(nc, "yidx", [128, 12], I32)
    gains = [_din(nc, "mix_gain_bc_%d" % L, [128, 1024]) for L in range(DEPTH_L)]
    shared = {k: _din(nc, k, M_SHAPES[k][0], F32 if M_SHAPES[k][1] == 'f' else I32) for k in SHARED_M}
    hx = _dint(nc, "hx", [4, 1024, 512], BF16)
    hall = _dint(nc, "hall", [4, 4096, 512], BF16)
    ycin = _dint(nc, "ycin", [4, 3, 128, 2048], BF16)
    yall = _dint(nc, "yall", [12, 512, 2048], BF16)
    xmid = _dint(nc, "xmid", [2048, 1024], F32)
    bufs = {k: P.reg(Buf(k)) for k in ["xloc_buf", "yrecv_buf", "xout_buf", "hT_buf", "yT_buf"]}
    C = consts(P, _din(nc, "consts", [128, 256]))

    def gather_h():
        P.barrier()
        for k in range(4):
            P.allgather(hx[k], hall[k], GROUPS)
        P.barrier()

    io = dict(bufs)
    io.update(xloc=x_in, mix_gain_bc=gains[0], hx=hx)
    phase_N0(P, C, io)
    gather_h()
    for L in range(DEPTH_L):
        last = L == DEPTH_L - 1
        moe = L % 2 == 1
        io = dict(bufs)
        for k, (shp, t) in M_SHAPES.items():
            io[k] = shared[k] if k in SHARED_M else _din(nc, "%s_%d" % (k, L), shp, F32 if t == 'f' else I32)
        def y_ag(qq):
            waits = P._deps("pool", [bufs["yT_buf"]], [])
            for br in range(3):
                P.allgather(ycin[qq][br], yall[qq * 3 + br], GROUPS, waits=waits if br == 0 else ())
        io.update(hall=hall, yT_out=ycin, y_ag=y_ag)
        phase_M(P, C, io)
        P.barrier()
        E, FF = (8, 3584) if moe else (1, 2816)
        io = dict(bufs)
        io.update(xloc=x_in if L == 0 else xmid, xout=x_out if last else xmid, yall=yall.rearrange("e r n -> (e r) n"), yidx=yidx,
                  w_out_r=_din(nc, "w_out_r_%d" % L, [3, 4, 128, 1024]), g_mla=_din(nc, "g_mla_%d" % L, [128, 4]),
                  g_lru=_din(nc, "g_lru_%d" % L, [128, 4]), ffn_gain_bc=_din(nc, "ffn_gain_bc_%d" % L, [128, 1024]),
                  wg=_din(nc, "wg_%d" % L, [E, 1024, FF]), wu=_din(nc, "wu_%d" % L, [E, 1024, FF]), wd=_din(nc, "wd_%d" % L, [E, FF, 1024]))
        if moe:
            io["router"] = _din(nc, "router_%d" % L, [1024, 8])
        if not last:
            io.update(mix_gain_bc=gains[L + 1], hx=hx)
        phase_F(P, C, io, dict(moe=moe, n_exp=E, d_ff=FF, last=last))
        if not last:
            gather_h()
    P.barrier()
    P.emit()
    return nc


def _bc(v, n):
    return np.ascontiguousarray(np.broadcast_to(np.asarray(v, np.float32), (128, n)))


def kernel(**inputs):
    inp = {k: np.asarray(v) for k, v in inputs.items()}
    cores = list(range(N_CORES))
    cst = consts_FN()
    x = inp['x'].astype(np.float32, copy=False)
    ins = []
    for c in cores:
        b, q = divmod(c, 4)
        d = dict(xloc=np.ascontiguousarray(x[b, 2048 * q:2048 * (q + 1)]), consts=cst)
        f = np.arange(128, dtype=np.int32)[:, None]
        col = np.arange(12, dtype=np.int32)[None, :]
        d["yidx"] = np.ascontiguousarray((((q * 3 + col // 4) * 4 + col % 4) * 128 + f).astype(np.int32))
        for L in range(DEPTH_L):
            moe = L % 2 == 1
            dm = prep_M(inp, L, b, q)
            for k, v in dm.items():
                d[k if k in SHARED_M else "%s_%d" % (k, L)] = v
            d["mix_gain_bc_%d" % L] = _bc(inp['mix_norm'][L], 1024)
            d["w_out_r_%d" % L] = np.ascontiguousarray(inp['w_out'][L].reshape(3, 4, 128, 1024))
            d["g_mla_%d" % L] = np.ascontiguousarray(inp['mla_out_norm'][L].reshape(4, 128).T)
            d["g_lru_%d" % L] = np.ascontiguousarray(inp['lru_out_norm'][L].reshape(4, 128).T)
            d["ffn_gain_bc_%d" % L] = _bc(inp['ffn_norm'][L], 1024)
            if moe:
                d["wg_%d" % L] = inp['moe_w_gate'][L // 2]
                d["wu_%d" % L] = inp['moe_w_up'][L // 2]
                d["wd_%d" % L] = inp['moe_w_down'][L // 2]
                d["router_%d" % L] = inp['router_w'][L // 2]
            else:
                d["wg_%d" % L] = inp['dense_w_gate'][L // 2][None]
                d["wu_%d" % L] = inp['dense_w_up'][L // 2][None]
                d["wd_%d" % L] = inp['dense_w_down'][L // 2][None]
        ins.append(d)
    res = run_bass_kernel_spmd(build_fused(), ins, core_ids=cores)
    out = np.empty((2, 8192, 1024), np.float32)
    for c in cores:
        out[c // 4, 2048 * (c % 4):2048 * (c % 4 + 1)] = np.asarray(res.results[c]["xout"])
    return out
```

```python
import contextlib, math
import ml_dtypes
import numpy as np
import concourse.bass as bass
import concourse.mybir as mybir
from concourse.bass_utils import run_bass_kernel_spmd

F32 = mybir.dt.float32
BF16 = mybir.dt.bfloat16
I32 = mybir.dt.int32
ALU = mybir.AluOpType
AF = mybir.ActivationFunctionType
AX = mybir.AxisListType


def I(name, *args, **kw):
    return (name, args, kw)


class Buf:
    __slots__ = ("name", "w", "r", "sem", "cnt", "t")

    def __init__(self, name, t=None):
        self.name = name
        self.w = {}
        self.r = {}
        self.sem = None
        self.cnt = 0
        self.t = t

    def __getitem__(self, idx):
        return self.t[idx]


def merge_streams(streams):
    out = []
    idx = [0] * len(streams)
    n = [len(x) for x in streams]
    while True:
        best, bi = None, -1
        for i in range(len(streams)):
            if idx[i] < n[i]:
                f = (idx[i] + 0.5) / n[i]
                if best is None or f < best:
                    best, bi = f, i
        if bi < 0:
            return out
        out.append(streams[bi][idx[bi]])
        idx[bi] += 1


class Prog:
    ENG = ("pe", "dve", "act", "pool", "sp")

    def __init__(self, nc):
        self.nc = nc
        self.q = {e: [] for e in self.ENG}
        self.cnt = {e: 0 for e in self.ENG}
        self.sem = {e: nc.alloc_semaphore("prog_" + e) for e in self.ENG}
        self.waited = {e: {} for e in self.ENG}
        self.nsem = 5
        self.free_sems = []
        self.sem_bufs = []
        self.extra_sems = []
        self.all_bufs = []
        self.ccsem = None
        self._cap = None
        self.cccnt = 0
        self.uid = 0

    def sb(self, name, shape, dt):
        t = self.nc.alloc_sbuf_tensor(name, list(shape), dt)
        return self.reg(Buf(name, t))

    def ps(self, name, shape, dt=F32):
        t = self.nc.alloc_psum_tensor(name, list(shape), dt)
        return self.reg(Buf(name, t))

    def dram(self, name, shape, dt, kind="Internal"):
        t = self.nc.dram_tensor(name, list(shape), dt, kind=kind)
        return self.reg(Buf(name, t))

    def reg(self, b):
        self.all_bufs.append(b)
        return b

    def _bufsem(self, b):
        if b.sem is None:
            if self.free_sems:
                b.sem, b.cnt = self.free_sems.pop()
            else:
                b.sem = self.nc.alloc_semaphore("bs_%d" % self.nsem)
                self.nsem += 1
                b.cnt = 0
            self.sem_bufs.append(b)
        return b.sem

    def barrier(self):
        allw = [(self.sem[e], self.cnt[e]) for e in self.ENG if self.cnt[e] > 0]
        allw += [(b.sem, b.cnt) for b in self.sem_bufs if b.cnt > 0]
        allw += [(s, c) for s, c in self.free_sems if c > 0]
        allw += [(s, c) for s, c in self.extra_sems if c > 0]
        if self.ccsem is not None and self.cccnt > 0:
            allw.append((self.ccsem, self.cccnt))
        for e in self.ENG:
            wd = self.waited[e]
            wl = []
            for s, v in allw:
                if wd.get(s, 0) < v:
                    wd[s] = v
                    wl.append((s, v))
            self.q[e].append((wl, None, None, 0))
        for b in self.sem_bufs:
            self.free_sems.append((b.sem, b.cnt))
            b.sem = None
            b.w = {}
            b.r = {}
        self.sem_bufs = []
        for b in self.all_bufs:
            b.w = {}
            b.r = {}

    def _deps(self, eng, reads, writes):
        deps = {}

        def add(s, v):
            if deps.get(s, 0) < v:
                deps[s] = v
        for b in reads:
            for s, v in b.w.items():
                add(s, v)
        for b in writes:
            for s, v in b.w.items():
                add(s, v)
            for s, v in b.r.items():
                add(s, v)
        wl = []
        wd = self.waited[eng]
        for s, v in deps.items():
            if wd.get(s, 0) < v:
                wd[s] = v
                wl.append((s, v))
        return wl

    def capture(self):
        self._cap = []

    def end_capture(self):
        c, self._cap = self._cap, None
        return c

    def replay(self, items):
        for kind, a, kw in items:
            getattr(self, kind)(*a, **kw)

    def op(self, eng, ins, reads=(), writes=()):
        if self._cap is not None:
            self._cap.append(("op", (eng, ins, reads, writes), {}))
            return None
        if isinstance(ins, tuple):
            ins = [ins]
        e = self._eng(eng)

        def fn(ins=ins, e=e):
            r = None
            for name, args, kw in ins:
                r = getattr(e, name)(*args, **kw)
            return r
        waits = self._deps(eng, reads, writes)
        self.cnt[eng] += 1
        s, v = self.sem[eng], self.cnt[eng]
        self.q[eng].append((waits, fn, s, 1))
        for b in reads:
            if b.r.get(s, 0) < v:
                b.r[s] = v
        for b in writes:
            b.w = {s: v}
            b.r = {}
        return (s, v)

    def dma(self, eng, out, in_, owner, reads=(), writes=(), merge=False, **kw):
        if self._cap is not None:
            self._cap.append(("dma", (eng, out, in_, owner, reads, writes, merge), kw))
            return None
        waits = self._deps(eng, reads, writes if not merge else ())
        s = self._bufsem(owner)
        owner.cnt += 16
        v = owner.cnt
        e = self._eng(eng)

        def fn():
            return e.dma_start(out=out, in_=in_, **kw)
        self.q[eng].append((waits, fn, s, 16))
        for b in reads:
            if b.r.get(s, 0) < v:
                b.r[s] = v
        for b in writes:
            if merge:
                b.w[s] = v
            else:
                b.w = {s: v}
                b.r = {}
        return (s, v)

    def dma_fn(self, eng, fn, owner, reads=(), writes=(), merge=False):
        waits = self._deps(eng, reads, writes if not merge else ())
        s = self._bufsem(owner)
        owner.cnt += 16
        v = owner.cnt
        self.q[eng].append((waits, fn, s, 16))
        for b in reads:
            if b.r.get(s, 0) < v:
                b.r[s] = v
        for b in writes:
            if merge:
                b.w[s] = v
            else:
                b.w = {s: v}
                b.r = {}
        return (s, v)

    def allgather(self, in_ap, out_ap, groups, waits=()):
        nc = self.nc
        if self.ccsem is None:
            self.ccsem = nc.alloc_semaphore("prog_cc")
        self.cccnt += 1

        def fn():
            return nc.gpsimd.collective_compute("AllGather", ALU.bypass, replica_groups=groups, ins=[in_ap.opt()], outs=[out_ap.opt()])
        self.q["pool"].append((list(waits), fn, self.ccsem, 1))

    def wait_all(self, eng, bufs):
        waits = self._deps(eng, bufs, ())
        self.q[eng].append((waits, None, None, 0))

    def _eng(self, e):
        nc = self.nc
        return {"pe": nc.tensor, "dve": nc.vector, "act": nc.scalar, "pool": nc.gpsimd, "sp": nc.sync}[e]

    def emit(self):
        nc = self.nc
        with nc.Block() as block:
            def mk(ename):
                def body(engine):
                    for waits, fn, s, inc in self.q[ename]:
                        for ws, wv in waits:
                            engine.wait_ge(ws, wv)
                        if fn is not None:
                            ins = fn()
                            if s is not None:
                                ins.then_inc(s, inc)
                return body
            block.tensor(mk("pe"))
            block.vector(mk("dve"))
            block.scalar(mk("act"))
            block.gpsimd(mk("pool"))
            block.sync(mk("sp"))


D = 1024
NT = 16
TOK = 2048
EPS = 1e-6


def consts(P, cdram):
    nc = P.nc
    c = {}
    c["ident_f"] = P.sb("ident_f", [128, 128], F32)
    c["ident_b"] = P.sb("ident_b", [128, 128], BF16)
    c["ones_b"] = P.sb("ones_b", [128, 128], BF16)
    c["ones_f"] = P.sb("ones_f", [128, 128], F32)
    idf, idb, ob, of = c["ident_f"], c["ident_b"], c["ones_b"], c["ones_f"]
    P.dma("sp", idf[:, :], cdram[:, 0:128], idf, writes=[idf])
    P.dma("sp", of[:, :], cdram[:, 128:256], of, writes=[of])
    P.op("pool", I("tensor_copy", idb[:, :], idf[:, :]), reads=[idf], writes=[idb])
    P.op("pool", I("tensor_copy", ob[:, :], of[:, :]), reads=[of], writes=[ob])
    return c


def scope(P, es):
    nc = P.nc
    P.uid += 1
    sfx = "_u%d" % P.uid
    def sb(name, shape, dt):
        t = es.enter_context(nc.sbuf_tensor(name + sfx, list(shape), dt))
        return Buf(name, t)
    def ps(name):
        t = es.enter_context(nc.psum_tensor(name + sfx, [128, 512], F32))
        return Buf(name, t)
    return sb, ps


def emit_norm_T(P, C, xt, gain_bc, hT, t, ps_bank, tmpb, junk, ssb, also_f32=None):
    P.op("act", I("activation", junk[:, :], xt[:, :], AF.Square, accum_out=ssb[:, 0:1]), reads=[xt], writes=[junk, ssb])
    P.op("act", I("activation", ssb[:, 1:2], ssb[:, 0:1], AF.Ln, bias=EPS, scale=1.0 / D), reads=[ssb], writes=[ssb])
    P.op("act", I("activation", ssb[:, 2:3], ssb[:, 1:2], AF.Exp, scale=-0.5), reads=[ssb], writes=[ssb])
    if also_f32 is not None:
        hf, hTf, psf = also_f32
        P.op("dve", I("scalar_tensor_tensor", hf[:, :], xt[:, :], ssb[:, 2:3], gain_bc[:, :], ALU.mult, ALU.mult),
             reads=[xt, ssb, gain_bc], writes=[hf])
        P.op("pool", I("tensor_copy", tmpb[:, :], hf[:, :]), reads=[hf], writes=[tmpb])
        for half in range(2):
            P.op("pe", [I("transpose", psf[:, k * 128:(k + 1) * 128], hf[:, (half * 4 + k) * 128:(half * 4 + k + 1) * 128], C["ident_f"][:, :]) for k in range(4)],
                 reads=[hf, C["ident_f"]], writes=[psf])
            P.op("act", I("copy", hTf[:, half * 4:(half + 1) * 4, :], psf[:, :].rearrange("p (k n) -> p k n", k=4)), reads=[psf], writes=[hTf])
    else:
        P.op("dve", I("scalar_tensor_tensor", tmpb[:, :], xt[:, :], ssb[:, 2:3], gain_bc[:, :], ALU.mult, ALU.mult),
             reads=[xt, ssb, gain_bc], writes=[tmpb])
    pb = ps_bank[:, :].bitcast(BF16)
    P.op("pe", [I("transpose", pb[:, kc * 128:(kc + 1) * 128], tmpb[:, kc * 128:(kc + 1) * 128], C["ident_b"][:, :]) for kc in range(8)],
         reads=[tmpb, C["ident_b"]], writes=[ps_bank])
    P.op("act", I("copy", hT[:, :, t * 128:(t + 1) * 128], pb.rearrange("p (k n) -> p k n", k=8)), reads=[ps_bank], writes=[hT])


def store_hT(P, hT, io):
    if "hx" in io:
        hv = io["hx"].rearrange("b (k p) n -> p k b n", p=128)
        for k in range(8):
            P.dma("sp" if k % 2 == 0 else "pool", hv[:, k, :, :], hT[:, k, :].rearrange("p (b n) -> p b n", b=4), hT, reads=[hT], writes=[io["hT_buf"]], merge=True)
        return
    hv = io["hT_out"].rearrange("(k p) n -> p k n", p=128)
    for k in range(8):
        P.dma("sp" if k % 2 == 0 else "pool", hv[:, k, :], hT[:, k, :], hT, reads=[hT], writes=[io["hT_buf"]], merge=True)


def phase_N0(P, C, io):
    with contextlib.ExitStack() as es:
        sb, ps = scope(P, es)
        gain = sb("n0_gain", [128, D], F32)
        hT = sb("n0_hT", [128, 8, TOK], BF16)
        xts = [sb(f"n0_x{i}", [128, D], F32) for i in range(4)]
        tmpb = [sb(f"n0_tb{i}", [128, D], BF16) for i in range(2)]
        junk = sb("n0_junk", [128, D], F32)
        ssb = [sb(f"n0_ss{i}", [128, 4], F32) for i in range(2)]
        pbk = [ps(f"n0_ps{i}") for i in range(2)]
        P.dma("sp", gain[:, :], io["mix_gain_bc"], gain, writes=[gain])
        xv = io["xloc"].rearrange("(t p) d -> t p d", p=128)
        for t in range(NT):
            xt = xts[t % 4]
            P.dma("sp" if t % 2 == 0 else "pool", xt[:, :], xv[t], xt, reads=[io["xloc_buf"]], writes=[xt])
            emit_norm_T(P, C, xt, gain, hT, t, pbk[t % 2], tmpb[t % 2], junk, ssb[t % 2])
        store_hT(P, hT, io)
        P.barrier()


def phase_F(P, C, io, cfg):
    nc = P.nc
    moe = cfg["moe"]
    E = cfg["n_exp"]
    FF = cfg["d_ff"]
    NFC = FF // 128
    with contextlib.ExitStack() as es:
        sb, ps = scope(P, es)
        xl = [sb(f"f_xl{t}", [128, D], F32) for t in range(NT)]
        pbk = [ps(f"f_ps{i}") for i in range(8)]
        xv = io["xloc"].rearrange("(t p) d -> t p d", p=128)
        for t in range(NT):
            P.dma("sp" if t % 2 == 0 else "pool", xl[t][:, :], xv[t], xl[t], reads=[io["xloc_buf"]], writes=[xl[t]])
        with contextlib.ExitStack() as es2:
            sb2, _ = scope(P, es2)
            yT = [sb2(f"f_yT{s}", [128, 3, TOK], BF16) for s in range(4)]
            wo = [sb2(f"f_wo{j}", [128, D], BF16) for j in range(12)]
            wst = [sb2(f"f_wst{i}", [128, D], F32) for i in range(2)]
            gm = sb2("f_gm", [128, 8], F32)
            sq = [sb2(f"f_sq{i}", [128, 4, 2, 128], BF16) for i in range(2)]
            rs = [sb2(f"f_rs{i}", [128, 8], F32) for i in range(2)]
            P.dma("sp", gm[:, 0:4], io["g_mla"], gm, writes=[gm])
            P.dma("sp", gm[:, 4:8], io["g_lru"], gm, writes=[gm], merge=True)
            if "yall" in io:
                yidx = sb2("f_yidx", [128, 12], I32)
                P.dma("sp", yidx[:, :], io["yidx"], yidx, writes=[yidx])
                for s in range(4):
                    for br in range(3):
                        col = br * 4 + s

                        def gfn(s=s, br=br, col=col):
                            return nc.gpsimd.indirect_dma_start(out=yT[s][:, br, :], out_offset=None, in_=io["yall"],
                                                                in_offset=bass.IndirectOffsetOnAxis(ap=yidx[:, col:col + 1], axis=0))
                        P.dma_fn("pool", gfn, yT[s], reads=[yidx, io["yrecv_buf"]], writes=[yT[s]], merge=(br > 0))
            else:
                for s in range(4):
                    P.dma("sp" if s % 2 == 0 else "pool", yT[s][:, :, :], io["yrecv"][s].rearrange("b f n -> f b n"), yT[s],
                          reads=[io["yrecv_buf"]], writes=[yT[s]])
            for j in range(12):
                br, s = j // 4, j % 4
                st = wst[j % 2]
                P.dma("act", st[:, :], io["w_out_r"][br, s], st, writes=[st])
                if br < 2:
                    P.op("pool", I("tensor_scalar", wo[j][:, :], st[:, :], gm[:, br * 4 + s:br * 4 + s + 1], None, ALU.mult),
                         reads=[st, gm], writes=[wo[j]])
                else:
                    P.op("pool", I("tensor_copy", wo[j][:, :], st[:, :]), reads=[st], writes=[wo[j]])
            for t in range(NT):
                tsl = slice(t * 128, (t + 1) * 128)
                sqb = sq[t % 2]
                rsb = rs[t % 2]
                pss = pbk[6 + (t % 2)]
                for s in range(4):
                    P.op("act", I("activation", sqb[:, s, :, :], yT[s][:, 0:2, tsl], AF.Square), reads=[yT[s]], writes=[sqb])
                P.op("pe", [I("matmul", pss[:, 0:1], sqb[:, s, 0, :], C["ones_b"][:, 0:1], start=(s == 0), stop=(s == 3)) for s in range(4)]
                     + [I("matmul", pss[:, 1:2], sqb[:, s, 1, :], C["ones_b"][:, 0:1], start=(s == 0), stop=(s == 3)) for s in range(4)],
                     reads=[sqb, C["ones_b"]], writes=[pss])
                P.op("act", I("activation", rsb[:, 0:2], pss[:, 0:2], AF.Ln, bias=EPS, scale=1.0 / 512), reads=[pss], writes=[rsb])
                P.op("act", I("activation", rsb[:, 2:4], rsb[:, 0:2], AF.Exp, scale=-0.5), reads=[rsb], writes=[rsb])
                xs = xl[t]
                for br in range(3):
                    for dh in range(2):
                        pb_ = pbk[(br * 2 + dh) % 6]
                        dsl = slice(dh * 512, (dh + 1) * 512)
                        P.op("pe", [I("matmul", pb_[:, :], yT[s][:, br, tsl], wo[br * 4 + s][:, dsl], start=(s == 0), stop=(s == 3)) for s in range(4)],
                             reads=yT + wo[br * 4:br * 4 + 4], writes=[pb_])
                        if br < 2:
                            P.op("dve", I("scalar_tensor_tensor", xs[:, dsl], pb_[:, :], rsb[:, 2 + br:3 + br], xs[:, dsl], ALU.mult, ALU.add),
                                 reads=[pb_, rsb, xs], writes=[xs])
                        else:
                            P.op("dve", I("tensor_tensor", xs[:, dsl], pb_[:, :], xs[:, dsl], ALU.add), reads=[pb_, xs], writes=[xs])
            P.barrier()
        h2T = sb("f_h2T", [128, 8, TOK], BF16)
        comb = sb("f_comb", [128, NT, 8], F32) if moe else None
        with contextlib.ExitStack() as es3:
            sb3, _ = scope(P, es3)
            gain = sb3("f_gain", [128, D], F32)
            tmpb = [sb3(f"f_tb{i}", [128, D], BF16) for i in range(2)]
            junk = sb3("f_junk", [128, D], F32)
            ssb = [sb3(f"f_ss{i}", [128, 4], F32) for i in range(2)]
            P.dma("sp", gain[:, :], io["ffn_gain_bc"], gain, writes=[gain])
            if moe:
                hf = [sb3(f"f_hf{i}", [128, D], F32) for i in range(2)]
                hTf = [sb3(f"f_hTf{i}", [128, 8, 128], F32) for i in range(2)]
                rw = sb3("f_rw", [128, 8, 8], F32)
                lg = [sb3(f"f_lg{i}", [128, 40], F32) for i in range(2)]
                P.dma("sp", rw[:, :, :], io["router"].rearrange("(k p) e -> p k e", p=128), rw, writes=[rw])
            for t in range(NT):
                af = (hf[t % 2], hTf[t % 2], pbk[2 + (t % 2)]) if moe else None
                emit_norm_T(P, C, xl[t], gain, h2T, t, pbk[t % 2], tmpb[t % 2], junk, ssb[t % 2], also_f32=af)
                if moe:
                    hTf_, g_, pl = hTf[t % 2], lg[t % 2], pbk[4 + (t % 2)]
                    P.op("pe", [I("matmul", pl[:, 0:8], hTf_[:, kc, :], rw[:, kc, :], start=(kc == 0), stop=(kc == 7)) for kc in range(8)],
                         reads=[hTf_, rw], writes=[pl])
                    P.op("dve", I("tensor_copy", g_[:, 0:8], pl[:, 0:8]), reads=[pl], writes=[g_])
                    P.op("dve", I("tensor_reduce", g_[:, 8:9], g_[:, 0:8], AX.X, ALU.max), reads=[g_], writes=[g_])
                    P.op("dve", I("tensor_scalar", g_[:, 9:17], g_[:, 0:8], g_[:, 8:9], None, ALU.is_equal), reads=[g_], writes=[g_])
                    P.op("dve", I("scalar_tensor_tensor", g_[:, 17:25], g_[:, 9:17], -1e30, g_[:, 0:8], ALU.mult, ALU.add), reads=[g_], writes=[g_])
                    P.op("dve", I("tensor_reduce", g_[:, 25:26], g_[:, 17:25], AX.X, ALU.max), reads=[g_], writes=[g_])
                    P.op("dve", I("tensor_scalar", g_[:, 26:34], g_[:, 17:25], g_[:, 25:26], None, ALU.is_equal), reads=[g_], writes=[g_])
                    P.op("dve", I("tensor_tensor", g_[:, 34:35], g_[:, 25:26], g_[:, 8:9], ALU.subtract), reads=[g_], writes=[g_])
                    P.op("act", I("activation", g_[:, 34:35], g_[:, 34:35], AF.Sigmoid), reads=[g_], writes=[g_])
                    P.op("dve", I("tensor_scalar", g_[:, 35:36], g_[:, 34:35], -1.0, 1.0, ALU.mult, ALU.add), reads=[g_], writes=[g_])
                    P.op("dve", I("tensor_scalar", g_[:, 9:17], g_[:, 9:17], g_[:, 35:36], None, ALU.mult), reads=[g_], writes=[g_])
                    P.op("dve", I("scalar_tensor_tensor", comb[:, t, :], g_[:, 26:34], g_[:, 34:35], g_[:, 9:17], ALU.mult, ALU.add),
                         reads=[g_], writes=[comb])
            P.barrier()
        with contextlib.ExitStack() as es4:
            sb4, _ = scope(P, es4)
            groups = []
            GMAX = 8 if NFC % 7 else 7
            f0 = 0
            while f0 < NFC:
                n = min(GMAX, NFC - f0)
                if NFC == 22:
                    n = 8 if f0 == 0 else 7
                groups.append((f0, n))
                f0 += n
            act = sb4("f_act", [128, GMAX, TOK], BF16)
            wgs = [sb4(f"f_wgs{i}", [128, 8, 128], F32) for i in range(2)]
            wus = [sb4(f"f_wus{i}", [128, 8, 128], F32) for i in range(2)]
            wgb = [sb4(f"f_wgb{i}", [128, 8, 128], BF16) for i in range(2)]
            wub = [sb4(f"f_wub{i}", [128, 8, 128], BF16) for i in range(2)]
            wds = [sb4(f"f_wds{i}", [128, D], F32) for i in range(2)]
            wdb = [sb4(f"f_wdb{i}", [128, D], BF16) for i in range(GMAX)]
            sil = [sb4(f"f_sil{i}", [128, 512], F32) for i in range(2)]
            ci = 0
            for e in range(E):
                wgv = io["wg"][e].rearrange("(k p) f -> p k f", p=128)
                wuv = io["wu"][e].rearrange("(k p) f -> p k f", p=128)
                for (g0, gn) in groups:
                    for j in range(gn):
                        fc = g0 + j
                        b = ci % 2
                        ci += 1
                        fsl = slice(fc * 128, (fc + 1) * 128)
                        P.dma("sp", wgs[b][:, :, :], wgv[:, :, fsl], wgs[b], writes=[wgs[b]])
                        P.dma("pool", wus[b][:, :, :], wuv[:, :, fsl], wus[b], writes=[wus[b]])
                        P.dma("sp", wds[b][:, :], io["wd"][e, fsl, :], wds[b], writes=[wds[b]])
                        P.op("pool", I("tensor_copy", wgb[b][:, :, :], wgs[b][:, :, :]), reads=[wgs[b]], writes=[wgb[b]])
                        P.op("pool", I("tensor_copy", wub[b][:, :, :], wus[b][:, :, :]), reads=[wus[b]], writes=[wub[b]])
                        P.op("pool", I("tensor_copy", wdb[j][:, :], wds[b][:, :]), reads=[wds[b]], writes=[wdb[j]])
                        for nt in range(4):
                            pg, pu = pbk[(nt % 2) * 2], pbk[(nt % 2) * 2 + 1]
                            nsl = slice(nt * 512, (nt + 1) * 512)
                            P.op("pe", [I("matmul", pg[:, :], wgb[b][:, kc, :], h2T[:, kc, nsl], start=(kc == 0), stop=(kc == 7)) for kc in range(8)],
                                 reads=[wgb[b], h2T], writes=[pg])
                            P.op("pe", [I("matmul", pu[:, :], wub[b][:, kc, :], h2T[:, kc, nsl], start=(kc == 0), stop=(kc == 7)) for kc in range(8)],
                                 reads=[wub[b], h2T], writes=[pu])
                            sl_ = sil[nt % 2]
                            P.op("act", I("activation", sl_[:, :], pg[:, :], AF.Silu), reads=[pg], writes=[sl_])
                            P.op("dve", I("tensor_tensor", act[:, j, nsl], pu[:, :], sl_[:, :], ALU.mult), reads=[pu, sl_], writes=[act])
                    for t in range(NT):
                        tsl = slice(t * 128, (t + 1) * 128)
                        xs = xl[t]
                        for dh in range(2):
                            pd = pbk[4 + ((t * 2 + dh) % 4)]
                            dsl = slice(dh * 512, (dh + 1) * 512)
                            P.op("pe", [I("matmul", pd[:, :], act[:, j, tsl], wdb[j][:, dsl], start=(j == 0), stop=(j == gn - 1)) for j in range(gn)],
                                 reads=[act] + wdb[:gn], writes=[pd])
                            if moe:
                                P.op("dve", I("scalar_tensor_tensor", xs[:, dsl], pd[:, :], comb[:, t, e:e + 1], xs[:, dsl], ALU.mult, ALU.add),
                                     reads=[pd, comb, xs], writes=[xs])
                            else:
                                P.op("dve", I("tensor_tensor", xs[:, dsl], pd[:, :], xs[:, dsl], ALU.add), reads=[pd, xs], writes=[xs])
            P.barrier()
        ov = io["xout"].rearrange("(t p) d -> t p d", p=128)
        for t in range(NT):
            P.dma("sp" if t % 2 == 0 else "pool", ov[t], xl[t][:, :], xl[t], reads=[xl[t]], writes=[io["xout_buf"]], merge=True)
        if not cfg["last"]:
            with contextlib.ExitStack() as es5:
                sb5, _ = scope(P, es5)
                gain = sb5("f_ngain", [128, D], F32)
                tmpb = [sb5(f"f_ntb{i}", [128, D], BF16) for i in range(2)]
                junk = sb5("f_njunk", [128, D], F32)
                ssb = [sb5(f"f_nss{i}", [128, 4], F32) for i in range(2)]
                P.dma("sp", gain[:, :], io["mix_gain_bc"], gain, writes=[gain])
                for t in range(NT):
                    emit_norm_T(P, C, xl[t], gain, h2T, t, pbk[t % 2], tmpb[t % 2], junk, ssb[t % 2])
                store_hT(P, h2T, io)
                P.barrier()
        P.barrier()


S_LEN = 8192
NG = 16
EPS = 1e-6
NFM = 1664
NTM = 130
NV = 32
SCALE = 192 ** -0.5
O_CQ, O_CKV, O_KR, O_LX, O_LG, O_GQ, O_GK, O_GV, O_BR = 0, 512, 768, 896, 1024, 1152, 1280, 1408, 1536


def phase_M(P, C, io, ngroups=NG, stages=('att', 'lru', 'gdn'), gstop=99):
    nc = P.nc
    ident_f, ones_f, ones_b = C["ident_f"], C["ones_f"], C["ones_b"]
    with contextlib.ExitStack() as es:
        sb, ps = scope(P, es)
        bank = [ps(f"m_ps{i}") for i in range(8)]
        rot = [0]

        def nb():
            b = bank[2 + rot[0] % 2]
            rot[0] += 1
            return b
        OPS, OB, LB = bank[5], bank[6], bank[7]
        Wfm = sb("m_Wfm", [128, 8, NFM], BF16)
        Wtm = sb("m_Wtm", [128, 8, NTM], BF16)
        Wuq = sb("m_Wuq", [128, 4, 256], BF16)
        Wukv = sb("m_Wukv", [128, 2, 256], BF16)
        wst = sb("m_wst", [128, 4, 256], F32)
        wst2 = sb("m_wst2", [128, 2, 256], F32)
        Wa = sb("m_Wa", [128, 128], BF16)
        Wx = sb("m_Wx", [128, 128], BF16)
        vec = sb("m_vec", [128, NV], F32)
        dv = sb("m_dv", [128, 16], F32)
        gq = sb("m_gq", [128, 8], F32)
        mc = sb("m_mc", [128, 7, 128], F32)
        ggbc = sb("m_ggbc", [128, 128], F32)
        KnT = sb("m_KnT", [128, S_LEN], BF16)
        KrT = sb("m_KrT", [128, S_LEN // 2], BF16)
        Vr = sb("m_V", [128, S_LEN // 128, 128], BF16)
        for kc in range(8):
            P.dma("pool", Wfm[:, kc, :], io["win_fm"][kc * 128:(kc + 1) * 128, :], Wfm, writes=[Wfm], merge=True)
            P.dma("pool", Wtm[:, kc, :], io["win_tm"][kc * 128:(kc + 1) * 128, :], Wtm, writes=[Wtm], merge=True)
        P.dma("pool", Wa[:, :], io["wa_bd"], Wa, writes=[Wa])
        P.dma("pool", Wx[:, :], io["wx_bd"], Wx, writes=[Wx])
        P.dma("sp", vec[:, :], io["vecs"], vec, writes=[vec])
        P.dma("sp", gq[:, 0:4], io["gq"], gq, writes=[gq])
        P.dma("sp", gq[:, 4:6], io["gkv"], gq, writes=[gq], merge=True)
        P.dma("sp", mc[:, :, :], io["mconst"].rearrange("p (a b) -> p a b", a=7), mc, writes=[mc])
        P.dma("sp", ggbc[:, :], io["gdn_gain_bc"], ggbc, writes=[ggbc])
        P.dma("sp", wst[:, :, :], io["wuq"].rearrange("(k p) n -> p k n", p=128), wst, writes=[wst])
        P.dma("sp", wst2[:, :, :], io["wukv"].rearrange("(k p) n -> p k n", p=128), wst2, writes=[wst2])
        for kc in range(4):
            P.op("pool", I("tensor_scalar", Wuq[:, kc, :], wst[:, kc, :], gq[:, kc:kc + 1], None, ALU.mult), reads=[wst, gq], writes=[Wuq])
        for kc in range(2):
            P.op("pool", I("tensor_scalar", Wukv[:, kc, :], wst2[:, kc, :], gq[:, 4 + kc:5 + kc], None, ALU.mult), reads=[wst2, gq], writes=[Wukv])
        U2, B2, MLs, MU, SU01, SI, CI = [mc[:, i, :] for i in range(7)]
        P.op("dve", I("tensor_scalar", dv[:, 0:1], vec[:, 0:1], SCALE, None, ALU.mult), reads=[vec], writes=[dv])
        P.op("dve", I("tensor_scalar", dv[:, 1:2], vec[:, 1:2], vec[:, 4:5], SCALE, ALU.mult, ALU.mult), reads=[vec, dv], writes=[dv])
        P.op("dve", I("tensor_scalar", dv[:, 2:3], vec[:, 3:4], vec[:, 4:5], None, ALU.mult), reads=[vec, dv], writes=[dv])
        P.op("act", I("activation", dv[:, 6:7], vec[:, 12:13], AF.Exp, scale=-1.0), reads=[vec, dv], writes=[dv])
        P.op("act", I("activation", dv[:, 6:7], dv[:, 6:7], AF.Ln, bias=1.0), reads=[dv], writes=[dv])
        P.op("dve", I("tensor_scalar", dv[:, 3:4], dv[:, 6:7], -8.0, None, ALU.mult), reads=[dv], writes=[dv])
        P.op("dve", I("tensor_scalar", dv[:, 4:5], dv[:, 6:7], -16.0, None, ALU.mult), reads=[dv], writes=[dv])
        P.op("act", I("activation", dv[:, 5:6], vec[:, 25:26], AF.Exp), reads=[vec, dv], writes=[dv])
        P.op("dve", I("tensor_scalar", dv[:, 5:6], dv[:, 5:6], -1.0, None, ALU.mult), reads=[dv], writes=[dv])
        hTg = [sb(f"m_hTg{i}", [128, 8, 512], BF16) for i in range(2)]
        posg = sb("m_posg", [128, 512], I32)
        ti = sb("m_ti", [128, 512], I32)
        f1 = sb("m_f1", [128, 512], F32)
        f2 = sb("m_f2", [128, 512], F32)
        tab = sb("m_tab", [128, 512], F32)
        cq_f = sb("m_cqf", [128, 4, 512], F32)
        ckv_f = sb("m_ckvf", [128, 2, 512], F32)
        cqn = sb("m_cqn", [128, 4, 512], BF16)
        ckvn = sb("m_ckvn", [128, 2, 512], BF16)
        sqb = [sb(f"m_sqb{i}", [128, 512], BF16) for i in range(2)]
        rstd = [sb(f"m_rstd{i}", [128, 512], F32) for i in range(2)]
        krf = sb("m_krf", [128, 512], F32)
        QnT = sb("m_QnT", [128, 512], BF16)
        QrT = sb("m_QrT", [128, 512], BF16)
        PT = [sb(f"m_PT{i}", [128, 512], BF16) for i in range(3)]
        yst = [sb(f"m_yst{i}", [128, 3, 512], BF16) for i in range(2)]
        ysv = [[Buf("m_ys%d%s" % (i, c), yst[i].t) for c in "ALG"] for i in range(2)]
        xpl = sb("m_xpl", [128, 515], F32)
        lgf = sb("m_lgf", [128, 512], F32)
        xc = sb("m_xc", [128, 512], F32)
        xcb = sb("m_xcb", [128, 512], BF16)
        lr = sb("m_lr", [128, 512], F32)
        li = sb("m_li", [128, 512], F32)
        la = sb("m_la", [128, 512], F32)
        lu = sb("m_lu", [128, 512], F32)
        lh = sb("m_lh", [128, 512], F32)
        hlast = sb("m_hlast", [128, 1], F32)
        xpq = sb("m_xpq", [128, 515], F32)
        xpk = sb("m_xpk", [128, 515], F32)
        xpv = sb("m_xpv", [128, 515], F32)
        gt1 = sb("m_gt1", [128, 512], F32)
        QT = sb("m_QT", [128, 512], F32)
        KT = sb("m_KT", [128, 512], F32)
        VT = sb("m_VT", [128, 512], F32)
        bbc = sb("m_bbc", [128, 512], F32)
        Sst = sb("m_S", [128, 128], F32)
        tl_names = ["sz", "gU", "gneg", "DmL", "DmU", "decLs", "decU", "decUs", "egbc", "KTb", "QDT", "X0", "Y0", "attnT",
                    "TtA", "TtB", "XA", "XB", "YA", "YB", "ke", "vb", "kbg", "u", "wT", "vnew", "t1", "t2", "junk"]
        T = {n: sb("m_t_" + n, [128, 128], F32) for n in tl_names}
        T2 = dict(T)
        for n_ in ("wT", "u", "QDT", "attnT", "ke", "sz"):
            T2[n_] = sb("m_t2_" + n_, [128, 128], F32)
        TT = [T, T2]
        gbs = [sb("m_gb%d" % i, [128, 16], F32) for i in range(2)]
        decbs = [sb("m_decb%d" % i, [128, 2], F32) for i in range(2)]
        for xp in (xpl, xpq, xpk, xpv):
            P.op("pool", I("memset", xp[:, 0:3], 0.0), writes=[xp])
        P.op("pool", I("memset", hlast[:, :], 0.0), writes=[hlast])
        P.op("pool", I("memset", Sst[:, :], 0.0), writes=[Sst])

        def rsq(dst, src_ps, n, reads):
            P.op("act", I("activation", dst, src_ps, AF.Ln, bias=EPS, scale=1.0 / n), reads=reads[0], writes=reads[1])
            P.op("act", I("activation", dst, dst, AF.Exp, scale=-0.5), reads=reads[1], writes=reads[1])

        for g in range(ngroups):
            h = hTg[g % 2]
            gsl = slice(g * 512, (g + 1) * 512)
            if "h_wait" in io:
                P.q["sp"].append(([io["h_wait"]], None, None, 0))
            if "hall" in io:
                P.dma("sp", h[:, :, :], io["hall"][g % 4][(g // 4) * 1024:(g // 4 + 1) * 1024, :].rearrange("(k p) n -> p k n", p=128), h,
                      reads=[io["hT_buf"]], writes=[h])
            else:
                P.dma("sp", h[:, :, :], io["hT_all"][g // 4].rearrange("(k p) n -> p k n", p=128)[:, :, (g % 4) * 512:(g % 4 + 1) * 512], h,
                      reads=[io["hT_buf"]], writes=[h])
            P.dma("act", posg[:, :], io["posb"][:, gsl], posg, writes=[posg])
            ys = yst[g % 2]
            ysA, ysL, ysG = ysv[g % 2]
            P.op("dve", I("tensor_copy", f1[:, :], posg[:, :]), reads=[posg], writes=[f1])
            P.op("dve", I("tensor_scalar", f1[:, :], f1[:, :], vec[:, 27:28], None, ALU.mult), reads=[f1, vec], writes=[f1])
            P.op("dve", I("tensor_scalar", f1[:, :], f1[:, :], 1.0 / (2 * math.pi), vec[:, 28:29], ALU.mult, ALU.add), reads=[f1, vec], writes=[f1])
            P.op("dve", I("tensor_copy", ti[:, :], f1[:, :]), reads=[f1], writes=[ti])
            P.op("dve", I("tensor_copy", f2[:, :], ti[:, :]), reads=[ti], writes=[f2])
            P.op("dve", I("tensor_tensor", f1[:, :], f1[:, :], f2[:, :], ALU.subtract), reads=[f1, f2], writes=[f1])
            P.op("dve", I("scalar_tensor_tensor", f2[:, :], f1[:, :], 0.5, f1[:, :], ALU.is_gt, ALU.subtract), reads=[f1], writes=[f2])
            P.op("dve", I("scalar_tensor_tensor", f1[:, :], f2[:, :], 0.5, f2[:, :], ALU.is_gt, ALU.subtract), reads=[f2], writes=[f1])
            P.op("act", I("activation", tab[:, :], f1[:, :], AF.Sin, scale=6.2831845), reads=[f1], writes=[tab])
            ssq, sskv = bank[3], bank[4]

            def proj(off):
                pb_ = bank[proj.i % 3]
                proj.i += 1
                P.op("pe", [I("matmul", pb_[:, :], Wfm[:, kc, off:off + 128], h[:, kc, :], start=(kc == 0), stop=(kc == 7)) for kc in range(8)],
                     reads=[Wfm, h], writes=[pb_])
                return pb_
            proj.i = 0
            for m in range(4):
                pb_ = proj(O_CQ + m * 128)
                s_ = sqb[m % 2]
                P.op("act", I("copy", cq_f[:, m, :], pb_[:, :]), reads=[pb_], writes=[cq_f])
                P.op("act", I("activation", s_[:, :], pb_[:, :], AF.Square), reads=[pb_], writes=[s_])
                P.op("pe", I("matmul", ssq[:, :], ones_b[:, :], s_[:, :], start=(m == 0), stop=(m == 3)), reads=[s_, ones_b], writes=[ssq])
            for m in range(2):
                pb_ = proj(O_CKV + m * 128)
                s_ = sqb[m % 2]
                P.op("act", I("copy", ckv_f[:, m, :], pb_[:, :]), reads=[pb_], writes=[ckv_f])
                P.op("act", I("activation", s_[:, :], pb_[:, :], AF.Square), reads=[pb_], writes=[s_])
                P.op("pe", I("matmul", sskv[:, :], ones_b[:, :], s_[:, :], start=(m == 0), stop=(m == 1)), reads=[s_, ones_b], writes=[sskv])
            rsq(rstd[0][:, :], ssq[:, :], 512, ([ssq], [rstd[0]]))
            rsq(rstd[1][:, :], sskv[:, :], 256, ([sskv], [rstd[1]]))
            for m in range(4):
                P.op("dve" if m % 2 == 0 else "pool", I("tensor_tensor", cqn[:, m, :], cq_f[:, m, :], rstd[0][:, :], ALU.mult), reads=[cq_f, rstd[0]], writes=[cqn])
            for m in range(2):
                P.op("dve" if m % 2 == 0 else "pool", I("tensor_tensor", ckvn[:, m, :], ckv_f[:, m, :], rstd[1][:, :], ALU.mult), reads=[ckv_f, rstd[1]], writes=[ckvn])
            pb_ = proj(O_KR)
            P.op("act", I("copy", krf[:, :], pb_[:, :]), reads=[pb_], writes=[krf])
            for off, dst in ((O_LX, xpl), (O_GQ, xpq), (O_GK, xpk), (O_GV, xpv)):
                pb_ = proj(off)
                P.op("act", I("copy", dst[:, 3:515], pb_[:, :]), reads=[pb_], writes=[dst])
            pb_ = proj(O_LG)
            P.op("act", I("copy", lgf[:, :], pb_[:, :]), reads=[pb_], writes=[lgf])
            pb_ = proj(O_BR)
            P.op("act", I("activation", bbc[:, :], pb_[:, :], AF.Sigmoid), reads=[pb_], writes=[bbc])
            qn_ps, qr_ps, ss_ps, ro_ps = bank[0], bank[1], bank[2], bank[3]
            P.op("pe", [I("matmul", qn_ps[:, :], Wuq[:, kc, 0:128], cqn[:, kc, :], start=(kc == 0), stop=(kc == 3)) for kc in range(4)], reads=[Wuq, cqn], writes=[qn_ps])
            P.op("pe", [I("matmul", qr_ps[:, :], Wuq[:, kc, 128:256], cqn[:, kc, :], start=(kc == 0), stop=(kc == 3)) for kc in range(4)], reads=[Wuq, cqn], writes=[qr_ps])
            P.op("act", I("activation", sqb[0][:, :], qn_ps[:, :], AF.Square), reads=[qn_ps], writes=[sqb[0]])
            P.op("act", I("activation", sqb[1][0:64, :], qr_ps[0:64, :], AF.Square), reads=[qr_ps], writes=[sqb[1]])
            P.op("pe", [I("matmul", ss_ps[:, :], ones_b[:, :], sqb[0][:, :], start=True, stop=False),
                        I("matmul", ss_ps[:, :], ones_b[0:64, :], sqb[1][0:64, :], start=False, stop=True)], reads=[sqb[0], sqb[1], ones_b], writes=[ss_ps])
            rsq(rstd[0][:, :], ss_ps[:, :], 192, ([ss_ps], [rstd[0]]))
            P.op("dve", I("scalar_tensor_tensor", QnT[:, :], qn_ps[:, :], dv[:, 0:1], rstd[0][:, :], ALU.mult, ALU.mult), reads=[qn_ps, dv, rstd[0]], writes=[QnT])
            P.op("dve", I("scalar_tensor_tensor", f2[:, :], qr_ps[:, :], dv[:, 1:2], rstd[0][:, :], ALU.mult, ALU.mult), reads=[qr_ps, dv, rstd[0]], writes=[f2])
            P.op("pool", I("tensor_tensor", f2[:, :], f2[:, :], tab[:, :], ALU.mult), reads=[f2, tab], writes=[f2])
            P.op("pe", I("matmul", ro_ps[:, :], SI, f2[:, :], start=True, stop=True), reads=[mc, f2], writes=[ro_ps])
            P.op("act", I("copy", QrT[:, :], ro_ps[:, :]), reads=[ro_ps], writes=[QrT])
            kn_ps = bank[0]
            P.op("pe", [I("matmul", kn_ps[:, :], Wukv[:, kc, 0:128], ckvn[:, kc, :], start=(kc == 0), stop=(kc == 1)) for kc in range(2)], reads=[Wukv, ckvn], writes=[kn_ps])
            P.op("act", I("activation", sqb[0][:, :], kn_ps[:, :], AF.Square), reads=[kn_ps], writes=[sqb[0]])
            P.op("act", I("activation", sqb[1][0:64, :], krf[0:64, :], AF.Square), reads=[krf], writes=[sqb[1]])
            ss_ps = bank[2]
            P.op("pe", [I("matmul", ss_ps[:, :], ones_b[:, :], sqb[0][:, :], start=True, stop=False),
                        I("matmul", ss_ps[:, :], ones_b[0:64, :], sqb[1][0:64, :], start=False, stop=True)], reads=[sqb[0], sqb[1], ones_b], writes=[ss_ps])
            rsq(rstd[1][:, :], ss_ps[:, :], 192, ([ss_ps], [rstd[1]]))
            P.op("dve", I("scalar_tensor_tensor", KnT[:, gsl], kn_ps[:, :], vec[:, 2:3], rstd[1][:, :], ALU.mult, ALU.mult), reads=[kn_ps, vec, rstd[1]], writes=[KnT])
            P.op("dve", I("scalar_tensor_tensor", f2[:, :], krf[:, :], dv[:, 2:3], rstd[1][:, :], ALU.mult, ALU.mult), reads=[krf, dv, rstd[1]], writes=[f2])
            P.op("pool", I("tensor_tensor", f2[:, :], f2[:, :], tab[:, :], ALU.mult), reads=[f2, tab], writes=[f2])
            ro_ps = bank[3]
            P.op("pe", I("matmul", ro_ps[:, :], SI, f2[:, :], start=True, stop=True), reads=[mc, f2], writes=[ro_ps])
            kr_rows = slice(0, 64) if g < 8 else slice(64, 128)
            kr_cols = slice((g % 8) * 512, (g % 8 + 1) * 512)
            P.op("act", I("copy", KrT[kr_rows, kr_cols], ro_ps[kr_rows, :]), reads=[ro_ps], writes=[KrT])
            for tl in range(4):
                v_ps = bank[tl % 2]
                P.op("pe", [I("matmul", v_ps[:, 0:128], ckvn[:, kc, tl * 128:(tl + 1) * 128], Wukv[:, kc, 128:256], start=(kc == 0), stop=(kc == 1)) for kc in range(2)],
                     reads=[ckvn, Wukv], writes=[v_ps])
                P.op("act", I("copy", Vr[:, 4 * g + tl, :], v_ps[:, 0:128]), reads=[v_ps], writes=[Vr])
            P.capture()
            nkb = 4 * g + 4 if 'att' in stages else 0
            for kb in range(nkb):
                c = kb - 4 * g
                q0 = 128 * max(c, 0)
                st = bank[kb % 2]
                pt = PT[kb % 3]
                rr = slice(0, 64) if kb < 32 else slice(64, 128)
                kc0 = (kb % 32) * 128
                P.op("pe", [I("matmul", st[:, q0:512], KnT[:, kb * 128:(kb + 1) * 128], QnT[:, q0:512], start=True, stop=False),
                            I("matmul", st[:, q0:512], KrT[rr, kc0:kc0 + 128], QrT[rr, q0:512], start=False, stop=True)],
                     reads=[KnT, KrT, QnT, QrT], writes=[st])
                P.op("act", I("activation", pt[:, q0:512], st[:, q0:512], AF.Exp), reads=[st], writes=[pt])
                if c >= 0:
                    P.op("pool", I("memset", pt[64:128, q0:q0 + 64], 0.0), reads=[pt], writes=[pt])
                P.op("pe", [I("matmul", OB[:, q0:512], Vr[:, kb, :], pt[:, q0:512], start=(kb == 0), stop=(kb == nkb - 1)),
                            I("matmul", LB[:, q0:512], ones_b[:, :], pt[:, q0:512], start=(kb == 0), stop=(kb == nkb - 1))],
                     reads=[Vr, pt, ones_b], writes=[OB, LB])
            if 'att' in stages:
                P.op("dve", I("reciprocal", f2[:, :], LB[:, :]), reads=[LB], writes=[f2])
                P.op("dve", I("tensor_tensor", ys[:, 0, :], OB[:, :], f2[:, :], ALU.mult), reads=[OB, f2], writes=[ysA])
            else:
                P.op("dve", I("tensor_copy", ys[:, 0, :], QnT[:, :]), reads=[QnT], writes=[ysA])
                P.op("dve", I("tensor_copy", ys[:, 1, :], QrT[:, :]), reads=[QrT], writes=[ysA])
                P.op("dve", I("tensor_copy", ys[:, 2, :], KnT[:, gsl]), reads=[KnT], writes=[ysA])
            s_att = P.end_capture()
            P.capture()
            if 'lru' in stages:
                P.op("dve", I("tensor_scalar", xc[:, :], xpl[:, 0:512], vec[:, 6:7], vec[:, 5:6], ALU.mult, ALU.add), reads=[xpl, vec], writes=[xc])
                for k in range(1, 4):
                    P.op("dve", I("scalar_tensor_tensor", xc[:, :], xpl[:, k:k + 512], vec[:, 6 + k:7 + k], xc[:, :], ALU.mult, ALU.add), reads=[xpl, vec, xc], writes=[xc])
                P.op("pool", I("tensor_copy", xpl[:, 0:3], xpl[:, 512:515]), reads=[xpl], writes=[xpl])
                P.op("pool", I("tensor_copy", xcb[:, :], xc[:, :]), reads=[xc], writes=[xcb])
                r_ps, i_ps = bank[0], bank[1]
                P.op("pe", I("matmul", r_ps[:, :], Wa[:, :], xcb[:, :], start=True, stop=True), reads=[Wa, xcb], writes=[r_ps])
                P.op("pe", I("matmul", i_ps[:, :], Wx[:, :], xcb[:, :], start=True, stop=True), reads=[Wx, xcb], writes=[i_ps])
                P.op("act", I("activation", lr[:, :], r_ps[:, :], AF.Sigmoid, bias=vec[:, 10:11]), reads=[r_ps, vec], writes=[lr])
                P.op("act", I("activation", li[:, :], i_ps[:, :], AF.Sigmoid, bias=vec[:, 11:12]), reads=[i_ps, vec], writes=[li])
                P.op("act", I("activation", la[:, :], lr[:, :], AF.Exp, scale=dv[:, 3:4]), reads=[lr, dv], writes=[la])
                P.op("act", I("activation", lu[:, :], lr[:, :], AF.Exp, scale=dv[:, 4:5]), reads=[lr, dv], writes=[lu])
                P.op("dve", I("tensor_scalar", lu[:, :], lu[:, :], -1.0, 1.0, ALU.mult, ALU.add), reads=[lu], writes=[lu])
                P.op("act", I("activation", lu[:, :], lu[:, :], AF.Sqrt), reads=[lu], writes=[lu])
                P.op("pool", I("tensor_tensor", li[:, :], li[:, :], xc[:, :], ALU.mult), reads=[li, xc], writes=[li])
                P.op("pool", I("tensor_tensor", lu[:, :], lu[:, :], li[:, :], ALU.mult), reads=[lu, li], writes=[lu])
                P.op("dve", I("tensor_tensor_scan", lh[:, :], la[:, :], lu[:, :], hlast[:, 0:1], ALU.mult, ALU.add), reads=[la, lu, hlast], writes=[lh])
                P.op("dve", I("tensor_copy", hlast[:, 0:1], lh[:, 511:512]), reads=[lh], writes=[hlast])
                P.op("pool", I("tensor_tensor", lr[:, :], lgf[:, :], lgf[:, :], ALU.mult), reads=[lgf], writes=[lr])
                P.op("pool", I("tensor_scalar", lr[:, :], lr[:, :], 0.044715, 1.0, ALU.mult, ALU.add), reads=[lr], writes=[lr])
                P.op("pool", I("tensor_tensor", lr[:, :], lr[:, :], lgf[:, :], ALU.mult), reads=[lr, lgf], writes=[lr])
                P.op("act", I("activation", lr[:, :], lr[:, :], AF.Sigmoid, scale=1.5957691216), reads=[lr], writes=[lr])
                P.op("pool", I("tensor_tensor", lr[:, :], lr[:, :], lgf[:, :], ALU.mult), reads=[lr, lgf], writes=[lr])
                P.op("dve", I("tensor_tensor", ys[:, 1, :], lh[:, :], lr[:, :], ALU.mult), reads=[lh, lr], writes=[ysL])
            s_lru = P.end_capture()
            P.capture()
            if 'gdn' in stages:
                for xp, dst, w0 in ((xpq, QT, 13), (xpk, KT, 17), (xpv, VT, 21)):
                    P.op("pool", I("tensor_scalar", gt1[:, :], xp[:, 0:512], vec[:, w0:w0 + 1], None, ALU.mult), reads=[xp, vec], writes=[gt1])
                    for k in range(1, 4):
                        P.op("dve", I("scalar_tensor_tensor", gt1[:, :], xp[:, k:k + 512], vec[:, w0 + k:w0 + k + 1], gt1[:, :], ALU.mult, ALU.add), reads=[xp, vec, gt1], writes=[gt1])
                    P.op("pool", I("tensor_copy", xp[:, 0:3], xp[:, 512:515]), reads=[xp], writes=[xp])
                    P.op("act", I("activation", dst[:, :], gt1[:, :], AF.Silu), reads=[gt1], writes=[dst])
                for src, sc in ((QT, 128 ** -0.5), (KT, 1.0)):
                    P.op("act", I("activation", sqb[0][:, :], src[:, :], AF.Square), reads=[src], writes=[sqb[0]])
                    ss_ps = nb()
                    P.op("pe", I("matmul", ss_ps[:, :], ones_b[:, :], sqb[0][:, :], start=True, stop=True), reads=[sqb[0], ones_b], writes=[ss_ps])
                    rsq(rstd[0][:, :], ss_ps[:, :], 1.0, ([ss_ps], [rstd[0]]))
                    P.op("dve", I("scalar_tensor_tensor", src[:, :], src[:, :], sc, rstd[0][:, :], ALU.mult, ALU.mult), reads=[src, rstd[0]], writes=[src])
                s_gpre = P.end_capture()
                s_prep, s_chain = [], []
                for tl in range(4):
                    T_, gb, decb = TT[tl % 2], gbs[tl % 2], decbs[tl % 2]
                    P.capture()
                    tsl = slice(tl * 128, (tl + 1) * 128)
                    tm = nb()
                    P.op("pe", [I("matmul", tm[:, 0:NTM], h[:, kc, tsl], Wtm[:, kc, :], start=(kc == 0), stop=(kc == 7)) for kc in range(8)], reads=[h, Wtm], writes=[tm])
                    P.op("act", I("activation", T_["sz"][:, :], tm[:, 0:128], AF.Silu), reads=[tm], writes=[T_["sz"]])
                    P.op("act", I("activation", gb[:, 0:1], tm[:, 128:129], AF.Exp, bias=vec[:, 26:27]), reads=[tm, vec, gb], writes=[gb])
                    P.op("act", I("activation", gb[:, 1:2], gb[:, 0:1], AF.Ln, bias=1.0), reads=[gb], writes=[gb])
                    P.op("act", I("activation", gb[:, 3:4], tm[:, 129:130], AF.Exp, scale=-1.0), reads=[tm, gb], writes=[gb])
                    P.op("dve", I("tensor_scalar", gb[:, 3:4], gb[:, 3:4], 1.0, None, ALU.add), reads=[gb], writes=[gb])
                    P.op("dve", I("reciprocal", gb[:, 3:4], gb[:, 3:4]), reads=[gb], writes=[gb])
                    P.op("dve", I("tensor_tensor", gb[:, 2:3], gb[:, 1:2], dv[:, 5:6], ALU.mult), reads=[gb, dv], writes=[gb])
                    P.op("dve", I("tensor_scalar", T_["gU"][:, :], U2, gb[:, 2:3], None, ALU.mult), reads=[mc, gb], writes=[T_["gU"]])
                    P.op("dve", I("tensor_scalar", T_["gneg"][:, :], ones_f[:, :], gb[:, 2:3], -1.0, ALU.mult, ALU.mult), reads=[ones_f, gb], writes=[T_["gneg"]])
                    D_ps = nb()
                    P.op("pe", [I("matmul", D_ps[:, 0:128], T_["gU"][:, :], ones_f[:, :], start=True, stop=False),
                                I("matmul", D_ps[:, 0:128], T_["gneg"][:, :], U2, start=False, stop=True),
                                I("matmul", D_ps[:, 128:129], U2, gb[:, 2:3], start=True, stop=True),
                                I("matmul", D_ps[:, 129:130], B2, gb[:, 2:3], start=True, stop=True)],
                         reads=[T_["gU"], T_["gneg"], ones_f, mc, gb], writes=[D_ps])
                    G_ps = nb()
                    P.op("pe", [I("matmul", G_ps[:, 0:128], T_["gneg"][:, :], U2, start=True, stop=True),
                                I("matmul", G_ps[:, 128:130], T_["gneg"][:, :], CI[:, 0:2], start=True, stop=True)],
                         reads=[T_["gneg"], mc], writes=[G_ps])
                    P.op("dve", I("tensor_tensor", T_["DmL"][:, :], D_ps[:, 0:128], MLs, ALU.add), reads=[D_ps, mc], writes=[T_["DmL"]])
                    P.op("dve", I("scalar_tensor_tensor", T_["DmU"][:, :], D_ps[:, 0:128], -1.0, MU, ALU.mult, ALU.add), reads=[D_ps, mc], writes=[T_["DmU"]])
                    P.op("act", I("activation", T_["decLs"][:, :], T_["DmL"][:, :], AF.Exp), reads=[T_["DmL"]], writes=[T_["decLs"]])
                    P.op("act", I("activation", T_["decU"][:, :], T_["DmU"][:, :], AF.Exp), reads=[T_["DmU"]], writes=[T_["decU"]])
                    P.op("pool", I("tensor_tensor", T_["decUs"][:, :], T_["decU"][:, :], SU01, ALU.mult), reads=[T_["decU"], mc], writes=[T_["decUs"]])
                    P.op("act", I("copy", gb[:, 10:12], D_ps[:, 128:130]), reads=[D_ps, gb], writes=[gb])
                    P.op("act", I("activation", gb[:, 4:5], gb[:, 10:11], AF.Exp), reads=[gb], writes=[gb])
                    P.op("dve", I("tensor_tensor", gb[:, 6:7], gb[:, 11:12], gb[:, 10:11], ALU.subtract), reads=[gb], writes=[gb])
                    P.op("act", I("activation", gb[:, 5:6], gb[:, 6:7], AF.Exp), reads=[gb], writes=[gb])
                    P.op("dve", I("tensor_tensor", gb[:, 7:8], gb[:, 3:4], gb[:, 4:5], ALU.mult), reads=[gb], writes=[gb])
                    P.op("act", I("activation", T_["egbc"][:, :], G_ps[:, 0:128], AF.Exp, scale=-1.0), reads=[G_ps], writes=[T_["egbc"]])
                    P.op("act", I("activation", decb[:, 0:2], G_ps[:, 128:130], AF.Exp, scale=-1.0), reads=[G_ps], writes=[decb])
                    P.op("pool", I("tensor_tensor", T_["KTb"][:, :], KT[:, tsl], bbc[:, tsl], ALU.mult), reads=[KT, bbc], writes=[T_["KTb"]])
                    P.op("pool", I("tensor_tensor", T_["QDT"][:, :], QT[:, tsl], T_["egbc"][:, :], ALU.mult), reads=[QT, T_["egbc"]], writes=[T_["QDT"]])
                    X_ps, Y_ps = nb(), nb()
                    A_ps = X_ps
                    P.op("pe", I("matmul", X_ps[:, 0:128], KT[:, tsl], T_["KTb"][:, :], start=True, stop=True), reads=[KT, T_["KTb"]], writes=[X_ps])
                    P.op("pe", I("matmul", Y_ps[:, 0:128], T_["KTb"][:, :], KT[:, tsl], start=True, stop=True), reads=[KT, T_["KTb"]], writes=[Y_ps])
                    P.op("pe", I("matmul", A_ps[:, 128:256], KT[:, tsl], QT[:, tsl], start=True, stop=True), reads=[KT, QT], writes=[A_ps])
                    P.op("dve", I("tensor_tensor", T_["X0"][:, :], X_ps[:, 0:128], T_["decUs"][:, :], ALU.mult), reads=[X_ps, T_["decUs"]], writes=[T_["X0"]])
                    P.op("dve", I("tensor_tensor", T_["Y0"][:, :], Y_ps[:, 0:128], T_["decLs"][:, :], ALU.mult), reads=[Y_ps, T_["decLs"]], writes=[T_["Y0"]])
                    P.op("dve", I("tensor_tensor", T_["attnT"][:, :], A_ps[:, 128:256], T_["decU"][:, :], ALU.mult), reads=[A_ps, T_["decU"]], writes=[T_["attnT"]])
                    P.op("pool", I("tensor_tensor", T_["TtA"][:, :], ident_f[:, :], T_["X0"][:, :], ALU.subtract), reads=[ident_f, T_["X0"]], writes=[T_["TtA"]])
                    Xp, Yp, Tt = T_["X0"], T_["Y0"], T_["TtA"]
                    for n in range(1, 6):
                        Yn = T_["YA"] if n % 2 else T_["YB"]
                        Xn = T_["XA"] if n % 2 else T_["XB"]
                        Tn = T_["TtB"] if n % 2 else T_["TtA"]
                        yp_ = nb()
                        P.op("pe", I("matmul", yp_[:, 0:128], Xp[:, :], Yp[:, :], start=True, stop=True), reads=[Xp, Yp], writes=[yp_])
                        P.op("act", I("copy", Yn[:, :], yp_[:, 0:128]), reads=[yp_], writes=[Yn])
                        if n < 5:
                            xp_ = nb()
                            P.op("pe", I("matmul", xp_[:, 0:128], Yp[:, :], Xp[:, :], start=True, stop=True), reads=[Xp, Yp], writes=[xp_])
                            P.op("act", I("copy", Xn[:, :], xp_[:, 0:128]), reads=[xp_], writes=[Xn])
                        cp_ = nb()
                        P.op("pe", I("matmul", cp_[:, 0:128], Yn[:, :], Tt[:, :], start=True, stop=True), reads=[Yn, Tt], writes=[cp_])
                        P.op("dve", I("tensor_tensor", Tn[:, :], cp_[:, 0:128], Tt[:, :], ALU.add), reads=[cp_, Tt], writes=[Tn])
                        Xp, Yp, Tt = Xn, Yn, Tn
                    kt_ps, vt_ps = nb(), nb()
                    P.op("pe", I("transpose", kt_ps[:, 0:128], KT[:, tsl], ident_f[:, :]), reads=[KT, ident_f], writes=[kt_ps])
                    P.op("pe", I("transpose", vt_ps[:, 0:128], VT[:, tsl], ident_f[:, :]), reads=[VT, ident_f], writes=[vt_ps])
                    P.op("dve", I("tensor_scalar", T_["kbg"][:, :], kt_ps[:, 0:128], gb[:, 7:8], None, ALU.mult), reads=[kt_ps, gb], writes=[T_["kbg"]])
                    P.op("dve", I("tensor_scalar", T_["ke"][:, :], kt_ps[:, 0:128], gb[:, 5:6], None, ALU.mult), reads=[kt_ps, gb], writes=[T_["ke"]])
                    P.op("dve", I("tensor_scalar", T_["vb"][:, :], vt_ps[:, 0:128], gb[:, 3:4], None, ALU.mult), reads=[vt_ps, gb], writes=[T_["vb"]])
                    u_ps, w_ps = nb(), nb()
                    P.op("pe", I("matmul", u_ps[:, 0:128], Tt[:, :], T_["vb"][:, :], start=True, stop=True), reads=[Tt, T_["vb"]], writes=[u_ps])
                    P.op("pe", I("matmul", w_ps[:, 0:128], T_["kbg"][:, :], Tt[:, :], start=True, stop=True), reads=[Tt, T_["kbg"]], writes=[w_ps])
                    P.op("act", I("copy", T_["u"][:, :], u_ps[:, 0:128]), reads=[u_ps], writes=[T_["u"]])
                    P.op("act", I("copy", T_["wT"][:, :], w_ps[:, 0:128]), reads=[w_ps], writes=[T_["wT"]])
                    s_prep.append(P.end_capture())
                    P.capture()
                    for c in range(2):
                        cs = slice(64 * c, 64 * c + 64)
                        vn_ps = bank[4]
                        P.op("pe", I("matmul", vn_ps[cs, 0:128], T_["wT"][:, cs], Sst[:, :], start=True, stop=True), reads=[T_["wT"], Sst], writes=[vn_ps])
                        P.op("dve", I("tensor_tensor", T_["vnew"][cs, :], T_["u"][cs, :], vn_ps[cs, 0:128], ALU.subtract), reads=[T_["u"], vn_ps], writes=[T_["vnew"]])
                        P.op("pe", [I("matmul", OPS[cs, 0:128], T_["QDT"][:, cs], Sst[:, :], start=True, stop=False),
                                    I("matmul", OPS[cs, 0:128], T_["attnT"][cs, cs], T_["vnew"][cs, :], start=False, stop=True)],
                             reads=[T_["QDT"], Sst, T_["attnT"], T_["vnew"]], writes=[OPS])
                        s_ps = bank[4]
                        P.op("pe", I("matmul", s_ps[:, 128:256], T_["ke"][cs, :], T_["vnew"][cs, :], start=True, stop=True), reads=[T_["ke"], T_["vnew"]], writes=[s_ps])
                        P.op("dve", I("scalar_tensor_tensor", Sst[:, :], Sst[:, :], decb[:, c:c + 1], s_ps[:, 128:256], ALU.mult, ALU.add), reads=[Sst, decb, s_ps], writes=[Sst])
                    P.op("act", I("activation", T_["junk"][:, :], OPS[:, 0:128], AF.Square, accum_out=gb[:, 8:9]), reads=[OPS, gb], writes=[T_["junk"], gb])
                    P.op("act", I("activation", gb[:, 9:10], gb[:, 8:9], AF.Ln, bias=EPS, scale=1.0 / 128), reads=[gb], writes=[gb])
                    P.op("act", I("activation", gb[:, 9:10], gb[:, 9:10], AF.Exp, scale=-0.5), reads=[gb], writes=[gb])
                    P.op("dve", I("scalar_tensor_tensor", T_["t1"][:, :], OPS[:, 0:128], gb[:, 9:10], ggbc[:, :], ALU.mult, ALU.mult), reads=[OPS, gb, ggbc], writes=[T_["t1"]])
                    P.op("pool", I("tensor_tensor", T_["t2"][:, :], T_["t1"][:, :], T_["sz"][:, :], ALU.mult), reads=[T_["t1"], T_["sz"]], writes=[T_["t2"]])
                    o_ps = bank[4]
                    P.op("pe", I("transpose", o_ps[:, 256:384], T_["t2"][:, :], ident_f[:, :]), reads=[T_["t2"], ident_f], writes=[o_ps])
                    P.op("act", I("copy", ys[:, 2, tsl], o_ps[:, 256:384]), reads=[o_ps], writes=[ysG])
                    s_chain.append(P.end_capture())
            if 'gdn' in stages:
                s_gdn = s_gpre + s_prep[0]
                for tl in range(4):
                    s_gdn = s_gdn + (merge_streams([s_chain[tl], s_prep[tl + 1]]) if tl < 3 else s_chain[tl])
            else:
                s_gdn = P.end_capture()
            P.replay(merge_streams([s_lru + s_att, s_gdn]))
            P.dma("sp", io["yT_out"][g // 4][:, :, (g % 4) * 512:(g % 4 + 1) * 512].rearrange("b f n -> f b n"), ys[:, :, :], ysA,
                  reads=[ysA, ysL, ysG], writes=[io["yT_buf"]], merge=True)
            if "y_ag" in io and g % 4 == 3:
                io["y_ag"](g // 4)
        P.barrier()


def consts_FN():
    return np.concatenate([np.eye(128, dtype=np.float32), np.ones((128, 128), np.float32)], 1)

def mconst():
    k = np.arange(128)[:, None]; i = np.arange(128)[None, :]
    same = (k // 64) == (i // 64)
    U2 = (same & (k <= i)).astype(np.float32)
    B2 = same.astype(np.float32)
    MLs = np.where(same & (k > i), 0.0, -3e4).astype(np.float32)
    MU = np.where(same & (i >= k), 0.0, -3e4).astype(np.float32)
    SU01 = (same & (i > k)).astype(np.float32)
    SI = ((k % 64) == (i % 64)).astype(np.float32)
    CI = np.zeros((128, 128), np.float32); CI[:64, 0] = 1; CI[64:, 1] = 1
    return np.ascontiguousarray(np.concatenate([U2, B2, MLs, MU, SU01, SI, CI], 1))

def prep_M(inp, L, b, j):
    w = inp['w_in'][L]
    kr = w[:, 768:832]
    krp = np.concatenate([kr[:, 32:64], kr[:, 0:32]], 1)
    cols = [w[:, 0:512], w[:, 512:768], kr, krp, w[:, 832 + 128 * j:832 + 128 * (j + 1)], w[:, 1344 + 128 * j:1344 + 128 * (j + 1)],
            w[:, 1856 + 128 * j:1856 + 128 * (j + 1)], w[:, 2368 + 128 * j:2368 + 128 * (j + 1)], w[:, 2880 + 128 * j:2880 + 128 * (j + 1)],
            np.repeat(w[:, 3908 + j:3909 + j], 128, axis=1)]
    win_fm = np.ascontiguousarray(np.concatenate(cols, 1))
    win_tm = np.ascontiguousarray(np.concatenate([w[:, 3392 + 128 * j:3392 + 128 * (j + 1)], w[:, 3904 + j:3905 + j], w[:, 3908 + j:3909 + j]], 1))
    uq = inp['mla_w_uq'][L][:, j * 192:(j + 1) * 192]
    wuq = np.ascontiguousarray(np.concatenate([uq, uq[:, 160:192], uq[:, 128:160]], 1))
    wukv = np.ascontiguousarray(inp['mla_w_ukv'][L][:, j * 256:(j + 1) * 256])
    vec = np.zeros((128, 32), np.float32)
    qh = inp['mla_q_head_norm'][L]; kh = inp['mla_k_head_norm'][L]
    vec[:, 0] = qh[0:128]
    vec[:, 1] = np.concatenate([qh[128:192], qh[160:192], qh[128:160]])
    vec[:, 2] = kh[0:128]
    vec[:, 3] = np.concatenate([kh[128:192], kh[160:192], kh[128:160]])
    vec[:, 4] = np.concatenate([np.ones(64), -np.ones(32), np.ones(32)])
    cs = slice(128 * j, 128 * (j + 1))
    vec[:, 5] = inp['lru_conv_b'][L][cs]
    for k in range(4):
        vec[:, 6 + k] = inp['lru_conv_w'][L][k, cs]
    vec[:, 10] = inp['lru_b_a'][L][cs]; vec[:, 11] = inp['lru_b_x'][L][cs]; vec[:, 12] = inp['lru_lambda'][L][cs]
    gw = inp['gdn_conv_w'][L]
    for k in range(4):
        vec[:, 13 + k] = gw[k, 128 * j:128 * (j + 1)]
        vec[:, 17 + k] = gw[k, 512 + 128 * j:512 + 128 * (j + 1)]
        vec[:, 21 + k] = gw[k, 1024 + 128 * j:1024 + 128 * (j + 1)]
    vec[:, 25] = inp['gdn_a_log'][L][j]; vec[:, 26] = inp['gdn_dt_bias'][L][j]
    invf = (np.float32(10000.0) ** (-np.arange(0, 64, 2, dtype=np.float32) / np.float32(64))).astype(np.float32)
    vec[:, 27] = np.tile(invf, 4)
    vec[:, 28] = np.concatenate([np.full(64, 0.25), np.zeros(64)])
    wa = np.zeros((128, 128), np.float32); wx = np.zeros((128, 128), np.float32)
    for t in range(2):
        wa[64 * t:64 * (t + 1), 64 * t:64 * (t + 1)] = inp['lru_w_a'][L][2 * j + t]
        wx[64 * t:64 * (t + 1), 64 * t:64 * (t + 1)] = inp['lru_w_x'][L][2 * j + t]
    return dict(win_fm=win_fm, win_tm=win_tm, wuq=wuq, wukv=wukv, vecs=vec,
                gq=np.ascontiguousarray(inp['mla_q_norm'][L].reshape(4, 128).T), gkv=np.ascontiguousarray(inp['mla_kv_norm'][L].reshape(2, 128).T),
                wa_bd=wa, wx_bd=wx, gdn_gain_bc=np.ascontiguousarray(np.broadcast_to(inp['gdn_out_norm'][L], (128, 128))),
                posb=np.ascontiguousarray(np.broadcast_to(inp['positions'][b].astype(np.int32), (128, 8192))), mconst=mconst())

M_SHAPES = dict(win_fm=([1024, 1664], 'f'), win_tm=([1024, 130], 'f'), wuq=([512, 256], 'f'), wukv=([256, 256], 'f'), vecs=([128, 32], 'f'),
                gq=([128, 4], 'f'), gkv=([128, 2], 'f'), wa_bd=([128, 128], 'f'), wx_bd=([128, 128], 'f'), gdn_gain_bc=([128, 128], 'f'),
                posb=([128, 8192], 'i'), mconst=([128, 896], 'f'))


BF_NP = ml_dtypes.bfloat16
N_CORES = 8
DEPTH_L = 2
GROUPS = [[0, 1, 2, 3], [4, 5, 6, 7]]
SHARED_M = ("posb", "mconst")


def _din(nc, name, shape, dt=F32):
    return nc.dram_tensor(name, list(shape), dt, kind="ExternalInput").ap()


def _dout(nc, name, shape, dt=F32):
    return nc.dram_tensor(name, list(shape), dt, kind="ExternalOutput").ap()


def _dint(nc, name, shape, dt):
    return nc.dram_tensor(name, list(shape), dt, kind="Internal").ap()


def build_fused():
    nc = bass.Bass("TRN2", target_bir_lowering=False)
    P = Prog(nc)
    x_in = _din(nc, "xloc", [2048, 1024])
    x_out = _dout(nc, "xout", [2048, 1024])
    yidx = _din(nc, "yidx", [128, 12], I32)
    gains = [_din(nc, "mix_gain_bc_%d" % L, [128, 1024]) for L in range(DEPTH_L)]
    shared = {k: _din(nc, k, M_SHAPES[k][0], F32 if M_SHAPES[k][1] == 'f' else I32) for k in SHARED_M}
    hx = _dint(nc, "hx", [4, 1024, 512], BF16)
    hall = _dint(nc, "hall", [4, 4096, 512], BF16)
    ycin = _dint(nc, "ycin", [4, 3, 128, 2048], BF16)
    yall = _dint(nc, "yall", [12, 512, 2048], BF16)
    xmid = _dint(nc, "xmid", [2048, 1024], F32)
    bufs = {k: P.reg(Buf(k)) for k in ["xloc_buf", "yrecv_buf", "xout_buf", "hT_buf", "yT_buf"]}
    C = consts(P, _din(nc, "consts", [128, 256]))

    def gather_h():
        P.barrier()
        for k in range(4):
            P.allgather(hx[k], hall[k], GROUPS)
        return (P.ccsem, P.cccnt)

    io = dict(bufs)
    io.update(xloc=x_in, mix_gain_bc=gains[0], hx=hx)
    phase_N0(P, C, io)
    hw = gather_h()
    for L in range(DEPTH_L):
        last = L == DEPTH_L - 1
        moe = L % 2 == 1
        io = dict(bufs)
        for k, (shp, t) in M_SHAPES.items():
            io[k] = shared[k] if k in SHARED_M else _din(nc, "%s_%d" % (k, L), shp, F32 if t == 'f' else I32)
        def y_ag(qq):
            waits = P._deps("pool", [bufs["yT_buf"]], [])
            for br in range(3):
                P.allgather(ycin[qq][br], yall[qq * 3 + br], GROUPS, waits=waits if br == 0 else ())
        io.update(hall=hall, yT_out=ycin, y_ag=y_ag, h_wait=hw)
        phase_M(P, C, io)
        P.barrier()
        E, FF = (8, 3584) if moe else (1, 2816)
        io = dict(bufs)
        io.update(xloc=x_in if L == 0 else xmid, xout=x_out if last else xmid, yall=yall.rearrange("e r n -> (e r) n"), yidx=yidx,
                  w_out_r=_din(nc, "w_out_r_%d" % L, [3, 4, 128, 1024]), g_mla=_din(nc, "g_mla_%d" % L, [128, 4]),
                  g_lru=_din(nc, "g_lru_%d" % L, [128, 4]), ffn_gain_bc=_din(nc, "ffn_gain_bc_%d" % L, [128, 1024]),
                  wg=_din(nc, "wg_%d" % L, [E, 1024, FF]), wu=_din(nc, "wu_%d" % L, [E, 1024, FF]), wd=_din(nc, "wd_%d" % L, [E, FF, 1024]))
        if moe:
            io["router"] = _din(nc, "router_%d" % L, [1024, 8])
        if not last:
            io.update(mix_gain_bc=gains[L + 1], hx=hx)
        phase_F(P, C, io, dict(moe=moe, n_exp=E, d_ff=FF, last=last))
        if not last:
            hw = gather_h()
    P.barrier()
    P.emit()
    return nc


def _bc(v, n):
    return np.ascontiguousarray(np.broadcast_to(np.asarray(v, np.float32), (128, n)))


def kernel(**inputs):
    inp = {k: np.asarray(v) for k, v in inputs.items()}
    cores = list(range(N_CORES))
    cst = consts_FN()
    x = inp['x'].astype(np.float32, copy=False)
    ins = []
    for c in cores:
        b, q = divmod(c, 4)
        d = dict(xloc=np.ascontiguousarray(x[b, 2048 * q:2048 * (q + 1)]), consts=cst)
        f = np.arange(128, dtype=np.int32)[:, None]
        col = np.arange(12, dtype=np.int32)[None, :]
        d["yidx"] = np.ascontiguousarray((((q * 3 + col // 4) * 4 + col % 4) * 128 + f).astype(np.int32))
        for L in range(DEPTH_L):
            moe = L % 2 == 1
            dm = prep_M(inp, L, b, q)
            for k, v in dm.items():
                d[k if k in SHARED_M else "%s_%d" % (k, L)] = v
            d["mix_gain_bc_%d" % L] = _bc(inp['mix_norm'][L], 1024)
            d["w_out_r_%d" % L] = np.ascontiguousarray(inp['w_out'][L].reshape(3, 4, 128, 1024))
            d["g_mla_%d" % L] = np.ascontiguousarray(inp['mla_out_norm'][L].reshape(4, 128).T)
            d["g_lru_%d" % L] = np.ascontiguousarray(inp['lru_out_norm'][L].reshape(4, 128).T)
            d["ffn_gain_bc_%d" % L] = _bc(inp['ffn_norm'][L], 1024)
            if moe:
                d["wg_%d" % L] = inp['moe_w_gate'][L // 2]
                d["wu_%d" % L] = inp['moe_w_up'][L // 2]
                d["wd_%d" % L] = inp['moe_w_down'][L // 2]
                d["router_%d" % L] = inp['router_w'][L // 2]
            else:
                d["wg_%d" % L] = inp['dense_w_gate'][L // 2][None]
                d["wu_%d" % L] = inp['dense_w_up'][L // 2][None]
                d["wd_%d" % L] = inp['dense_w_down'][L // 2][None]
        ins.append(d)
    res = run_bass_kernel_spmd(build_fused(), ins, core_ids=cores)
    out = np.empty((2, 8192, 1024), np.float32)
    for c in cores:
        out[c // 4, 2048 * (c % 4):2048 * (c % 4 + 1)] = np.asarray(res.results[c]["xout"])
    return out
```
